# Optimizing a Trainium2 kernel written in Bass

```python
import math
import jax
import jax.numpy as jnp
from jax import lax
import numpy as np

D_MODEL = 1024
BATCH = 2
SEQ = 8192
DEPTH = 1

HEAD_DIM = 64
SB_HEADS = 8
DIL_PATTERNS = ((128, 1), (512, 4), (2048, 16))
DIL_HEADS_PER_GROUP = 4
DIL_HEADS = DIL_HEADS_PER_GROUP * len(DIL_PATTERNS)
Q_BLOCK = 128
SB_WIDTH = SB_HEADS * HEAD_DIM
DIL_WIDTH = DIL_HEADS * HEAD_DIM
DIL_OUT_WIDTH = DIL_HEADS_PER_GROUP * HEAD_DIM
IN_SPLITS = (SB_WIDTH, SB_WIDTH, SB_WIDTH, DIL_WIDTH, DIL_WIDTH, DIL_WIDTH, D_MODEL, D_MODEL)
IN_WIDTH = int(sum(IN_SPLITS))
IN_SPLIT_POINTS = tuple(int(v) for v in np.cumsum(IN_SPLITS)[:-1])

REL_BUCKETS = 32
REL_MAX_DISTANCE = 2048

N_EXPERTS = 256
TOP_K = 8
N_GROUPS = 8
TOPK_GROUPS = 4
EXPERT_HIDDEN = 256
SHARED_HIDDEN = 256
ROUTED_SCALE = 2.5
EXPERT_BLOCK = 128

LN_EPS = 1e-5
DN_ALPHA = (2 * DEPTH) ** 0.25
DN_BETA = (8 * DEPTH) ** -0.25

kernel_name = "hybrid_stickbreak_dilated_moe_block"


def _layer_norm(x, g, b):
    xf = x.astype(jnp.float32)
    mu = jnp.mean(xf, axis=-1, keepdims=True)
    var = jnp.mean(jnp.square(xf - mu), axis=-1, keepdims=True)
    y = (xf - mu) * lax.rsqrt(var + LN_EPS)
    return (y * g.astype(jnp.float32) + b.astype(jnp.float32)).astype(x.dtype)


def _split_heads(t, n_heads):
    b, s, _ = t.shape
    return t.reshape(b, s, n_heads, HEAD_DIM).transpose(0, 2, 1, 3)


def _merge_heads(t):
    b, h, s, d = t.shape
    return t.transpose(0, 2, 1, 3).reshape(b, s, h * d)


def _to_query_blocks(t):
    b, h, s, d = t.shape
    return t.reshape(b, h, s // Q_BLOCK, Q_BLOCK, d).transpose(2, 0, 1, 3, 4)


def _from_query_blocks(t):
    nb, b, h, q, d = t.shape
    return t.transpose(1, 2, 0, 3, 4).reshape(b, h, nb * q, d)


def _stick_breaking_attention(q, k, v):
    s = q.shape[2]
    scale = 1.0 / math.sqrt(HEAD_DIM)
    key_pos = jnp.arange(s, dtype=jnp.int32)

    def one_block(args):
        q_blk, blk = args
        q_pos = blk * Q_BLOCK + jnp.arange(Q_BLOCK, dtype=jnp.int32)
        before = key_pos[None, :] < q_pos[:, None]
        z = jnp.einsum('bhqd,bhkd->bhqk', q_blk, k).astype(jnp.float32) * scale
        log_keep = jnp.where(before, jax.nn.log_sigmoid(-z), 0.0)
        log_keep_between = lax.cumsum(log_keep, axis=3, reverse=True) - log_keep
        weight = jnp.where(before, jnp.exp(jax.nn.log_sigmoid(z) + log_keep_between), 0.0)
        return jnp.einsum('bhqk,bhkd->bhqd', weight.astype(v.dtype), v)

    n_blocks = s // Q_BLOCK
    out = lax.map(one_block, (_to_query_blocks(q), jnp.arange(n_blocks, dtype=jnp.int32)))
    return _from_query_blocks(out)


def _rel_bucket(dist):
    max_exact = REL_BUCKETS // 2
    d = jnp.maximum(dist, 1).astype(jnp.float32)
    large = max_exact + (jnp.log(d / max_exact) / math.log(REL_MAX_DISTANCE / max_exact)
                         * (REL_BUCKETS - max_exact)).astype(jnp.int32)
    large = jnp.minimum(large, REL_BUCKETS - 1)
    return jnp.where(dist < max_exact, dist, large)


def _dilated_attention(q, k, v, rel_bias):
    s = q.shape[2]
    scale = 1.0 / math.sqrt(HEAD_DIM)
    n_blocks = s // Q_BLOCK
    groups = []
    for g, (window, dilation) in enumerate(DIL_PATTERNS):
        offsets = jnp.arange(window // dilation + 1, dtype=jnp.int32) * dilation
        heads = slice(g * DIL_HEADS_PER_GROUP, (g + 1) * DIL_HEADS_PER_GROUP)
        bias = rel_bias[_rel_bucket(offsets)][:, heads].T.astype(jnp.float32)
        groups.append((heads, offsets, bias, k[:, heads], v[:, heads]))

    def one_block(args):
        q_blk, blk = args
        q_pos = blk * Q_BLOCK + jnp.arange(Q_BLOCK, dtype=jnp.int32)
        outs, lses = [], []
        for heads, offsets, bias, k_g, v_g in groups:
            idx = q_pos[:, None] - offsets[None, :]
            valid = idx >= 0
            idx = jnp.maximum(idx, 0)
            k_sel = jnp.take(k_g, idx, axis=2)
            v_sel = jnp.take(v_g, idx, axis=2)
            logits = (jnp.einsum('bhqd,bhqmd->bhqm', q_blk[:, heads], k_sel).astype(jnp.float32) * scale
                      + bias[:, None, :])
            logits = jnp.where(valid, logits, -jnp.inf)
            lse = jax.nn.logsumexp(logits, axis=-1)
            probs = jnp.exp(logits - lse[..., None])
            outs.append(jnp.einsum('bhqm,bhqmd->bhqd', probs.astype(v_g.dtype), v_sel).astype(jnp.float32))
            lses.append(lse)
        mix = jax.nn.softmax(jnp.stack(lses), axis=0)
        out = jnp.sum(mix[..., None] * jnp.stack(outs), axis=0)
        return out.astype(q_blk.dtype)

    out = lax.map(one_block, (_to_query_blocks(q), jnp.arange(n_blocks, dtype=jnp.int32)))
    return _from_query_blocks(out)


def _mixer(h, w_in, b_gate, w_br_sb, w_br_dil, w_out, rel_bias):
    proj = jnp.einsum('bsd,df->bsf', h, w_in)
    q_sb, k_sb, v_sb, q_dl, k_dl, v_dl, g_sb, g_dl = jnp.split(proj, IN_SPLIT_POINTS, axis=-1)
    o_sb = _stick_breaking_attention(_split_heads(q_sb, SB_HEADS), _split_heads(k_sb, SB_HEADS),
                                     _split_heads(v_sb, SB_HEADS))
    o_dl = _dilated_attention(_split_heads(q_dl, DIL_HEADS), _split_heads(k_dl, DIL_HEADS),
                              _split_heads(v_dl, DIL_HEADS), rel_bias)
    gates = jax.nn.sigmoid(jnp.concatenate([g_sb, g_dl], axis=-1) + b_gate)
    gate_sb, gate_dl = jnp.split(gates, 2, axis=-1)
    merged = (gate_sb * jnp.einsum('bsf,fd->bsd', _merge_heads(o_sb), w_br_sb)
              + gate_dl * jnp.einsum('bsf,fd->bsd', _merge_heads(o_dl), w_br_dil))
    return jnp.einsum('bsd,de->bse', merged, w_out)


def _route(h_flat, w_router, router_bias):
    n_tok = h_flat.shape[0]
    scores = jax.nn.sigmoid(h_flat.astype(jnp.float32) @ w_router.astype(jnp.float32))
    biased = scores + router_bias.astype(jnp.float32)
    grp = biased.reshape(n_tok, N_GROUPS, N_EXPERTS // N_GROUPS)
    grp_score = jnp.sum(lax.top_k(grp, 2)[0], axis=-1)
    _, top_groups = lax.top_k(grp_score, TOPK_GROUPS)
    grp_mask = jnp.any(top_groups[..., None] == jnp.arange(N_GROUPS, dtype=top_groups.dtype), axis=1)
    expert_mask = jnp.repeat(grp_mask, N_EXPERTS // N_GROUPS, axis=1)
    _, idx = lax.top_k(jnp.where(expert_mask, biased, -jnp.inf), TOP_K)
    w = jnp.take_along_axis(scores, idx, axis=1)
    w = w / jnp.sum(w, axis=-1, keepdims=True) * ROUTED_SCALE
    return idx.astype(jnp.int32), w


def _routed_experts(h_flat, idx, w, w_gate_e, w_up_e, w_down_e):
    n_tok, d = h_flat.shape
    n_assign = n_tok * TOP_K
    e_flat = idx.reshape(-1)
    tok_flat = jnp.arange(n_assign, dtype=jnp.int32) // TOP_K
    g_flat = w.reshape(-1)
    order = jnp.argsort(e_flat)
    e_sorted = e_flat[order]
    counts = jnp.zeros((N_EXPERTS,), jnp.int32).at[e_flat].add(1)
    padded = ((counts + EXPERT_BLOCK - 1) // EXPERT_BLOCK) * EXPERT_BLOCK
    starts = jnp.cumsum(counts) - counts
    padded_ends = jnp.cumsum(padded)
    padded_starts = padded_ends - padded
    rank = jnp.arange(n_assign, dtype=jnp.int32) - starts[e_sorted]
    dest = padded_starts[e_sorted] + rank
    n_blocks = (n_assign + EXPERT_BLOCK - 1) // EXPERT_BLOCK + N_EXPERTS
    n_slots = n_blocks * EXPERT_BLOCK
    slot_tok = jnp.zeros((n_slots,), jnp.int32).at[dest].set(tok_flat[order])
    slot_gate = jnp.zeros((n_slots,), g_flat.dtype).at[dest].set(g_flat[order])
    block_start = jnp.arange(n_blocks, dtype=jnp.int32) * EXPERT_BLOCK
    block_expert = jnp.minimum(jnp.searchsorted(padded_ends, block_start, side='right'),
                               N_EXPERTS - 1).astype(jnp.int32)

    def run_block(args):
        toks, e = args
        xb = h_flat[toks]
        hid = jax.nn.silu(xb @ w_gate_e[e]) * (xb @ w_up_e[e])
        return hid @ w_down_e[e]

    y = lax.map(run_block, (slot_tok.reshape(n_blocks, EXPERT_BLOCK), block_expert))
    y = y.reshape(n_slots, d) * slot_gate[:, None]
    return jax.ops.segment_sum(y, slot_tok, num_segments=n_tok).astype(h_flat.dtype)


def _moe(h, w_router, router_bias, w_gate_e, w_up_e, w_down_e, w_gate_s, w_up_s, w_down_s):
    b, s, d = h.shape
    h_flat = h.reshape(b * s, d)
    idx, w = _route(h_flat, w_router, router_bias)
    routed = _routed_experts(h_flat, idx, w, w_gate_e, w_up_e, w_down_e)
    shared = (jax.nn.silu(h_flat @ w_gate_s) * (h_flat @ w_up_s)) @ w_down_s
    return (routed + shared).reshape(b, s, d)


def setup_inputs(seed: int = 0) -> dict:
    key = jax.random.key(seed)
    ks = jax.random.split(key, 21)
    L, D, E, H, HS = DEPTH, D_MODEL, N_EXPERTS, EXPERT_HIDDEN, SHARED_HIDDEN
    f32 = jnp.float32
    col_scale = jnp.concatenate([
        jnp.full((n,), c, f32) for n, c in zip(
            IN_SPLITS, (1.0, 1.0, DN_BETA, 1.0, 1.0, DN_BETA, 1.0, 1.0))]) * D ** -0.5
    return {
        "x": jax.random.normal(ks[0], (BATCH, SEQ, D), f32),
        "ln_in_g": 1.0 + 0.02 * jax.random.normal(ks[1], (D,), f32),
        "ln_in_b": 0.02 * jax.random.normal(ks[2], (D,), f32),
        "rel_bias": 0.2 * jax.random.normal(ks[3], (REL_BUCKETS, DIL_HEADS), f32),
        "w_in": jax.random.normal(ks[4], (L, D, IN_WIDTH), f32) * col_scale,
        "b_gate": 0.02 * jax.random.normal(ks[5], (L, 2 * D), f32),
        "w_br_sb": jax.random.normal(ks[6], (L, SB_WIDTH, D), f32) * (DN_BETA * SB_WIDTH ** -0.5),
        "w_br_dil": jax.random.normal(ks[7], (L, DIL_OUT_WIDTH, D), f32) * (DN_BETA * DIL_OUT_WIDTH ** -0.5),
        "w_out": jax.random.normal(ks[8], (L, D, D), f32) * (DN_BETA * D ** -0.5),
        "ln1_g": 1.0 + 0.02 * jax.random.normal(ks[9], (L, D), f32),
        "ln1_b": 0.02 * jax.random.normal(ks[10], (L, D), f32),
        "w_router": jax.random.normal(ks[11], (L, D, E), f32) * D ** -0.5,
        "router_bias": 0.01 * jax.random.normal(ks[12], (L, E), f32),
        "w_gate_e": jax.random.normal(ks[13], (L, E, D, H), f32) * (DN_BETA * D ** -0.5),
        "w_up_e": jax.random.normal(ks[14], (L, E, D, H), f32) * (DN_BETA * D ** -0.5),
        "w_down_e": jax.random.normal(ks[15], (L, E, H, D), f32) * (DN_BETA * H ** -0.5),
        "w_gate_s": jax.random.normal(ks[16], (L, D, HS), f32) * (DN_BETA * D ** -0.5),
        "w_up_s": jax.random.normal(ks[17], (L, D, HS), f32) * (DN_BETA * D ** -0.5),
        "w_down_s": jax.random.normal(ks[18], (L, HS, D), f32) * (DN_BETA * HS ** -0.5),
        "ln2_g": 1.0 + 0.02 * jax.random.normal(ks[19], (L, D), f32),
        "ln2_b": 0.02 * jax.random.normal(ks[20], (L, D), f32),
    }


def reference(x, ln_in_g, ln_in_b, rel_bias, w_in, b_gate, w_br_sb, w_br_dil, w_out, ln1_g, ln1_b,
              w_router, router_bias, w_gate_e, w_up_e, w_down_e, w_gate_s, w_up_s, w_down_s,
              ln2_g, ln2_b):
    h = _layer_norm(x, ln_in_g, ln_in_b)
    for l in range(DEPTH):
        mix = _mixer(h, w_in[l], b_gate[l], w_br_sb[l], w_br_dil[l], w_out[l], rel_bias)
        h = _layer_norm(DN_ALPHA * h + mix, ln1_g[l], ln1_b[l])
        ffn = _moe(h, w_router[l], router_bias[l], w_gate_e[l], w_up_e[l], w_down_e[l],
                   w_gate_s[l], w_up_s[l], w_down_s[l])
        h = _layer_norm(DN_ALPHA * h + ffn, ln2_g[l], ln2_b[l])
    return h
```

```python
import contextlib
import numpy as np
import ml_dtypes
import concourse.bass as bass
import concourse.mybir as mybir
from concourse.bass_utils import run_bass_kernel_spmd

F32 = mybir.dt.float32
BF16 = mybir.dt.bfloat16
I32 = mybir.dt.int32
U32 = mybir.dt.uint32
AF = mybir.ActivationFunctionType
ALU = mybir.AluOpType
AX = mybir.AxisListType

NCORES = 8
D = 1024
S = 8192
NB = 64
NOWN = 16
TOWN = NOWN * 128
LN_EPS = 1e-5
ALPHA = 2.0 ** 0.25
NEG = -30000.0
NEXP = 256
CAP = 256
TOPK = 8
VW = 512 + 12 * 65


class Op:
    __slots__ = ("eng", "fn", "deps", "chan", "sig", "count", "idx", "chan_count")


class Prog:
    ENGS = ("tensor", "vector", "scalar", "gpsimd", "sync")

    def __init__(self, nc, stack):
        self.nc = nc
        self.stack = stack
        self.sems = {e: stack.enter_context(nc.semaphore("sem_" + e)) for e in self.ENGS}
        self.sig_total = {e: 0 for e in self.ENGS}
        self.chan_sem = {}
        self.chan_total = {}
        self.reset_phase()

    def reset_phase(self):
        self.ops = []
        self.last_w = {}
        self.readers = {}
        self.chan_emitted = dict(self.chan_total)

    def chan(self, name):
        if name not in self.chan_sem:
            self.chan_sem[name] = self.stack.enter_context(self.nc.semaphore("ch_" + name))
            self.chan_total[name] = 0
            self.chan_emitted[name] = 0
        return name

    def add(self, eng, fn, r=(), w=(), chan=None):
        op = Op()
        op.eng = eng
        op.fn = fn
        op.chan = chan
        op.sig = False
        op.idx = len(self.ops)
        deps = set()
        for k in r:
            lw = self.last_w.get(k)
            if lw is not None:
                deps.add(lw)
        for k in w:
            lw = self.last_w.get(k)
            if lw is not None:
                deps.add(lw)
            for rd in self.readers.get(k, ()):
                deps.add(rd)
        deps.discard(op.idx)
        op.deps = []
        for d in deps:
            dop = self.ops[d]
            if dop.chan is not None:
                op.deps.append(("chan", dop.chan, self.chan_emitted[dop.chan]))
            else:
                if dop.eng == "tensor" and eng == "tensor":
                    continue
                dop.sig = True
                op.deps.append(("eng", dop.eng, dop))
        if chan is not None:
            self.chan(chan)
            self.chan_emitted[chan] += 16
            op.chan_count = self.chan_emitted[chan]
        self.ops.append(op)
        for k in r:
            self.readers.setdefault(k, []).append(op.idx)
        for k in w:
            self.last_w[k] = op.idx
            self.readers[k] = []
        return op

    def emit_phase(self):
        nc = self.nc
        last = {}
        for op in self.ops:
            if op.chan is None:
                last[op.eng] = op
        for op in last.values():
            op.sig = True
        tot = dict(self.sig_total)
        for op in self.ops:
            if op.chan is None and op.sig:
                tot[op.eng] += 1
                op.count = tot[op.eng]
        final_eng = dict(tot)
        final_chan = dict(self.chan_emitted)
        per_eng = {e: [o for o in self.ops if o.eng == e] for e in self.ENGS}
        sems = self.sems
        chan_sem = self.chan_sem

        def run(e, eobj):
            waited = {}

            def wait(kind, name, val):
                key = (kind, name)
                if waited.get(key, -1) >= val:
                    return
                waited[key] = val
                eobj.wait_ge(sems[name] if kind == "eng" else chan_sem[name], val)

            for op in per_eng[e]:
                for kind, name, v in op.deps:
                    wait(kind, name, v.count if kind == "eng" else v)
                ins = op.fn(eobj)
                if op.chan is not None:
                    ins.then_inc(chan_sem[op.chan], 16)
                elif op.sig:
                    ins.then_inc(sems[e], 1)
            for name, v in final_chan.items():
                if v > 0:
                    wait("chan", name, v)
            for name, v in final_eng.items():
                if v > 0 and name != e:
                    wait("eng", name, v)

        with nc.Block() as block:
            @block.tensor
            def _(e):
                run("tensor", e)

            @block.vector
            def _(e):
                run("vector", e)

            @block.scalar
            def _(e):
                run("scalar", e)

            @block.gpsimd
            def _(e):
                run("gpsimd", e)

            @block.sync
            def _(e):
                run("sync", e)

        self.sig_total = final_eng
        self.chan_total = final_chan
        self.reset_phase()


class Ctx:
    pass


def phase_a(P, nc, g, ntg=16):
    st = contextlib.ExitStack()
    with st:
        sb = lambda name, shape, dt: st.enter_context(nc.sbuf_tensor(name, shape, dt))
        ps = lambda name, shape, dt: st.enter_context(nc.psum_tensor(name, shape, dt))
        winb = sb("a_winb", [128, 8, 3840], BF16)
        gT = sb("a_gT", [128, 8], F32)
        bT = sb("a_bT", [128, 8], F32)
        grep = sb("a_grep", [128, 1024], F32)
        brep = sb("a_brep", [128, 1024], F32)
        valid = sb("a_valid", [128, NB], F32)
        ident = sb("a_ident", [128, 128], BF16)
        NX = 3
        xt = [sb(f"a_x{i}", [128, 1024], F32) for i in range(NX)]
        stats = [sb(f"a_stats{i}", [128, 2, 6], F32) for i in range(2)]
        mv = [sb(f"a_mv{i}", [128, 2], F32) for i in range(2)]
        rstd = [sb(f"a_rstd{i}", [128, 1], F32) for i in range(2)]
        std = [sb(f"a_std{i}", [128, 1], F32) for i in range(2)]
        ybf = [sb(f"a_ybf{i}", [128, 1024], BF16) for i in range(2)]
        y32 = sb("a_y32", [128, 1024], F32)
        hTg = [sb(f"a_hTg{i}", [128, 8, 512], BF16) for i in range(2)]
        kst = [sb(f"a_kst{i}", [128, 512], BF16) for i in range(3)]
        vst = [sb(f"a_vst{i}", [128, VW], BF16) for i in range(2)]
        qst = [sb(f"a_qst{i}", [128, 10, 128], BF16) for i in range(2)]
        ones12 = sb("a_ones12", [128, 12, 1], F32)
        tp = [ps(f"a_tp{i}", [128, 8, 128], BF16) for i in range(2)]
        pm = [ps(f"a_pm{i}", [128, 512], F32) for i in range(4)]

        for dc in range(8):
            P.add("gpsimd", lambda e, dc=dc: e.dma_start(
                out=winb[:, dc, :], in_=g.w_in[dc * 128:(dc + 1) * 128, 0:3840]),
                w=[("winb", dc)], chan="a_w")
        P.add("sync", lambda e: e.dma_start(out=gT[:], in_=g.ln_in_gT),
              w=["gT"], chan="a_c")
        P.add("sync", lambda e: e.dma_start(out=bT[:], in_=g.ln_in_bT),
              w=["bT"], chan="a_c")
        P.add("sync", lambda e: e.dma_start(out=grep[:], in_=g.ln_in_g.partition_broadcast(128)),
              w=["grep"], chan="a_c")
        P.add("sync", lambda e: e.dma_start(out=brep[:], in_=g.ln_in_b.partition_broadcast(128)),
              w=["brep"], chan="a_c")
        P.add("sync", lambda e: e.dma_start(out=valid[:], in_=g.valid), w=["valid"], chan="a_c")
        P.add("sync", lambda e: e.dma_start(out=ident[:], in_=g.ident), w=["ident"], chan="a_c")

        P.add("vector", lambda e: e.memset(ones12[:], 1.0), w=["ones12"])
        kcols = [512 + 128 * i for i in range(4)] + [2304 + 128 * i for i in range(6)]
        qcols = [0 + 128 * i for i in range(4)] + [1536 + 128 * i for i in range(6)]
        vgroups = [(1024, 512, 0), (3072, 512, 512), (3584, 256, 1024)]
        cnt = {'pmi': 0, 'ksi': 0}

        def lnt(tg, bi):
            hT = hTg[tg % 2]
            hk = ("hTg", tg % 2)
            c = 4 * tg + bi
            xs = c % NX
            s2 = c % 2
            x_t = xt[xs]
            P.add("sync", lambda e, x_t=x_t, c=c: e.dma_start(
                out=x_t[:], in_=g.x_ctx[c * 128:(c + 1) * 128, :]),
                w=[("x", xs)], chan=f"a_x{xs}")
            for hh in range(2):
                P.add("vector", lambda e, x_t=x_t, s2=s2, hh=hh: e.bn_stats(
                    out=stats[s2][:, hh, :], in_=x_t[:, hh * 512:(hh + 1) * 512]),
                    r=[("x", xs)], w=[("stats", s2, hh)])
            P.add("vector", lambda e, s2=s2: e.bn_aggr(
                out=mv[s2][:], in_=stats[s2][:].rearrange("p a b -> p (a b)")),
                r=[("stats", s2, 0), ("stats", s2, 1)], w=[("mv", s2)])
            P.add("scalar", lambda e, s2=s2: e.activation(
                out=std[s2][:], in_=mv[s2][:, 1:2], func=AF.Sqrt, bias=LN_EPS),
                r=[("mv", s2)], w=[("std", s2)])
            P.add("vector", lambda e, s2=s2: e.reciprocal(out=rstd[s2][:], in_=std[s2][:]),
                r=[("std", s2)], w=[("rstd", s2)])
            P.add("vector", lambda e, x_t=x_t, s2=s2: e.tensor_scalar(
                out=ybf[s2][:], in0=x_t[:], scalar1=mv[s2][:, 0:1], scalar2=rstd[s2][:, 0:1],
                op0=ALU.subtract, op1=ALU.mult),
                r=[("x", xs), ("mv", s2), ("rstd", s2)], w=[("ybf", s2)])
            if bi == 3:
                so = tg
                P.add("gpsimd", lambda e, x_t=x_t, s2=s2: e.tensor_scalar(
                    out=y32[:], in0=x_t[:], scalar1=mv[s2][:, 0:1], scalar2=rstd[s2][:, 0:1],
                    op0=ALU.subtract, op1=ALU.mult),
                    r=[("x", xs), ("mv", s2), ("rstd", s2)], w=["y32"])
                P.add("gpsimd", lambda e: e.tensor_tensor(out=y32[:], in0=y32[:], in1=grep[:], op=ALU.mult),
                      r=["y32", "grep"], w=["y32"])
                P.add("gpsimd", lambda e: e.tensor_tensor(out=y32[:], in0=y32[:], in1=brep[:], op=ALU.add),
                      r=["y32", "brep"], w=["y32"])
                P.add("gpsimd", lambda e, so=so: e.dma_start(
                    out=g.h_own_d[so * 128:(so + 1) * 128, :], in_=y32[:]),
                    r=["y32"], w=[("h_own_d", so)], chan="a_y32")

        def tr(tg, bi):
            hT = hTg[tg % 2]
            hk = ("hTg", tg % 2)
            c = 4 * tg + bi
            s2 = c % 2
            tps = tp[c % 2]
            for dc in range(8):
                P.add("tensor", lambda e, tps=tps, s2=s2, dc=dc: e.transpose(
                    out=tps[:, dc, :], in_=ybf[s2][:, dc * 128:(dc + 1) * 128], identity=ident[:]),
                    r=[("ybf", s2), "ident"], w=[("tp", c % 2)])
            for dc in range(8):
                P.add("scalar", lambda e, tps=tps, dc=dc, hT=hT, bi=bi: e.activation(
                    out=hT[:, dc, bi * 128:(bi + 1) * 128], in_=tps[:, dc, :], func=AF.Identity,
                    scale=gT[:, dc:dc + 1], bias=bT[:, dc:dc + 1]),
                    r=[("tp", c % 2), "gT", "bT"], w=[hk + (bi,)])

        def mm(tg):
            hT = hTg[tg % 2]
            hk = ("hTg", tg % 2)
            pmi = cnt['pmi']
            ksi = cnt['ksi']
            hkall = [hk + (bi,) for bi in range(4)]
            P.add("gpsimd", lambda e, hT=hT, tg=tg: e.dma_start(
                out=g.hT_own_d[:, :, tg * 128:(tg + 1) * 128], in_=hT[:, :, 384:512]),
                r=[hk + (3,)], w=[("hT_own_d", tg)], chan=f"a_hT{tg % 2}")
            for kc in range(10):
                pmt = pm[pmi % 4]
                pk = ("pm", pmi % 4)
                pmi += 1
                for dc in range(8):
                    P.add("tensor", lambda e, pmt=pmt, dc=dc, kc=kc, hT=hT: e.matmul(
                        pmt[:], lhsT=winb[:, dc, kcols[kc]:kcols[kc] + 128], rhs=hT[:, dc, :],
                        start=(dc == 0), stop=(dc == 7)),
                        r=[("winb", dc)] + hkall, w=[pk])
                ks = kst[ksi % 3]
                kk = ("kst", ksi % 3)
                ksi_l = ksi % 3
                ksi += 1
                eng = "scalar" if kc % 2 == 0 else "vector"
                if eng == "scalar":
                    P.add("scalar", lambda e, ks=ks, pmt=pmt: e.activation(out=ks[:], in_=pmt[:], func=AF.Copy),
                          r=[pk], w=[kk])
                else:
                    P.add("vector", lambda e, ks=ks, pmt=pmt: e.tensor_copy(out=ks[:], in_=pmt[:]),
                          r=[pk], w=[kk])
                P.add("gpsimd", lambda e, ks=ks, kc=kc, tg=tg: e.dma_start(
                    out=g.kT_d[kc, :, tg * 512:(tg + 1) * 512], in_=ks[:]),
                    r=[kk], w=[("kT_d", kc, tg)], chan=f"a_kst{ksi_l}")
                yield
            for bi in range(4):
                c = 4 * tg + bi
                vs = vst[c % 2]
                vk = ("vst", c % 2)
                for (c0, ncol, o0) in vgroups:
                    pmt = pm[pmi % 4]
                    pk = ("pm", pmi % 4)
                    pmi += 1
                    for dc in range(8):
                        P.add("tensor", lambda e, pmt=pmt, dc=dc, c0=c0, ncol=ncol, hT=hT, bi=bi: e.matmul(
                            pmt[:, 0:ncol], lhsT=hT[:, dc, bi * 128:(bi + 1) * 128], rhs=winb[:, dc, c0:c0 + ncol],
                            start=(dc == 0), stop=(dc == 7)),
                            r=[("winb", dc), hk + (bi,)], w=[pk])
                    if o0 == 0:
                        o_ap = vs[:, 0:512]
                        i_ap = pmt[:, 0:512]
                    else:
                        h0 = (o0 - 512) // 64
                        nh = ncol // 64
                        o_ap = vs[:, 512:VW].rearrange("p (h e) -> p h e", e=65)[:, h0:h0 + nh, 0:64]
                        i_ap = pmt[:, 0:ncol].rearrange("p (h e) -> p h e", e=64)
                    P.add("vector", lambda e, o_ap=o_ap, i_ap=i_ap, c=c: e.tensor_scalar(
                        out=o_ap, in0=i_ap, scalar1=valid[:, c:c + 1], scalar2=None,
                        op0=ALU.mult),
                        r=[pk, "valid"], w=[vk + (o0,)])
                    yield
                P.add("vector", lambda e, vs=vs, c=c: e.tensor_scalar(
                    out=vs[:, 512:VW].rearrange("p (h e) -> p h e", e=65)[:, :, 64:65], in0=ones12[:],
                    scalar1=valid[:, c:c + 1], scalar2=None, op0=ALU.mult),
                    r=["ones12", "valid"], w=[vk + (1024,)])
                P.add("gpsimd", lambda e, vs=vs, c=c: e.dma_start(
                    out=g.v_d[c * 128:(c + 1) * 128, :], in_=vs[:]),
                    r=[vk + (0,), vk + (512,), vk + (1024,)], w=[("v_d", c)], chan=f"a_vst{c % 2}")
            for qc in range(10):
                pmt = pm[pmi % 4]
                pk = ("pm", pmi % 4)
                pmi += 1
                for dc in range(8):
                    P.add("tensor", lambda e, pmt=pmt, dc=dc, qc=qc, hT=hT: e.matmul(
                        pmt[:, 0:128], lhsT=winb[:, dc, qcols[qc]:qcols[qc] + 128], rhs=hT[:, dc, 384:512],
                        start=(dc == 0), stop=(dc == 7)),
                        r=[("winb", dc), hk + (3,)], w=[pk])
                P.add("scalar", lambda e, pmt=pmt, qc=qc, tg=tg: e.activation(
                    out=qst[tg % 2][:, qc, :], in_=pmt[:, 0:128], func=AF.Copy, scale=0.125),
                    r=[pk], w=[("qst", tg % 2, qc)])
                yield
            P.add("gpsimd", lambda e, tg=tg: e.dma_start(
                out=g.qT_d[:, :, tg * 128:(tg + 1) * 128], in_=qst[tg % 2][:]),
                r=[("qst", tg % 2, qc) for qc in range(10)], w=[("qT_d", tg)], chan=f"a_qst{tg % 2}")
            cnt['pmi'] = pmi
            cnt['ksi'] = ksi

        for bi in range(4):
            lnt(0, bi)
            tr(0, bi)
        for tg in range(ntg):
            gen = mm(tg)
            for gi_, _ in enumerate(gen):
                if tg + 1 < ntg and gi_ in (0, 8, 16, 24):
                    lnt(tg + 1, gi_ // 8)
                if tg + 1 < ntg and gi_ in (6, 14, 22, 30):
                    tr(tg + 1, (gi_ - 6) // 8)
        P.emit_phase()


def phase_b(P, nc, g, groups=(0, 1, 2, 3)):
    st = contextlib.ExitStack()
    with st:
        sb = lambda name, shape, dt: st.enter_context(nc.sbuf_tensor(name, shape, dt))
        ps = lambda name, shape, dt: st.enter_context(nc.psum_tensor(name, shape, dt))
        nblk_max = 16 * (max(groups) + 1)
        kTsb = sb("b_kT", [128, 4, S], BF16)
        vsb = sb("b_v", [128, NB, 512], BF16)
        sbmask = sb("b_mask", [128, 16, 512], BF16)
        ident = sb("b_ident", [128, 128], BF16)
        negU = sb("b_negU", [128, 128], BF16)
        negOnes = sb("b_negOnes", [128, 128], BF16)
        zer = sb("b_zer", [128, 64], BF16)
        qsb = [sb(f"b_q{i}", [128, 4, 512], BF16) for i in range(2)]
        e_sb = [sb(f"b_e{i}", [128, 512], F32) for i in range(4)]
        sp_sb = [sb(f"b_sp{i}", [128, 512], BF16) for i in range(4)]
        w_sb = [sb(f"b_w{i}", [128, 512], BF16) for i in range(4)]
        srun = [[sb(f"b_srun{i}_{j}", [128, 512], BF16) for j in range(2)] for i in range(4)]
        ost = [sb(f"b_ost{i}", [64, 512], BF16) for i in range(4)]
        pz = [ps(f"b_pz{i}", [128, 512], F32) for i in range(4)]
        po = [ps(f"b_po{i}", [64, 512], F32) for i in range(4)]

        P.add("sync", lambda e: e.dma_start(out=ident[:], in_=g.ident), w=["ident"], chan="b_c")
        P.add("sync", lambda e: e.dma_start(out=negU[:], in_=g.negU), w=["negU"], chan="b_c")
        P.add("sync", lambda e: e.dma_start(out=negOnes[:], in_=g.negOnes), w=["negOnes"], chan="b_c")
        P.add("sync", lambda e: e.dma_start(out=sbmask[:], in_=g.sbmask), w=["sbmask"], chan="b_c")
        P.add("gpsimd", lambda e: e.memset(zer[:], 0.0), w=["zer"])
        ntok = nblk_max * 128
        for kc in range(4):
            for hf in range(0, ntok, 2048):
                P.add("sync", lambda e, kc=kc, hf=hf: e.dma_start(
                    out=kTsb[:, kc, hf:hf + 2048], in_=g.kT_d[kc, :, hf:hf + 2048]),
                    w=[("kTsb", kc, hf // 2048)], chan="b_k")
        for cb in range(0, nblk_max, 16):
            P.add("sync", lambda e, cb=cb: e.dma_start(
                out=vsb[:, cb:cb + 16, :],
                in_=g.v_d[cb * 128:(cb + 16) * 128, 0:512].rearrange("(c p) n -> p c n", p=128)),
                w=[("vsb", cb // 16)], chan="b_v")

        for gi, gq in enumerate(groups):
            q_t = qsb[gi % 2]
            qk = ("qsb", gi % 2)
            P.add("sync", lambda e, q_t=q_t, gq=gq: e.dma_start(
                out=q_t[:], in_=g.qT_d[:, 0:4, gq * 512:(gq + 1) * 512]),
                w=[qk], chan=f"b_q{gi % 2}")
            nblk = 16 * (gq + 1)
            for hq in range(2):
                heads = [4 * hq + i for i in range(4)]
                for i in range(4):
                    for par in range(2):
                        P.add("gpsimd", lambda e, i=i, par=par: e.memset(srun[i][par][:], 0.0), w=[("srun", i, par)])
                    P.add("tensor", lambda e, i=i: e.matmul(
                        po[i][:], lhsT=zer[:], rhs=sbmask[:, 0, :], start=True, stop=True),
                        r=["zer", "sbmask"], w=[("po", i)])
                cs = list(range(nblk - 1, -1, -1))

                def prm(step):
                    c = cs[step]
                    rel_c = c - 16 * gq
                    q0 = 128 * (rel_c // 4) if rel_c >= 4 else 0
                    return c, rel_c, (rel_c >= 3), step % 2, q0

                def s1(step, i):
                    c, rel_c, need_mask, par, q0 = prm(step)
                    h = heads[i]
                    hc, half = h // 2, h % 2
                    p0 = 64 * half
                    P.add("tensor", lambda e, i=i, hc=hc, p0=p0, c=c, q_t=q_t, nm=need_mask, q0=q0: e.matmul(
                        pz[i][:, q0:512], lhsT=kTsb[p0:p0 + 64, hc, c * 128:(c + 1) * 128],
                        rhs=q_t[p0:p0 + 64, hc, q0:512], start=True, stop=(not nm)),
                        r=[("kTsb", hc, c // 16), qk], w=[("pz", i)])
                    if need_mask:
                        P.add("tensor", lambda e, i=i, rel_c=rel_c, q0=q0: e.matmul(
                            pz[i][:, q0:512], lhsT=ident[:], rhs=sbmask[:, rel_c, q0:512], start=False, stop=True),
                            r=["ident", "sbmask"], w=[("pz", i)])

                for i in range(4):
                    s1(0, i)
                for step in range(nblk):
                    c, rel_c, need_mask, par, q0 = prm(step)
                    for i, h in enumerate(heads):
                        P.add("scalar", lambda e, i=i, q0=q0: e.activation(
                            out=e_sb[i][:, q0:512], in_=pz[i][:, q0:512], func=AF.Exp),
                            r=[("pz", i)], w=[("e", i)])
                        P.add("scalar", lambda e, i=i, q0=q0: e.activation(
                            out=sp_sb[i][:, q0:512], in_=e_sb[i][:, q0:512], func=AF.Ln, bias=1.0),
                            r=[("e", i)], w=[("sp", i)])
                    for i, h in enumerate(heads):
                        last = (step == 0)
                        P.add("tensor", lambda e, i=i, last=last, q0=q0: e.matmul(
                            pz[i][:, q0:512], lhsT=negU[:], rhs=sp_sb[i][:, q0:512], start=False, stop=last,
                            skip_group_check=True),
                            r=["negU", ("sp", i)], w=[("pz", i)])
                        if step > 0:
                            P.add("tensor", lambda e, i=i, par=par, q0=q0: e.matmul(
                                pz[i][:, q0:512], lhsT=negOnes[:], rhs=srun[i][1 - par][:, q0:512], start=False,
                                stop=True, skip_group_check=True),
                                r=["negOnes", ("srun", i, 1 - par)], w=[("pz", i)])
                    for i, h in enumerate(heads):
                        if step == 0:
                            P.add("vector", lambda e, i=i, par=par, q0=q0: e.tensor_copy(
                                out=srun[i][par][:, q0:512], in_=sp_sb[i][:, q0:512]),
                                r=[("sp", i)], w=[("srun", i, par)])
                        elif c > 0:
                            P.add("vector", lambda e, i=i, par=par, q0=q0: e.tensor_tensor(
                                out=srun[i][par][:, q0:512], in0=srun[i][1 - par][:, q0:512], in1=sp_sb[i][:, q0:512],
                                op=ALU.add),
                                r=[("sp", i), ("srun", i, 1 - par)], w=[("srun", i, par)])
                    for i, h in enumerate(heads):
                        P.add("scalar", lambda e, i=i, q0=q0: e.activation(
                            out=w_sb[i][:, q0:512], in_=pz[i][:, q0:512], func=AF.Exp),
                            r=[("pz", i)], w=[("w", i)])
                    for i, h in enumerate(heads):
                        if step + 1 < nblk:
                            s1(step + 1, i)
                        P.add("tensor", lambda e, i=i, h=h, c=c, q0=q0: e.matmul(
                            po[i][:, q0:512], lhsT=vsb[:, c, h * 64:(h + 1) * 64], rhs=w_sb[i][:, q0:512],
                            start=False, stop=True, skip_group_check=True),
                            r=[("vsb", c // 16), ("w", i)], w=[("po", i)])
                for i, h in enumerate(heads):
                    P.add("vector", lambda e, i=i: e.tensor_copy(out=ost[i][:], in_=po[i][:]),
                          r=[("po", i)], w=[("ost", i)])
                    P.add("sync", lambda e, i=i, h=h, gq=gq: e.dma_start(
                        out=g.osbT_d[h, :, gq * 512:(gq + 1) * 512], in_=ost[i][:]),
                        r=[("ost", i)], w=[("osbT_d", h, gq)], chan=f"b_ost{i}")
        P.emit_phase()


DIL = ((128, 1), (512, 4), (2048, 16))
DL_NB = [w // 128 + 1 for w, _ in DIL]
DL_TOFF = [0, 4 * DL_NB[0], 4 * (DL_NB[0] + DL_NB[1])]
DL_NT = 4 * sum(DL_NB)


def phase_c(P, nc, g, slots=tuple(range(NOWN))):
    st = contextlib.ExitStack()
    with st:
        sb = lambda name, shape, dt: st.enter_context(nc.sbuf_tensor(name, shape, dt))
        ps = lambda name, shape, dt: st.enter_context(nc.psum_tensor(name, shape, dt))
        WB = 17
        kdl = [sb(f"c_k{i}", [128, 6, WB * 128], BF16) for i in range(2)]
        vdl = [sb(f"c_v{i}", [128, WB, 780], BF16) for i in range(2)]
        qdl = [sb(f"c_q{i}", [128, 6, 128], BF16) for i in range(2)]
        dlbias = sb("c_bias", [128, DL_NT, 128], BF16)
        padbias = sb("c_pad", [128, NB], F32)
        ident = sb("c_ident", [128, 128], BF16)
        pT = [sb(f"c_pT{i}", [128, 4, 128], BF16) for i in range(4)]
        rden = sb("c_rden", [128, 4], F32)
        otok = [sb(f"c_otok{i}", [128, 256], BF16) for i in range(2)]
        oT = [sb(f"c_oT{i}", [128, 2, 128], BF16) for i in range(2)]
        pzd = [ps(f"c_pz{i}", [128, 4, 128], F32) for i in range(4)]
        pd = [ps(f"c_pd{i}", [128, 4, 65], F32) for i in range(2)]
        ptr = ps("c_ptr", [128, 2, 128], BF16)

        P.add("sync", lambda e: e.dma_start(out=ident[:], in_=g.ident), w=["ident"], chan="c_c")
        P.add("sync", lambda e: e.dma_start(out=padbias[:], in_=g.padbias), w=["padbias"], chan="c_c")
        for t0 in range(0, DL_NT, 24):
            P.add("sync", lambda e, t0=t0: e.dma_start(out=dlbias[:, t0:t0 + 24, :], in_=g.dlbias[:, t0:t0 + 24, :]),
                  w=[("dlbias", t0 // 24)], chan="c_c")
        ui = 0
        for si, s_ in enumerate(slots):
            cq = 4 * s_ + 3
            c_lo = max(0, cq - 16)
            nwb = cq - c_lo + 1
            b2 = si % 2
            P.add("sync", lambda e, b2=b2, c_lo=c_lo, nwb=nwb: e.dma_start(
                out=kdl[b2][:, :, 0:nwb * 128],
                in_=g.kT_d[4:10, :, c_lo * 128:(c_lo + nwb) * 128].rearrange("c p t -> p c t")),
                w=[("kdl", b2)], chan=f"c_k{b2}")
            P.add("sync", lambda e, b2=b2, c_lo=c_lo, nwb=nwb: e.dma_start(
                out=vdl[b2][:, 0:nwb, :],
                in_=g.v_d[c_lo * 128:(c_lo + nwb) * 128, 512:VW].rearrange("(c p) n -> p c n", p=128)),
                w=[("vdl", b2)], chan=f"c_v{b2}")
            P.add("sync", lambda e, b2=b2, s_=s_: e.dma_start(
                out=qdl[b2][:], in_=g.qT_d[:, 4:10, s_ * 128:(s_ + 1) * 128]),
                w=[("qdl", b2)], chan=f"c_q{b2}")
            pdt = pd[si % 2]
            pdk = ("pd", si % 2)
            units = []
            for hg in range(4):
                uh = []
                for gi in range(3):
                    for o in range(DL_NB[gi]):
                        c = cq - o
                        if c >= 0:
                            uh.append((gi, o, c))
                for k_, (gi, o, c) in enumerate(uh):
                    units.append((hg, k_, len(uh), gi, o, c))
            NBATCH = 4
            batches = [units[i:i + NBATCH] for i in range(0, len(units), NBATCH)]

            def s1(bt, zi):
                for j, u in enumerate(bt):
                    hg, k_, n, gi, o, c = u
                    hd = 4 * gi + hg
                    chn, half = hd // 2, hd % 2
                    p0 = 64 * half
                    wb = c - c_lo
                    tix = DL_TOFF[gi] + hg * DL_NB[gi] + o
                    P.add("tensor", lambda e, zi=zi, j=j, b2=b2, chn=chn, p0=p0, wb=wb: e.matmul(
                        pzd[zi][:, j, :], lhsT=kdl[b2][p0:p0 + 64, chn, wb * 128:(wb + 1) * 128],
                        rhs=qdl[b2][p0:p0 + 64, chn, :], start=True, stop=False),
                        r=[("kdl", b2), ("qdl", b2)], w=[("pzd", zi)])
                    P.add("tensor", lambda e, zi=zi, j=j, tix=tix: e.matmul(
                        pzd[zi][:, j, :], lhsT=ident[:], rhs=dlbias[:, tix, :], start=False, stop=True),
                        r=["ident", ("dlbias", tix // 24)], w=[("pzd", zi)])
                nb_ = len(bt)
                P.add("scalar", lambda e, zi=zi, nb_=nb_: e.activation(
                    out=pT[zi][:, 0:nb_, :], in_=pzd[zi][:, 0:nb_, :], func=AF.Exp),
                    r=[("pzd", zi)], w=[("pT", zi)])

            def s2(bt, zi):
                for j, u in enumerate(bt):
                    hg, k_, n, gi, o, c = u
                    hd = 4 * gi + hg
                    wb = c - c_lo
                    P.add("tensor", lambda e, zi=zi, j=j, b2=b2, wb=wb, hd=hd, hg=hg, k_=k_, n=n, pdt=pdt: e.matmul(
                        pdt[:, hg, :], lhsT=pT[zi][:, j, :], rhs=vdl[b2][:, wb, hd * 65:(hd + 1) * 65],
                        start=(k_ == 0), stop=(k_ == n - 1)),
                        r=[("pT", zi), ("vdl", b2)], w=[pdk])

            LOOK = 3
            zis = []
            for i in range(len(batches) + LOOK):
                if i < len(batches):
                    zis.append(ui % 4)
                    ui += 1
                    s1(batches[i], zis[i])
                if i - LOOK >= 0:
                    s2(batches[i - LOOK], zis[i - LOOK])
            P.add("vector", lambda e, pdt=pdt: e.reciprocal(out=rden[:], in_=pdt[:, :, 64]),
                  r=[pdk], w=["rden"])
            ot = otok[si % 2]
            for hg in range(4):
                P.add("vector", lambda e, pdt=pdt, hg=hg, ot=ot: e.tensor_scalar(
                    out=ot[:, hg * 64:(hg + 1) * 64], in0=pdt[:, hg, 0:64], scalar1=rden[:, hg:hg + 1],
                    scalar2=None, op0=ALU.mult),
                    r=[pdk, "rden"], w=[("otok", si % 2)])
            for cc in range(2):
                P.add("tensor", lambda e, cc=cc, ot=ot: e.transpose(
                    out=ptr[:, cc, :], in_=ot[:, cc * 128:(cc + 1) * 128], identity=ident[:]),
                    r=[("otok", si % 2), "ident"], w=["ptr"])
            P.add("vector", lambda e, si=si: e.tensor_copy(out=oT[si % 2][:], in_=ptr[:]),
                  r=["ptr"], w=[("oT", si % 2)])
            P.add("gpsimd", lambda e, si=si, s_=s_: e.dma_start(
                out=g.odlT_d[:, :, s_ * 128:(s_ + 1) * 128].rearrange("c p t -> p c t"), in_=oT[si % 2][:]),
                r=[("oT", si % 2)], w=[("odlT_d", s_)], chan=f"c_oT{si % 2}")
        P.emit_phase()


def _bounds_reg(P, g):
    def fn(e):
        if not g.bcreg:
            g.bcreg.append(e.alloc_register("bc"))
        return e.reg_mov(g.bcreg[0], NEXP * CAP - 1)
    P.add("gpsimd", fn)


def phase_d(P, nc, g, qgroups=(0, 1, 2, 3), d_stop=9):
    st = contextlib.ExitStack()
    with st:
        sb = lambda name, shape, dt: st.enter_context(nc.sbuf_tensor(name, shape, dt))
        ps = lambda name, shape, dt: st.enter_context(nc.psum_tensor(name, shape, dt))
        wg = sb("d_wg", [128, 8, 2048], BF16)
        wbs = sb("d_wbs", [64, 8, 1024], BF16)
        wbd = sb("d_wbd", [128, 2, 1024], BF16)
        wo = sb("d_wo", [128, 8, 1024], BF16)
        wr = sb("d_wr", [128, 8, 256], F32)
        bgT = sb("d_bgT", [128, 16], F32)
        g1rep = sb("d_g1", [128, 1024], F32)
        b1rep = sb("d_b1", [128, 1024], F32)
        rbrep = sb("d_rb", [128, 256], F32)
        iota = sb("d_iota", [128, 256], F32)
        identb = sb("d_identb", [128, 128], BF16)
        wr_hi = sb("d_wr_hi", [128, 8, 256], BF16)
        wr_lo = sb("d_wr_lo", [128, 8, 256], BF16)
        h1lo = sb("d_h1lo", [128, 1024], BF16)
        h1Tlo = sb("d_h1Tlo", [128, 8, 128], BF16)
        lstrict = sb("d_lstrict", [128, 128], BF16)
        ones = sb("d_ones", [128, 128], BF16)
        hTo = sb("d_hTo", [128, 8, 512], BF16)
        osb = sb("d_osb", [64, 8, 512], BF16)
        odl = sb("d_odl", [128, 2, 512], BF16)
        gs = sb("d_gs", [128, 512], F32)
        gd = sb("d_gd", [128, 512], F32)
        m1 = sb("d_m1", [128, 512], F32)
        m2 = sb("d_m2", [128, 512], F32)
        mT = sb("d_mT", [128, 8, 512], BF16)
        hown = [sb(f"d_hown{i}", [128, 1024], F32) for i in range(2)]
        rr = sb("d_r", [128, 1024], F32)
        h1 = [sb(f"d_h1_{i}", [128, 1024], F32) for i in range(2)]
        h1b = [sb(f"d_h1b{i}", [128, 1024], BF16) for i in range(2)]
        h1Tb = [sb(f"d_h1Tb{i}", [128, 8, 128], BF16) for i in range(2)]
        stats = sb("d_stats", [128, 2, 6], F32)
        mv = sb("d_mv", [128, 2], F32)
        std = sb("d_std", [128, 1], F32)
        rstd = sb("d_rstd", [128, 1], F32)
        sc = [sb(f"d_sc{i}", [128, 256], F32) for i in range(2)]
        biased = [sb(f"d_biased{i}", [128, 256], F32) for i in range(2)]
        masked = sb("d_masked", [128, 256], F32)
        junk = sb("d_junk", [128, 256], F32)
        selb = sb("d_selb", [128, NOWN, 256], BF16)
        m8g = sb("d_m8g", [128, 8, 8], F32)
        gscore = sb("d_gscore", [128, 8], F32)
        gm8 = sb("d_gm8", [128, 8], F32)
        pen = sb("d_pen", [128, 8], F32)
        t8 = sb("d_t8", [128, 8], F32)
        wk = sb("d_wk", [128, 8], F32)
        rk = sb("d_rk", [128, 8], F32)
        ik = sb("d_ik", [128, 8], F32)
        wsum = sb("d_wsum", [128, 1], F32)
        sif = sb("d_sif", [128, 8], F32)
        ovf = sb("d_ovf", [128, 8], F32)
        pA = ps("d_pA", [128, 512], F32)
        pB = ps("d_pB", [128, 512], F32)
        pC = ps("d_pC", [128, 512], F32)
        pD = ps("d_pD", [128, 512], F32)
        pmix = ps("d_pmix", [128, 1024], F32)
        ptr = ps("d_ptr", [128, 8, 128], BF16)
        ptr_lo = ps("d_ptr_lo", [128, 8, 128], BF16)

        _bounds_reg(P, g)
        for dc in range(8):
            P.add("gpsimd", lambda e, dc=dc: e.dma_start(
                out=wg[:, dc, :], in_=g.w_in[dc * 128:(dc + 1) * 128, 3840:5888]), w=[("wg", dc)], chan="d_w")
        P.add("gpsimd", lambda e: e.dma_start(
            out=wbs[:], in_=g.w_br_sb.rearrange("(h p) n -> p h n", p=64)), w=["wbs"], chan="d_w")
        P.add("gpsimd", lambda e: e.dma_start(
            out=wbd[:], in_=g.w_br_dil.rearrange("(c p) n -> p c n", p=128)), w=["wbd"], chan="d_w")
        for dc in range(8):
            P.add("gpsimd", lambda e, dc=dc: e.dma_start(
                out=wo[:, dc, :], in_=g.w_out[dc * 128:(dc + 1) * 128, :]), w=[("wo", dc)], chan="d_w")
        P.add("sync", lambda e: e.dma_start(out=wr[:], in_=g.w_router.rearrange("(c p) n -> p c n", p=128)),
              w=["wr"], chan="d_wr")
        P.add("sync", lambda e: e.dma_start(out=bgT[:], in_=g.b_gateT), w=["bgT"], chan="d_c")
        P.add("sync", lambda e: e.dma_start(out=g1rep[:], in_=g.ln1_g.partition_broadcast(128)), w=["g1rep"], chan="d_c")
        P.add("sync", lambda e: e.dma_start(out=b1rep[:], in_=g.ln1_b.partition_broadcast(128)), w=["b1rep"], chan="d_c")
        P.add("sync", lambda e: e.dma_start(out=rbrep[:], in_=g.router_bias.partition_broadcast(128)), w=["rbrep"], chan="d_c")
        P.add("sync", lambda e: e.dma_start(out=iota[:], in_=g.iota256), w=["iota"], chan="d_c")
        P.add("sync", lambda e: e.dma_start(out=identb[:], in_=g.ident), w=["identb"], chan="d_c")
        P.add("gpsimd", lambda e: e.dma_start(out=wr_hi[:], in_=g.w_router.rearrange("(c p) n -> p c n", p=128)),
              w=["wr_hi"], chan="d_wrh")
        P.add("vector", lambda e: e.tensor_tensor(out=wr_lo[:], in0=wr[:], in1=wr_hi[:], op=ALU.subtract),
              r=["wr", "wr_hi"], w=["wr_lo"])
        P.add("sync", lambda e: e.dma_start(out=lstrict[:], in_=g.lstrict), w=["lstrict"], chan="d_c")
        P.add("sync", lambda e: e.dma_start(out=ones[:], in_=g.ones), w=["ones"], chan="d_c")


        pend = [None]
        allk = lambda n: [(n, k) for k in range(8)]

        def xgen(bi, blk):
            b2 = blk % 2
            P.add("sync", lambda e, blk=blk, b2=b2: e.dma_start(
                out=hown[b2][:], in_=g.h_own_d[blk * 128:(blk + 1) * 128, :]), w=[("hown", b2)], chan=f"d_hown{b2}")
            for nh in range(2):
                for oc in range(8):
                    P.add("tensor", lambda e, bi=bi, nh=nh, oc=oc: e.matmul(
                        pmix[:, nh * 512:(nh + 1) * 512], lhsT=mT[:, oc, bi * 128:(bi + 1) * 128],
                        rhs=wo[:, oc, nh * 512:(nh + 1) * 512], start=(oc == 0), stop=(oc == 7)),
                        r=[("mT", oc), ("wo", oc)], w=[("pmix", nh)])
            yield
            for nh in range(2):
                P.add("vector", lambda e, b2=b2, nh=nh: e.scalar_tensor_tensor(
                    out=rr[:, nh * 512:(nh + 1) * 512], in0=hown[b2][:, nh * 512:(nh + 1) * 512], scalar=ALPHA,
                    in1=pmix[:, nh * 512:(nh + 1) * 512], op0=ALU.mult, op1=ALU.add),
                    r=[("hown", b2), ("pmix", nh)], w=[("rr", nh)])
            for hh in range(2):
                P.add("vector", lambda e, hh=hh: e.bn_stats(out=stats[:, hh, :], in_=rr[:, hh * 512:(hh + 1) * 512]),
                      r=[("rr", hh)], w=[("dstats", hh)])
            P.add("vector", lambda e: e.bn_aggr(out=mv[:], in_=stats[:].rearrange("p a b -> p (a b)")),
                  r=[("dstats", 0), ("dstats", 1)], w=["dmv"])
            P.add("scalar", lambda e: e.activation(out=std[:], in_=mv[:, 1:2], func=AF.Sqrt, bias=LN_EPS),
                  r=["dmv"], w=["dstd"])
            yield
            P.add("vector", lambda e: e.reciprocal(out=rstd[:], in_=std[:]), r=["dstd"], w=["drstd"])
            P.add("vector", lambda e, b2=b2: e.tensor_scalar(
                out=h1[b2][:], in0=rr[:], scalar1=mv[:, 0:1], scalar2=rstd[:, 0:1], op0=ALU.subtract, op1=ALU.mult),
                r=[("rr", 0), ("rr", 1), "dmv", "drstd"], w=[("h1", b2)])
            P.add("gpsimd", lambda e, b2=b2: e.tensor_tensor(out=h1[b2][:], in0=h1[b2][:], in1=g1rep[:], op=ALU.mult),
                  r=[("h1", b2), "g1rep"], w=[("h1", b2)])
            P.add("gpsimd", lambda e, b2=b2: e.tensor_tensor(out=h1[b2][:], in0=h1[b2][:], in1=b1rep[:], op=ALU.add),
                  r=[("h1", b2), "b1rep"], w=[("h1", b2)])
            P.add("scalar", lambda e, blk=blk, b2=b2: e.dma_start(
                out=g.h1_d[blk * 128:(blk + 1) * 128, :], in_=h1[b2][:]), r=[("h1", b2)], w=[("h1_d", blk)],
                chan=f"d_h1{b2}")
            P.add("scalar", lambda e, b2=b2: e.activation(out=h1b[b2][:], in_=h1[b2][:], func=AF.Copy),
                  r=[("h1", b2)], w=[("h1b", b2)])
            yield
            P.add("vector", lambda e, b2=b2: e.tensor_tensor(out=h1lo[:], in0=h1[b2][:], in1=h1b[b2][:], op=ALU.subtract),
                  r=[("h1", b2), ("h1b", b2)], w=["h1lo"])
            for dc in range(8):
                P.add("tensor", lambda e, b2=b2, dc=dc: e.transpose(
                    out=ptr[:, dc, :], in_=h1b[b2][:, dc * 128:(dc + 1) * 128], identity=identb[:]),
                    r=[("h1b", b2), "identb"], w=["ptr"])
            for dc in range(8):
                P.add("tensor", lambda e, dc=dc: e.transpose(
                    out=ptr_lo[:, dc, :], in_=h1lo[:, dc * 128:(dc + 1) * 128], identity=identb[:]),
                    r=["h1lo", "identb"], w=["ptr_lo"])
            P.add("scalar", lambda e, b2=b2: e.activation(out=h1Tb[b2][:], in_=ptr[:], func=AF.Copy),
                  r=["ptr"], w=[("h1Tb", b2)])
            P.add("scalar", lambda e, blk=blk, b2=b2: e.dma_start(
                out=g.h1T_d[:, :, blk * 128:(blk + 1) * 128], in_=h1Tb[b2][:]), r=[("h1Tb", b2)],
                w=[("h1T_d", blk)], chan=f"d_h1T{b2}")
            yield
            P.add("vector", lambda e: e.tensor_copy(out=h1Tlo[:], in_=ptr_lo[:]), r=["ptr_lo"], w=["h1Tlo"])
            combos = [(h1Tb[b2], ("h1Tb", b2), wr_hi, "wr_hi"), (h1Tb[b2], ("h1Tb", b2), wr_lo, "wr_lo"),
                      (h1Tlo, "h1Tlo", wr_hi, "wr_hi")]
            for ci, (lt, ltk, rt, rtk) in enumerate(combos):
                for dc in range(8):
                    P.add("tensor", lambda e, dc=dc, lt=lt, rt=rt, ci=ci: e.matmul(
                        pA[:, 0:256], lhsT=lt[:, dc, :], rhs=rt[:, dc, :], start=(ci == 0 and dc == 0),
                        stop=(ci == 2 and dc == 7)), r=[ltk, rtk], w=["pA"])
            P.add("scalar", lambda e, b2=b2: e.activation(out=sc[b2][:], in_=pA[:, 0:256], func=AF.Sigmoid),
                  r=["pA"], w=[("sc", b2)])
            yield
            P.add("vector", lambda e, b2=b2: e.tensor_tensor(out=biased[b2][:], in0=sc[b2][:], in1=rbrep[:], op=ALU.add),
                  r=[("sc", b2), "rbrep"], w=[("biased", b2)])

        def ygen(blk):
            b2 = blk % 2
            bia = biased[b2]
            bk = ("biased", b2)
            sct = sc[b2]
            sck = ("sc", b2)
            for gr in range(8):
                P.add("vector", lambda e, gr=gr: e.max(out=m8g[:, gr, :], in_=bia[:, gr * 32:(gr + 1) * 32]),
                      r=[bk], w=[("m8g", gr)])
            P.add("vector", lambda e: e.tensor_tensor(out=gscore[:], in0=m8g[:, :, 0], in1=m8g[:, :, 1], op=ALU.add),
                  r=[("m8g", gr) for gr in range(8)], w=["gscore"])
            P.add("vector", lambda e: e.max(out=gm8[:], in_=gscore[:]), r=["gscore"], w=["gm8"])
            P.add("vector", lambda e: e.tensor_scalar(
                out=pen[:], in0=gscore[:], scalar1=gm8[:, 3:4], scalar2=1.0e4, op0=ALU.is_lt, op1=ALU.mult),
                r=["gscore", "gm8"], w=["pen"])
            P.add("vector", lambda e: e.tensor_tensor(
                out=masked[:].rearrange("p (a b) -> p a b", b=32), in0=bia[:].rearrange("p (a b) -> p a b", b=32),
                in1=pen[:].unsqueeze(2).to_broadcast([128, 8, 32]), op=ALU.subtract),
                r=[bk, "pen"], w=["masked"])
            P.add("vector", lambda e: e.max(out=t8[:], in_=masked[:]), r=["masked"], w=["t8"])
            P.add("vector", lambda e, blk=blk: e.tensor_scalar(
                out=selb[:, blk, :], in0=masked[:], scalar1=t8[:, 7:8], scalar2=None, op0=ALU.is_ge),
                r=["masked", "t8"], w=[("selb", blk)])
            P.add("tensor", lambda e, blk=blk: e.matmul(
                pB[:, 0:256], lhsT=lstrict[:], rhs=selb[:, blk, :], start=True, stop=(blk == 0)),
                r=["lstrict", ("selb", blk)], w=["pB"])
            for pb_ in range(blk):
                P.add("tensor", lambda e, pb_=pb_, blk=blk: e.matmul(
                    pB[:, 0:256], lhsT=ones[:], rhs=selb[:, pb_, :], start=False, stop=(pb_ == blk - 1)),
                    r=["ones", ("selb", pb_)], w=["pB"])
            yield
            for k in range(8):
                P.add("vector", lambda e, k=k: e.scalar_tensor_tensor(
                    out=junk[:], in0=masked[:], scalar=t8[:, k:k + 1], in1=sct[:], op0=ALU.is_equal, op1=ALU.mult,
                    accum_out=wk[:, k:k + 1]), r=["masked", "t8", sck], w=["junk", ("wk", k)])
                P.add("vector", lambda e, k=k: e.scalar_tensor_tensor(
                    out=junk[:], in0=masked[:], scalar=t8[:, k:k + 1], in1=iota[:], op0=ALU.is_equal,
                    op1=ALU.mult, accum_out=ik[:, k:k + 1]), r=["masked", "t8", "iota"], w=["junk", ("ik", k)])
                if k % 3 == 2:
                    yield
            yield
            for k in range(8):
                P.add("vector", lambda e, k=k: e.scalar_tensor_tensor(
                    out=junk[:], in0=masked[:], scalar=t8[:, k:k + 1], in1=pB[:, 0:256], op0=ALU.is_equal,
                    op1=ALU.mult, accum_out=rk[:, k:k + 1]), r=["masked", "t8", "pB"], w=["junk", ("rk", k)])
            P.add("vector", lambda e: e.tensor_reduce(out=wsum[:], in_=wk[:], axis=AX.X, op=ALU.add),
                  r=allk("wk"), w=["wsum"])
            P.add("vector", lambda e: e.reciprocal(out=wsum[:], in_=wsum[:]), r=["wsum"], w=["wsum"])
            P.add("vector", lambda e, blk=blk: e.tensor_scalar(
                out=g.gk[:, blk, :], in0=wk[:], scalar1=wsum[:, 0:1], scalar2=2.5, op0=ALU.mult, op1=ALU.mult),
                r=allk("wk") + ["wsum"], w=[("gk", blk)])
            P.add("vector", lambda e: e.tensor_scalar(
                out=ovf[:], in0=rk[:], scalar1=float(CAP), scalar2=1.0e6, op0=ALU.is_ge, op1=ALU.mult),
                r=allk("rk"), w=["ovf"])
            P.add("vector", lambda e: e.scalar_tensor_tensor(
                out=sif[:], in0=ik[:], scalar=float(CAP), in1=rk[:], op0=ALU.mult, op1=ALU.add),
                r=allk("ik") + allk("rk"), w=["sif"])
            P.add("vector", lambda e: e.tensor_tensor(out=sif[:], in0=sif[:], in1=ovf[:], op=ALU.add),
                  r=["sif", "ovf"], w=["sif"])
            P.add("vector", lambda e, blk=blk: e.tensor_copy(out=g.sidx[:, blk, :], in_=sif[:]),
                  r=["sif"], w=[("sidx", blk)])
            for k in range(8):
                P.add("gpsimd", lambda e, blk=blk, k=k, b2=b2: e.indirect_dma_start(
                    out=g.xs_d[:, :], out_offset=bass.IndirectOffsetOnAxis(ap=g.sidx[:, blk, k:k + 1], axis=0),
                    in_=h1b[b2][:, :], in_offset=None, bounds_check=g.bcreg[0], oob_is_err=False),
                    r=[("h1b", b2), ("sidx", blk)], w=[("xs_d", blk, k)], chan=f"d_sc{b2}")

        for gq in qgroups:
            t0 = gq * 512
            P.add("sync", lambda e, t0=t0: e.dma_start(out=hTo[:], in_=g.hT_own_d[:, :, t0:t0 + 512]),
                  w=["hTo"], chan="d_hTo")
            P.add("sync", lambda e, t0=t0: e.dma_start(
                out=osb[:], in_=g.osbT_d[:, :, t0:t0 + 512].rearrange("h p t -> p h t")), w=["osb"], chan="d_osb")
            P.add("sync", lambda e, t0=t0: e.dma_start(
                out=odl[:], in_=g.odlT_d[:, :, t0:t0 + 512].rearrange("c p t -> p c t")), w=["odl"], chan="d_odl")
            for oc in range(8):
                for dc in range(8):
                    P.add("tensor", lambda e, oc=oc, dc=dc: e.matmul(
                        pA[:], lhsT=wg[:, dc, oc * 128:(oc + 1) * 128], rhs=hTo[:, dc, :],
                        start=(dc == 0), stop=(dc == 7)), r=[("wg", dc), "hTo"], w=["pA"])
                for dc in range(8):
                    P.add("tensor", lambda e, oc=oc, dc=dc: e.matmul(
                        pB[:], lhsT=wg[:, dc, 1024 + oc * 128:1024 + (oc + 1) * 128], rhs=hTo[:, dc, :],
                        start=(dc == 0), stop=(dc == 7)), r=[("wg", dc), "hTo"], w=["pB"])
                for h in range(8):
                    P.add("tensor", lambda e, oc=oc, h=h: e.matmul(
                        pC[:], lhsT=wbs[:, h, oc * 128:(oc + 1) * 128], rhs=osb[:, h, :],
                        start=(h == 0), stop=(h == 7)), r=["wbs", "osb"], w=["pC"])
                for c2 in range(2):
                    P.add("tensor", lambda e, oc=oc, c2=c2: e.matmul(
                        pD[:], lhsT=wbd[:, c2, oc * 128:(oc + 1) * 128], rhs=odl[:, c2, :],
                        start=(c2 == 0), stop=(c2 == 1)), r=["wbd", "odl"], w=["pD"])
                P.add("scalar", lambda e, oc=oc: e.activation(
                    out=gs[:], in_=pA[:], func=AF.Sigmoid, bias=bgT[:, oc:oc + 1]), r=["pA", "bgT"], w=["gs"])
                P.add("scalar", lambda e, oc=oc: e.activation(
                    out=gd[:], in_=pB[:], func=AF.Sigmoid, bias=bgT[:, 8 + oc:9 + oc]), r=["pB", "bgT"], w=["gd"])
                P.add("vector", lambda e: e.tensor_tensor(out=m1[:], in0=gs[:], in1=pC[:], op=ALU.mult),
                      r=["gs", "pC"], w=["m1"])
                P.add("vector", lambda e: e.tensor_tensor(out=m2[:], in0=gd[:], in1=pD[:], op=ALU.mult),
                      r=["gd", "pD"], w=["m2"])
                P.add("gpsimd", lambda e, oc=oc: e.tensor_tensor(out=mT[:, oc, :], in0=m1[:], in1=m2[:], op=ALU.add),
                      r=["m1", "m2"], w=[("mT", oc)])
            for bi in range(4):
                blk = gq * 4 + bi
                xg = xgen(bi, blk)
                yg = pend[0]
                xa, ya = True, yg is not None
                while xa or ya:
                    if xa:
                        try:
                            next(xg)
                        except StopIteration:
                            xa = False
                    if ya:
                        try:
                            next(yg)
                        except StopIteration:
                            ya = False
                pend[0] = ygen(blk)
        if pend[0] is not None:
            for _ in pend[0]:
                pass
        P.emit_phase()


def _layer_norm(P, x, xk0, xk1, stats, mv, std, rstd, out, outk, grep, gk_, brep, bk_, tag):
    for hh, xk in ((0, xk0), (1, xk1)):
        P.add("vector", lambda e, hh=hh: e.bn_stats(out=stats[:, hh, :], in_=x[:, hh * 512:(hh + 1) * 512]),
              r=[xk], w=[(tag + "stats", hh)])
    P.add("vector", lambda e: e.bn_aggr(out=mv[:], in_=stats[:].rearrange("p a b -> p (a b)")),
          r=[(tag + "stats", 0), (tag + "stats", 1)], w=[tag + "mv"])
    P.add("scalar", lambda e: e.activation(out=std[:], in_=mv[:, 1:2], func=AF.Sqrt, bias=LN_EPS),
          r=[tag + "mv"], w=[tag + "std"])
    P.add("vector", lambda e: e.reciprocal(out=rstd[:], in_=std[:]), r=[tag + "std"], w=[tag + "rstd"])
    P.add("vector", lambda e: e.tensor_scalar(
        out=out[:], in0=x[:], scalar1=mv[:, 0:1], scalar2=rstd[:, 0:1], op0=ALU.subtract, op1=ALU.mult),
        r=[xk0, xk1, tag + "mv", tag + "rstd"], w=[outk])
    P.add("gpsimd", lambda e: e.tensor_tensor(out=out[:], in0=out[:], in1=grep[:], op=ALU.mult),
          r=[outk, gk_], w=[outk])
    P.add("gpsimd", lambda e: e.tensor_tensor(out=out[:], in0=out[:], in1=brep[:], op=ALU.add),
          r=[outk, bk_], w=[outk])


def _swiglu_block(P, xT, xTk, wg_t, wu_t, wd_t, wkeys, pgu, pguk, sg, sgk, hid, hidk, py, pyk):
    for j, wt in enumerate((wg_t, wu_t)):
        for hh in range(2):
            for dc in range(8):
                P.add("tensor", lambda e, j=j, wt=wt, hh=hh, dc=dc: e.matmul(
                    pgu[:, 2 * j + hh, :], lhsT=wt[:, dc, hh * 128:(hh + 1) * 128], rhs=xT[:, dc, :],
                    start=(dc == 0), stop=(dc == 7)), r=[wkeys[j], xTk], w=[pguk])
    P.add("scalar", lambda e: e.activation(out=sg[:], in_=pgu[:, 0:2, :], func=AF.Silu), r=[pguk], w=[sgk])
    P.add("vector", lambda e: e.tensor_tensor(out=hid[:], in0=sg[:], in1=pgu[:, 2:4, :], op=ALU.mult),
          r=[sgk, pguk], w=[hidk])
    for nh in range(2):
        for hh in range(2):
            P.add("tensor", lambda e, nh=nh, hh=hh: e.matmul(
                py[:, nh * 512:(nh + 1) * 512], lhsT=hid[:, hh, :], rhs=wd_t[:, hh, nh * 512:(nh + 1) * 512],
                start=(hh == 0), stop=(hh == 1)), r=[hidk, wkeys[2]], w=[pyk + (nh,)])


def phase_f(P, nc, g, experts=tuple(range(NEXP))):
    st = contextlib.ExitStack()
    with st:
        sb = lambda name, shape, dt: st.enter_context(nc.sbuf_tensor(name, shape, dt))
        ps = lambda name, shape, dt: st.enter_context(nc.psum_tensor(name, shape, dt))
        NS = 3
        ident = sb("f_ident", [128, 128], BF16)
        xs = [sb(f"f_xs{i}", [128, 2, 1024], BF16) for i in range(3)]
        wg32 = [sb(f"f_wg32_{i}", [128, 8, 256], F32) for i in range(NS)]
        wu32 = [sb(f"f_wu32_{i}", [128, 8, 256], F32) for i in range(NS)]
        wd32 = [sb(f"f_wd32_{i}", [128, 2, 1024], F32) for i in range(NS)]
        wg_ = [sb(f"f_wg{i}", [128, 8, 256], BF16) for i in range(2)]
        wu_ = [sb(f"f_wu{i}", [128, 8, 256], BF16) for i in range(2)]
        wd_ = [sb(f"f_wd{i}", [128, 2, 1024], BF16) for i in range(2)]
        xT = [sb(f"f_xT{i}", [128, 8, 128], BF16) for i in range(3)]
        sg = [sb(f"f_sg{i}", [128, 2, 128], F32) for i in range(2)]
        hid = [sb(f"f_hid{i}", [128, 2, 128], BF16) for i in range(2)]
        ys = [sb(f"f_ys{i}", [128, 2, 1024], BF16) for i in range(2)]
        ptr = [ps(f"f_ptr{i}", [128, 8, 128], BF16) for i in range(2)]
        pgu = [ps(f"f_pgu{i}", [128, 4, 128], F32) for i in range(2)]
        py = [ps(f"f_py{i}", [128, 1024], F32) for i in range(2)]
        P.add("sync", lambda e: e.dma_start(out=ident[:], in_=g.ident), w=["ident"], chan="f_c")
        ne = len(experts)

        def load(n_):
            ex = experts[n_]
            b3 = n_ % NS
            P.add("sync", lambda e, ex=ex, b3=b3: e.dma_start(
                out=wg32[b3][:], in_=g.w_gate_e[ex].rearrange("(p c) n -> p c n", p=128)), w=[("wg32", b3)],
                chan=f"f_wg{b3}")
            P.add("scalar", lambda e, ex=ex, b3=b3: e.dma_start(
                out=wu32[b3][:], in_=g.w_up_e[ex].rearrange("(p c) n -> p c n", p=128)), w=[("wu32", b3)],
                chan=f"f_wu{b3}")
            P.add("sync", lambda e, ex=ex, b3=b3: e.dma_start(
                out=wd32[b3][:], in_=g.w_down_e[ex].rearrange("(p c) n -> p c n", p=128)), w=[("wd32", b3)],
                chan=f"f_wd{b3}")

        def cast(n_):
            b3 = n_ % NS
            b2 = n_ % 2
            P.add("scalar", lambda e, b3=b3, b2=b2: e.activation(out=wg_[b2][:], in_=wg32[b3][:], func=AF.Copy),
                  r=[("wg32", b3)], w=[("wg", b2)])
            P.add("vector", lambda e, b3=b3, b2=b2: e.tensor_copy(out=wu_[b2][:], in_=wu32[b3][:]),
                  r=[("wu32", b3)], w=[("wu", b2)])
            P.add("scalar", lambda e, b3=b3, b2=b2: e.activation(out=wd_[b2][:, 0, :], in_=wd32[b3][:, 0, :], func=AF.Copy),
                  r=[("wd32", b3)], w=[("wd", b2, 0)])
            P.add("vector", lambda e, b3=b3, b2=b2: e.tensor_copy(out=wd_[b2][:, 1, :], in_=wd32[b3][:, 1, :]),
                  r=[("wd32", b3)], w=[("wd", b2, 1)])

        NH = CAP // 128
        items = [(n_, half) for n_ in range(ne) for half in range(NH)]

        def st_lx(n_):
            x3 = n_ % 3
            r0 = experts[n_] * CAP
            P.add("sync", lambda e, r0=r0, x3=x3: e.dma_start(
                out=xs[x3][:], in_=g.xs_d[r0:r0 + CAP, :].rearrange("(p h) d -> p h d", h=2)),
                w=[("xs", x3)], chan=f"f_xs{x3}")

        def st_t(i):
            n_, half = items[i]
            x3 = i % 3
            xe = n_ % 3
            b2 = i % 2
            for dc in range(8):
                P.add("tensor", lambda e, dc=dc, b2=b2, xe=xe, half=half: e.transpose(
                    out=ptr[b2][:, dc, :], in_=xs[xe][:, half, :].rearrange("t (p c) -> t c p", c=8)[:, dc, :],
                    identity=ident[:]),
                    r=[("xs", xe), "ident"], w=[("ptr", b2)])
            P.add("vector", lambda e, b2=b2, x3=x3: e.tensor_copy(out=xT[x3][:], in_=ptr[b2][:]),
                  r=[("ptr", b2)], w=[("xT", x3)])

        def st_gu(i):
            n_, half = items[i]
            x3 = i % 3
            b2 = i % 2
            wb = n_ % 2
            for j, wt in enumerate((wg_[wb], wu_[wb])):
                wk_ = ("wg", wb) if j == 0 else ("wu", wb)
                for hh in range(2):
                    for dc in range(8):
                        P.add("tensor", lambda e, j=j, wt=wt, hh=hh, dc=dc, b2=b2, x3=x3: e.matmul(
                            pgu[b2][:, 2 * j + hh, :], lhsT=wt[:, dc, :].rearrange("p (m h) -> p h m", h=2)[:, hh, :],
                            rhs=xT[x3][:, dc, :],
                            start=(dc == 0), stop=(dc == 7)), r=[wk_, ("xT", x3)], w=[("pgu", b2)])
            P.add("scalar", lambda e, b2=b2: e.activation(out=sg[b2][:], in_=pgu[b2][:, 0:2, :], func=AF.Silu),
                  r=[("pgu", b2)], w=[("sg", b2)])
            P.add("vector", lambda e, b2=b2: e.tensor_tensor(out=hid[b2][:], in0=sg[b2][:], in1=pgu[b2][:, 2:4, :],
                                                            op=ALU.mult),
                  r=[("sg", b2), ("pgu", b2)], w=[("hid", b2)])

        def st_dn(i):
            n_, half = items[i]
            b2 = i % 2
            wb = n_ % 2
            r0 = experts[n_] * CAP + half * 128
            for nh in range(2):
                for hh in range(2):
                    P.add("tensor", lambda e, nh=nh, hh=hh, b2=b2, wb=wb: e.matmul(
                        py[b2][:, nh * 512:(nh + 1) * 512], lhsT=hid[b2][:, hh, :],
                        rhs=wd_[wb][:, hh, nh * 512:(nh + 1) * 512], start=(hh == 0), stop=(hh == 1)),
                        r=[("hid", b2), ("wd", wb, hh)], w=[("py", b2, nh)])
            y2 = n_ % 2
            P.add("scalar", lambda e, b2=b2, y2=y2, half=half: e.activation(
                out=ys[y2][:, half, 0:512], in_=py[b2][:, 0:512], func=AF.Copy),
                r=[("py", b2, 0)], w=[("ys", y2, half, 0)])
            P.add("vector", lambda e, b2=b2, y2=y2, half=half: e.tensor_copy(
                out=ys[y2][:, half, 512:1024], in_=py[b2][:, 512:1024]),
                r=[("py", b2, 1)], w=[("ys", y2, half, 1)])
            if half == NH - 1:
                rbase = experts[n_] * CAP
                P.add("gpsimd", lambda e, rbase=rbase, y2=y2: e.dma_start(
                    out=g.ys_d[rbase:rbase + CAP, :].rearrange("(p h) d -> p h d", h=2), in_=ys[y2][:]),
                    r=[("ys", y2, h_, q_) for h_ in range(2) for q_ in range(2)], w=[("ys_d", rbase)],
                    chan=f"f_ys{y2}")

        load(0)
        if ne > 1:
            load(1)
        cast(0)
        ni = len(items)
        st_lx(0)
        if ne > 1:
            st_lx(1)
        for step in range(ni + 2):
            if step < ni:
                n_t, half_t = items[step]
                if half_t == 0 and n_t + 2 < ne:
                    st_lx(n_t + 2)
                st_t(step)
            i1 = step - 1
            if 0 <= i1 < ni:
                n_, half = items[i1]
                if half == 0 and n_ + 2 < ne:
                    load(n_ + 2)
                st_gu(i1)
                if half == NH - 1 and n_ + 1 < ne:
                    cast(n_ + 1)
            i2 = step - 2
            if 0 <= i2 < ni:
                st_dn(i2)
        P.emit_phase()


def phase_g(P, nc, g, blocks=tuple(range(NOWN))):
    st = contextlib.ExitStack()
    with st:
        sb = lambda name, shape, dt: st.enter_context(nc.sbuf_tensor(name, shape, dt))
        ps = lambda name, shape, dt: st.enter_context(nc.psum_tensor(name, shape, dt))
        wgs = sb("g_wgs", [128, 8, 256], BF16)
        wus = sb("g_wus", [128, 8, 256], BF16)
        wds = sb("g_wds", [128, 2, 1024], BF16)
        g2rep = sb("g_g2", [128, 1024], F32)
        b2rep = sb("g_b2", [128, 1024], F32)
        h1 = [sb(f"g_h1_{i}", [128, 1024], F32) for i in range(2)]
        h1T = [sb(f"g_h1T{i}", [128, 8, 128], BF16) for i in range(2)]
        yk = [sb(f"g_yk{i}", [128, 1024], BF16) for i in range(6)]
        acc = sb("g_acc", [128, 1024], F32)
        sg = sb("g_sg", [128, 2, 128], F32)
        hid = sb("g_hid", [128, 2, 128], BF16)
        ot = [sb(f"g_ot{i}", [128, 1024], F32) for i in range(2)]
        stats = sb("g_stats", [128, 2, 6], F32)
        mv = sb("g_mv", [128, 2], F32)
        std = sb("g_std", [128, 1], F32)
        rstd = sb("g_rstd", [128, 1], F32)
        pgu = ps("g_pgu", [128, 4, 128], F32)
        py = ps("g_py", [128, 1024], F32)
        _bounds_reg(P, g)
        P.add("gpsimd", lambda e: e.dma_start(out=wgs[:], in_=g.w_gate_s.rearrange("(c p) n -> p c n", p=128)),
              w=["wgs"], chan="g_w")
        P.add("gpsimd", lambda e: e.dma_start(out=wus[:], in_=g.w_up_s.rearrange("(c p) n -> p c n", p=128)),
              w=["wus"], chan="g_w")
        P.add("gpsimd", lambda e: e.dma_start(out=wds[:], in_=g.w_down_s.rearrange("(c p) n -> p c n", p=128)),
              w=["wds"], chan="g_w")
        P.add("sync", lambda e: e.dma_start(out=g2rep[:], in_=g.ln2_g.partition_broadcast(128)), w=["g2rep"], chan="g_c")
        P.add("sync", lambda e: e.dma_start(out=b2rep[:], in_=g.ln2_b.partition_broadcast(128)), w=["b2rep"], chan="g_c")
        for i in range(6):
            P.add("gpsimd", lambda e, i=i: e.memset(yk[i][:], 0.0), w=[("yk", i)])
        yi = 0
        for blk in blocks:
            b2 = blk % 2
            P.add("sync", lambda e, blk=blk, b2=b2: e.dma_start(out=h1[b2][:], in_=g.h1_d[blk * 128:(blk + 1) * 128, :]),
                  w=[("h1", b2)], chan=f"g_h1{b2}")
            P.add("sync", lambda e, blk=blk, b2=b2: e.dma_start(out=h1T[b2][:], in_=g.h1T_d[:, :, blk * 128:(blk + 1) * 128]),
                  w=[("h1T", b2)], chan=f"g_h1T{b2}")
            for k in range(8):
                y3 = yi % 6
                yi += 1
                P.add("gpsimd", lambda e, blk=blk, k=k, y3=y3: e.indirect_dma_start(
                    out=yk[y3][:, :], out_offset=None, in_=g.ys_d[:, :],
                    in_offset=bass.IndirectOffsetOnAxis(ap=g.sidx[:, blk, k:k + 1], axis=0),
                    bounds_check=g.bcreg[0], oob_is_err=False),
                    r=[("sidx", blk)], w=[("yk", y3)], chan=f"g_yk{y3}")
                if k == 0:
                    P.add("vector", lambda e, blk=blk, y3=y3: e.tensor_scalar(
                        out=acc[:], in0=yk[y3][:], scalar1=g.gk[:, blk, 0:1], scalar2=None, op0=ALU.mult),
                        r=[("yk", y3), ("gk", blk)], w=["acc"])
                else:
                    P.add("vector", lambda e, blk=blk, k=k, y3=y3: e.scalar_tensor_tensor(
                        out=acc[:], in0=yk[y3][:], scalar=g.gk[:, blk, k:k + 1], in1=acc[:], op0=ALU.mult, op1=ALU.add),
                        r=[("yk", y3), ("gk", blk), "acc"], w=["acc"])
            _swiglu_block(P, h1T[b2], ("h1T", b2), wgs, wus, wds, ["wgs", "wus", "wds"],
                          pgu, "pgu", sg, "sg", hid, "hid", py, ("py",))
            o_t = ot[b2]
            P.add("vector", lambda e, b2=b2: e.scalar_tensor_tensor(
                out=acc[:], in0=h1[b2][:], scalar=ALPHA, in1=acc[:], op0=ALU.mult, op1=ALU.add),
                r=[("h1", b2), "acc"], w=["acc"])
            for nh in range(2):
                P.add("vector", lambda e, nh=nh: e.tensor_tensor(
                    out=acc[:, nh * 512:(nh + 1) * 512], in0=acc[:, nh * 512:(nh + 1) * 512],
                    in1=py[:, nh * 512:(nh + 1) * 512], op=ALU.add), r=["acc", ("py", nh)], w=["acc"])
            _layer_norm(P, acc, "acc", "acc", stats, mv, std, rstd, o_t, ("ot", b2), g2rep, "g2rep", b2rep, "b2rep", "g")
            P.add("scalar", lambda e, blk=blk, o_t=o_t: e.dma_start(out=g.out[blk * 128:(blk + 1) * 128, :], in_=o_t[:]),
                  r=[("ot", b2)], w=[("out", blk)], chan=f"g_out{b2}")
        P.emit_phase()


def build_program(debug=None, ntg=16, sb_groups=(0, 1, 2, 3), dl_slots=tuple(range(NOWN)), phases="abcdfg", d_groups=(0, 1, 2, 3),
                  f_experts=tuple(range(NEXP)), g_blocks=tuple(range(NOWN)), d_stop=9):
    nc = bass.Bass("TRN2", target_bir_lowering=False)
    g = Ctx()
    g.bcreg = []

    def din(name, shape, dt=F32):
        return nc.dram_tensor(name, shape, dt, kind="ExternalInput").ap()

    dbgnames = set(debug or ())

    def dscr(name, shape, dt):
        kind = "ExternalOutput" if name in dbgnames else "Internal"
        return nc.dram_tensor(name, shape, dt, kind=kind).ap()

    g.x_ctx = din("x_ctx", [NB * 128, D])
    g.valid = din("valid", [128, NB])
    g.ident = din("ident", [128, 128], BF16)
    g.negU = din("negU", [128, 128], BF16)
    g.negOnes = din("negOnes", [128, 128], BF16)
    g.sbmask = din("sbmask", [128, 16, 512], BF16)
    g.dlbias = din("dlbias", [128, DL_NT, 128], BF16)
    g.padbias = din("padbias", [128, NB])
    g.w_br_sb = din("w_br_sb", [512, D])
    g.w_br_dil = din("w_br_dil", [256, D])
    g.w_out = din("w_out", [D, D])
    g.w_router = din("w_router", [D, NEXP])
    g.b_gateT = din("b_gateT", [128, 16])
    g.ln1_g = din("ln1_g", [D])
    g.ln1_b = din("ln1_b", [D])
    g.router_bias = din("router_bias", [NEXP])
    ne_decl = NEXP if "f" in phases else 1
    g.w_gate_e = din("w_gate_e", [ne_decl, D, 256])
    g.w_up_e = din("w_up_e", [ne_decl, D, 256])
    g.w_down_e = din("w_down_e", [ne_decl, 256, D])
    g.w_gate_s = din("w_gate_s", [D, 256])
    g.w_up_s = din("w_up_s", [D, 256])
    g.w_down_s = din("w_down_s", [256, D])
    g.ln2_g = din("ln2_g", [D])
    g.ln2_b = din("ln2_b", [D])
    g.iota256 = din("iota256", [128, 256])
    g.lstrict = din("lstrict", [128, 128], BF16)
    g.ones = din("ones", [128, 128], BF16)
    g.ln_in_g = din("ln_in_g", [D])
    g.ln_in_b = din("ln_in_b", [D])
    g.ln_in_gT = din("ln_in_gT", [128, 8])
    g.ln_in_bT = din("ln_in_bT", [128, 8])
    g.w_in = din("w_in", [D, 5888])
    g.out = nc.dram_tensor("out", [TOWN, D], F32, kind="ExternalOutput").ap()

    g.kT_d = dscr("kT_d", [10, 128, S], BF16)
    g.v_d = dscr("v_d", [S, VW], BF16)
    g.qT_d = dscr("qT_d", [128, 10, TOWN], BF16)
    g.osbT_d = dscr("osbT_d", [8, 64, TOWN], BF16)
    g.odlT_d = dscr("odlT_d", [2, 128, TOWN], BF16)
    g.h_own_d = dscr("h_own_d", [TOWN, D], F32)
    g.h1_d = dscr("h1_d", [TOWN, D], F32)
    g.h1T_d = dscr("h1T_d", [128, 8, TOWN], BF16)
    g.xs_d = dscr("xs_d", [NEXP * CAP, D], BF16)
    g.ys_d = dscr("ys_d", [NEXP * CAP, D], BF16)
    g.hT_own_d = dscr("hT_own_d", [128, 8, TOWN], BF16)

    with contextlib.ExitStack() as stack:
        P = Prog(nc, stack)
        g.gk = stack.enter_context(nc.sbuf_tensor("gk", [128, NOWN, 8], F32))
        g.sidx = stack.enter_context(nc.sbuf_tensor("sidx", [128, NOWN, 8], I32))
        if "a" in phases:
            phase_a(P, nc, g, ntg)
        if "b" in phases:
            phase_b(P, nc, g, sb_groups)
        if "c" in phases:
            phase_c(P, nc, g, dl_slots)
        if "d" in phases:
            phase_d(P, nc, g, d_groups, d_stop)
        if "f" in phases:
            phase_f(P, nc, g, f_experts)
        if "g" in phases:
            phase_g(P, nc, g, g_blocks)
        if "gk" in dbgnames:
            dgk = nc.dram_tensor("dbg_gk", [128, NOWN, 8], F32, kind="ExternalOutput").ap()
            dsi = nc.dram_tensor("dbg_sidx", [128, NOWN, 8], I32, kind="ExternalOutput").ap()
            nbk = 4 * len(d_groups)
            P.add("sync", lambda e: e.dma_start(out=dgk[:, 0:nbk, :], in_=g.gk[:, 0:nbk, :]), w=["dgk"], chan="dbg")
            P.add("sync", lambda e: e.dma_start(out=dsi[:, 0:nbk, :], in_=g.sidx[:, 0:nbk, :]), w=["dsi"], chan="dbg")
            P.emit_phase()
    return nc


def _rel_bucket_np(dist):
    dist = np.asarray(dist, np.int64)
    max_exact = 16
    d = np.maximum(dist, 1).astype(np.float32)
    large = max_exact + (np.log(d / np.float32(max_exact)) / np.float32(np.log(2048 / 16))
                         * np.float32(32 - max_exact)).astype(np.int32)
    large = np.minimum(large, 31)
    return np.where(dist < max_exact, dist, large)


def host_consts(inputs):
    bf = ml_dtypes.bfloat16
    kl = np.arange(128)[:, None]
    ql = np.arange(128)[None, :]
    ident = np.eye(128, dtype=np.float32).astype(bf)
    negU = np.where(kl >= ql, -1.0, 0.0).astype(np.float32).astype(bf)
    negOnes = np.full((128, 128), -1.0, np.float32).astype(bf)
    sbmask = np.zeros((128, 16, 512), np.float32)
    for rel_c in range(16):
        for sl in range(4):
            rel_cq = 4 * sl + 3
            if rel_c == rel_cq:
                sbmask[:, rel_c, sl * 128:(sl + 1) * 128] = np.where(kl < ql, 0.0, NEG)
            elif rel_c > rel_cq:
                sbmask[:, rel_c, sl * 128:(sl + 1) * 128] = NEG
    rel_bias = np.asarray(inputs["rel_bias"], np.float32)
    dlbias = np.zeros((128, DL_NT, 128), np.float32)
    for gi, (w, dil) in enumerate(DIL):
        for hg in range(4):
            for o in range(DL_NB[gi]):
                dist = 128 * o + ql - kl
                ok = (dist >= 0) & (dist <= w) & (dist % dil == 0)
                bk = _rel_bucket_np(np.clip(dist, 0, None))
                val = rel_bias[bk, 4 * gi + hg]
                dlbias[:, DL_TOFF[gi] + hg * DL_NB[gi] + o, :] = np.where(ok, val, NEG)
    f32 = lambda k: np.ascontiguousarray(np.asarray(inputs[k], np.float32)[0])
    return dict(ident=ident, negU=negU, negOnes=negOnes, sbmask=sbmask.astype(bf), dlbias=dlbias.astype(bf),
                w_br_sb=f32("w_br_sb"), w_br_dil=f32("w_br_dil"), w_out=f32("w_out"), w_router=f32("w_router"),
                b_gateT=np.ascontiguousarray(f32("b_gate").reshape(16, 128).T),
                ln1_g=f32("ln1_g"), ln1_b=f32("ln1_b"), router_bias=f32("router_bias"),
                w_gate_e=f32("w_gate_e"), w_up_e=f32("w_up_e"), w_down_e=f32("w_down_e"),
                w_gate_s=f32("w_gate_s"), w_up_s=f32("w_up_s"), w_down_s=f32("w_down_s"),
                ln2_g=f32("ln2_g"), ln2_b=f32("ln2_b"),
                iota256=np.tile(np.arange(256, dtype=np.float32)[None, :], (128, 1)),
                lstrict=np.where(kl < ql, 1.0, 0.0).astype(np.float32).astype(bf),
                ones=np.ones((128, 128), np.float32).astype(bf))


def host_inputs(inputs):
    x = np.asarray(inputs["x"], dtype=np.float32)
    maps = []
    consts = host_consts(inputs)
    for core in range(NCORES):
        b, j = core // 4, core % 4
        xc = np.zeros((NB, 128, D), np.float32)
        valid = np.zeros((128, NB), np.float32)
        xb = x[b].reshape(64, 128, D)
        for c in range(NB):
            gb = c + j - 3
            if gb >= 0:
                xc[c] = xb[gb]
                valid[:, c] = 1.0
        m = {
            "x_ctx": xc.reshape(NB * 128, D),
            "valid": valid,
            "padbias": np.where(valid > 0, 0.0, NEG).astype(np.float32),
            "ln_in_g": np.asarray(inputs["ln_in_g"], np.float32),
            "ln_in_b": np.asarray(inputs["ln_in_b"], np.float32),
            "ln_in_gT": np.ascontiguousarray(np.asarray(inputs["ln_in_g"], np.float32).reshape(8, 128).T),
            "ln_in_bT": np.ascontiguousarray(np.asarray(inputs["ln_in_b"], np.float32).reshape(8, 128).T),
            "w_in": np.ascontiguousarray(np.asarray(inputs["w_in"], np.float32)[0]),
        }
        m.update(consts)
        maps.append(m)
    return maps


def kernel(**inputs):
    nc = build_program()
    maps = host_inputs(inputs)
    res = run_bass_kernel_spmd(nc, maps, core_ids=list(range(NCORES)))
    out = np.zeros((2, S, D), np.float32)
    for core in range(NCORES):
        b, j = core // 4, core % 4
        o = res.results[core]["out"].reshape(NOWN, 128, D)
        ob = out[b].reshape(64, 128, D)
        for s in range(NOWN):
            ob[4 * s + j] = o[s]
    return out
```

```python
import contextlib
import numpy as np
import ml_dtypes
import concourse.bass as bass
import concourse.mybir as mybir
from concourse.bass_utils import run_bass_kernel_spmd

F32 = mybir.dt.float32
BF16 = mybir.dt.bfloat16
I32 = mybir.dt.int32
U32 = mybir.dt.uint32
AF = mybir.ActivationFunctionType
ALU = mybir.AluOpType
AX = mybir.AxisListType

NCORES = 8
D = 1024
S = 8192
NB = 64
NOWN = 16
TOWN = NOWN * 128
LN_EPS = 1e-5
ALPHA = 2.0 ** 0.25
NEG = -30000.0
NEXP = 256
CAP = 256
TOPK = 8
VW = 512 + 12 * 65


class Op:
    __slots__ = ("eng", "fn", "deps", "chan", "sig", "count", "idx", "chan_count")


class Prog:
    ENGS = ("tensor", "vector", "scalar", "gpsimd", "sync")

    def __init__(self, nc, stack):
        self.nc = nc
        self.stack = stack
        self.sems = {e: stack.enter_context(nc.semaphore("sem_" + e)) for e in self.ENGS}
        self.sig_total = {e: 0 for e in self.ENGS}
        self.chan_sem = {}
        self.chan_total = {}
        self.reset_phase()

    def reset_phase(self):
        self.ops = []
        self.last_w = {}
        self.readers = {}
        self.chan_emitted = dict(self.chan_total)

    def chan(self, name):
        if name not in self.chan_sem:
            self.chan_sem[name] = self.stack.enter_context(self.nc.semaphore("ch_" + name))
            self.chan_total[name] = 0
            self.chan_emitted[name] = 0
        return name

    def add(self, eng, fn, r=(), w=(), chan=None):
        op = Op()
        op.eng = eng
        op.fn = fn
        op.chan = chan
        op.sig = False
        op.idx = len(self.ops)
        deps = set()
        for k in r:
            lw = self.last_w.get(k)
            if lw is not None:
                deps.add(lw)
        for k in w:
            lw = self.last_w.get(k)
            if lw is not None:
                deps.add(lw)
            for rd in self.readers.get(k, ()):
                deps.add(rd)
        deps.discard(op.idx)
        op.deps = []
        for d in deps:
            dop = self.ops[d]
            if dop.chan is not None:
                op.deps.append(("chan", dop.chan, self.chan_emitted[dop.chan]))
            else:
                if dop.eng == "tensor" and eng == "tensor":
                    continue
                dop.sig = True
                op.deps.append(("eng", dop.eng, dop))
        if chan is not None:
            self.chan(chan)
            self.chan_emitted[chan] += 16
            op.chan_count = self.chan_emitted[chan]
        self.ops.append(op)
        for k in r:
            self.readers.setdefault(k, []).append(op.idx)
        for k in w:
            self.last_w[k] = op.idx
            self.readers[k] = []
        return op

    def emit_phase(self):
        nc = self.nc
        last = {}
        for op in self.ops:
            if op.chan is None:
                last[op.eng] = op
        for op in last.values():
            op.sig = True
        tot = dict(self.sig_total)
        for op in self.ops:
            if op.chan is None and op.sig:
                tot[op.eng] += 1
                op.count = tot[op.eng]
        final_eng = dict(tot)
        final_chan = dict(self.chan_emitted)
        per_eng = {e: [o for o in self.ops if o.eng == e] for e in self.ENGS}
        sems = self.sems
        chan_sem = self.chan_sem

        def run(e, eobj):
            waited = {}

            def wait(kind, name, val):
                key = (kind, name)
                if waited.get(key, -1) >= val:
                    return
                waited[key] = val
                eobj.wait_ge(sems[name] if kind == "eng" else chan_sem[name], val)

            for op in per_eng[e]:
                for kind, name, v in op.deps:
                    wait(kind, name, v.count if kind == "eng" else v)
                ins = op.fn(eobj)
                if op.chan is not None:
                    ins.then_inc(chan_sem[op.chan], 16)
                elif op.sig:
                    ins.then_inc(sems[e], 1)
            for name, v in final_chan.items():
                if v > 0:
                    wait("chan", name, v)
            for name, v in final_eng.items():
                if v > 0 and name != e:
                    wait("eng", name, v)

        with nc.Block() as block:
            @block.tensor
            def _(e):
                run("tensor", e)

            @block.vector
            def _(e):
                run("vector", e)

            @block.scalar
            def _(e):
                run("scalar", e)

            @block.gpsimd
            def _(e):
                run("gpsimd", e)

            @block.sync
            def _(e):
                run("sync", e)

        self.sig_total = final_eng
        self.chan_total = final_chan
        self.reset_phase()


class Ctx:
    pass


def phase_a(P, nc, g, ntg=16):
    st = contextlib.ExitStack()
    with st:
        sb = lambda name, shape, dt: st.enter_context(nc.sbuf_tensor(name, shape, dt))
        ps = lambda name, shape, dt: st.enter_context(nc.psum_tensor(name, shape, dt))
        winb = sb("a_winb", [128, 8, 3840], BF16)
        gT = sb("a_gT", [128, 8], F32)
        bT = sb("a_bT", [128, 8], F32)
        grep = sb("a_grep", [128, 1024], F32)
        brep = sb("a_brep", [128, 1024], F32)
        valid = sb("a_valid", [128, NB], F32)
        ident = sb("a_ident", [128, 128], BF16)
        NX = 3
        xt = [sb(f"a_x{i}", [128, 1024], F32) for i in range(NX)]
        stats = [sb(f"a_stats{i}", [128, 2, 6], F32) for i in range(2)]
        mv = [sb(f"a_mv{i}", [128, 2], F32) for i in range(2)]
        rstd = [sb(f"a_rstd{i}", [128, 1], F32) for i in range(2)]
        std = [sb(f"a_std{i}", [128, 1], F32) for i in range(2)]
        ybf = [sb(f"a_ybf{i}", [128, 1024], BF16) for i in range(2)]
        y32 = sb("a_y32", [128, 1024], F32)
        hTg = [sb(f"a_hTg{i}", [128, 8, 512], BF16) for i in range(2)]
        kst = [sb(f"a_kst{i}", [128, 512], BF16) for i in range(3)]
        vst = [sb(f"a_vst{i}", [128, VW], BF16) for i in range(2)]
        qst = [sb(f"a_qst{i}", [128, 10, 128], BF16) for i in range(2)]
        ones12 = sb("a_ones12", [128, 12, 1], F32)
        tp = [ps(f"a_tp{i}", [128, 8, 128], BF16) for i in range(2)]
        pm = [ps(f"a_pm{i}", [128, 512], F32) for i in range(4)]

        for dc in range(8):
            P.add("gpsimd", lambda e, dc=dc: e.dma_start(
                out=winb[:, dc, :], in_=g.w_in[dc * 128:(dc + 1) * 128, 0:3840]),
                w=[("winb", dc)], chan="a_w")
        P.add("sync", lambda e: e.dma_start(out=gT[:], in_=g.ln_in_gT),
              w=["gT"], chan="a_c")
        P.add("sync", lambda e: e.dma_start(out=bT[:], in_=g.ln_in_bT),
              w=["bT"], chan="a_c")
        P.add("sync", lambda e: e.dma_start(out=grep[:], in_=g.ln_in_g.partition_broadcast(128)),
              w=["grep"], chan="a_c")
        P.add("sync", lambda e: e.dma_start(out=brep[:], in_=g.ln_in_b.partition_broadcast(128)),
              w=["brep"], chan="a_c")
        P.add("sync", lambda e: e.dma_start(out=valid[:], in_=g.valid), w=["valid"], chan="a_c")
        P.add("sync", lambda e: e.dma_start(out=ident[:], in_=g.ident), w=["ident"], chan="a_c")

        P.add("vector", lambda e: e.memset(ones12[:], 1.0), w=["ones12"])
        kcols = [512 + 128 * i for i in range(4)] + [2304 + 128 * i for i in range(6)]
        qcols = [0 + 128 * i for i in range(4)] + [1536 + 128 * i for i in range(6)]
        vgroups = [(1024, 512, 0), (3072, 512, 512), (3584, 256, 1024)]
        cnt = {'pmi': 0, 'ksi': 0}

        def lnt(tg, bi):
            hT = hTg[tg % 2]
            hk = ("hTg", tg % 2)
            c = 4 * tg + bi
            xs = c % NX
            s2 = c % 2
            x_t = xt[xs]
            P.add("sync", lambda e, x_t=x_t, c=c: e.dma_start(
                out=x_t[:], in_=g.x_ctx[c * 128:(c + 1) * 128, :]),
                w=[("x", xs)], chan=f"a_x{xs}")
            for hh in range(2):
                P.add("vector", lambda e, x_t=x_t, s2=s2, hh=hh: e.bn_stats(
                    out=stats[s2][:, hh, :], in_=x_t[:, hh * 512:(hh + 1) * 512]),
                    r=[("x", xs)], w=[("stats", s2, hh)])
            P.add("vector", lambda e, s2=s2: e.bn_aggr(
                out=mv[s2][:], in_=stats[s2][:].rearrange("p a b -> p (a b)")),
                r=[("stats", s2, 0), ("stats", s2, 1)], w=[("mv", s2)])
            P.add("scalar", lambda e, s2=s2: e.activation(
                out=std[s2][:], in_=mv[s2][:, 1:2], func=AF.Sqrt, bias=LN_EPS),
                r=[("mv", s2)], w=[("std", s2)])
            P.add("vector", lambda e, s2=s2: e.reciprocal(out=rstd[s2][:], in_=std[s2][:]),
                r=[("std", s2)], w=[("rstd", s2)])
            P.add("vector", lambda e, x_t=x_t, s2=s2: e.tensor_scalar(
                out=ybf[s2][:], in0=x_t[:], scalar1=mv[s2][:, 0:1], scalar2=rstd[s2][:, 0:1],
                op0=ALU.subtract, op1=ALU.mult),
                r=[("x", xs), ("mv", s2), ("rstd", s2)], w=[("ybf", s2)])
            if bi == 3:
                so = tg
                P.add("gpsimd", lambda e, x_t=x_t, s2=s2: e.tensor_scalar(
                    out=y32[:], in0=x_t[:], scalar1=mv[s2][:, 0:1], scalar2=rstd[s2][:, 0:1],
                    op0=ALU.subtract, op1=ALU.mult),
                    r=[("x", xs), ("mv", s2), ("rstd", s2)], w=["y32"])
                P.add("gpsimd", lambda e: e.tensor_tensor(out=y32[:], in0=y32[:], in1=grep[:], op=ALU.mult),
                      r=["y32", "grep"], w=["y32"])
                P.add("gpsimd", lambda e: e.tensor_tensor(out=y32[:], in0=y32[:], in1=brep[:], op=ALU.add),
                      r=["y32", "brep"], w=["y32"])
                P.add("gpsimd", lambda e, so=so: e.dma_start(
                    out=g.h_own_d[so * 128:(so + 1) * 128, :], in_=y32[:]),
                    r=["y32"], w=[("h_own_d", so)], chan="a_y32")

        def tr(tg, bi):
            hT = hTg[tg % 2]
            hk = ("hTg", tg % 2)
            c = 4 * tg + bi
            s2 = c % 2
            tps = tp[c % 2]
            for dc in range(8):
                P.add("tensor", lambda e, tps=tps, s2=s2, dc=dc: e.transpose(
                    out=tps[:, dc, :], in_=ybf[s2][:, dc * 128:(dc + 1) * 128], identity=ident[:]),
                    r=[("ybf", s2), "ident"], w=[("tp", c % 2)])
            for dc in range(8):
                P.add("scalar", lambda e, tps=tps, dc=dc, hT=hT, bi=bi: e.activation(
                    out=hT[:, dc, bi * 128:(bi + 1) * 128], in_=tps[:, dc, :], func=AF.Identity,
                    scale=gT[:, dc:dc + 1], bias=bT[:, dc:dc + 1]),
                    r=[("tp", c % 2), "gT", "bT"], w=[hk + (bi,)])

        def mm(tg):
            hT = hTg[tg % 2]
            hk = ("hTg", tg % 2)
            pmi = cnt['pmi']
            ksi = cnt['ksi']
            hkall = [hk + (bi,) for bi in range(4)]
            P.add("gpsimd", lambda e, hT=hT, tg=tg: e.dma_start(
                out=g.hT_own_d[:, :, tg * 128:(tg + 1) * 128], in_=hT[:, :, 384:512]),
                r=[hk + (3,)], w=[("hT_own_d", tg)], chan=f"a_hT{tg % 2}")
            for kc in range(10):
                pmt = pm[pmi % 4]
                pk = ("pm", pmi % 4)
                pmi += 1
                for dc in range(8):
                    P.add("tensor", lambda e, pmt=pmt, dc=dc, kc=kc, hT=hT: e.matmul(
                        pmt[:], lhsT=winb[:, dc, kcols[kc]:kcols[kc] + 128], rhs=hT[:, dc, :],
                        start=(dc == 0), stop=(dc == 7)),
                        r=[("winb", dc)] + hkall, w=[pk])
                ks = kst[ksi % 3]
                kk = ("kst", ksi % 3)
                ksi_l = ksi % 3
                ksi += 1
                eng = "scalar" if kc % 2 == 0 else "vector"
                if eng == "scalar":
                    P.add("scalar", lambda e, ks=ks, pmt=pmt: e.activation(out=ks[:], in_=pmt[:], func=AF.Copy),
                          r=[pk], w=[kk])
                else:
                    P.add("vector", lambda e, ks=ks, pmt=pmt: e.tensor_copy(out=ks[:], in_=pmt[:]),
                          r=[pk], w=[kk])
                P.add("gpsimd", lambda e, ks=ks, kc=kc, tg=tg: e.dma_start(
                    out=g.kT_d[kc, :, tg * 512:(tg + 1) * 512], in_=ks[:]),
                    r=[kk], w=[("kT_d", kc, tg)], chan=f"a_kst{ksi_l}")
                yield
            for bi in range(4):
                c = 4 * tg + bi
                vs = vst[c % 2]
                vk = ("vst", c % 2)
                for (c0, ncol, o0) in vgroups:
                    pmt = pm[pmi % 4]
                    pk = ("pm", pmi % 4)
                    pmi += 1
                    for dc in range(8):
                        P.add("tensor", lambda e, pmt=pmt, dc=dc, c0=c0, ncol=ncol, hT=hT, bi=bi: e.matmul(
                            pmt[:, 0:ncol], lhsT=hT[:, dc, bi * 128:(bi + 1) * 128], rhs=winb[:, dc, c0:c0 + ncol],
                            start=(dc == 0), stop=(dc == 7)),
                            r=[("winb", dc), hk + (bi,)], w=[pk])
                    if o0 == 0:
                        o_ap = vs[:, 0:512]
                        i_ap = pmt[:, 0:512]
                    else:
                        h0 = (o0 - 512) // 64
                        nh = ncol // 64
                        o_ap = vs[:, 512:VW].rearrange("p (h e) -> p h e", e=65)[:, h0:h0 + nh, 0:64]
                        i_ap = pmt[:, 0:ncol].rearrange("p (h e) -> p h e", e=64)
                    P.add("vector", lambda e, o_ap=o_ap, i_ap=i_ap, c=c: e.tensor_scalar(
                        out=o_ap, in0=i_ap, scalar1=valid[:, c:c + 1], scalar2=None,
                        op0=ALU.mult),
                        r=[pk, "valid"], w=[vk + (o0,)])
                    yield
                P.add("vector", lambda e, vs=vs, c=c: e.tensor_scalar(
                    out=vs[:, 512:VW].rearrange("p (h e) -> p h e", e=65)[:, :, 64:65], in0=ones12[:],
                    scalar1=valid[:, c:c + 1], scalar2=None, op0=ALU.mult),
                    r=["ones12", "valid"], w=[vk + (1024,)])
                P.add("gpsimd", lambda e, vs=vs, c=c: e.dma_start(
                    out=g.v_d[c * 128:(c + 1) * 128, :], in_=vs[:]),
                    r=[vk + (0,), vk + (512,), vk + (1024,)], w=[("v_d", c)], chan=f"a_vst{c % 2}")
            for qc in range(10):
                pmt = pm[pmi % 4]
                pk = ("pm", pmi % 4)
                pmi += 1
                for dc in range(8):
                    P.add("tensor", lambda e, pmt=pmt, dc=dc, qc=qc, hT=hT: e.matmul(
                        pmt[:, 0:128], lhsT=winb[:, dc, qcols[qc]:qcols[qc] + 128], rhs=hT[:, dc, 384:512],
                        start=(dc == 0), stop=(dc == 7)),
                        r=[("winb", dc), hk + (3,)], w=[pk])
                P.add("scalar", lambda e, pmt=pmt, qc=qc, tg=tg: e.activation(
                    out=qst[tg % 2][:, qc, :], in_=pmt[:, 0:128], func=AF.Copy, scale=0.125),
                    r=[pk], w=[("qst", tg % 2, qc)])
                yield
            P.add("gpsimd", lambda e, tg=tg: e.dma_start(
                out=g.qT_d[:, :, tg * 128:(tg + 1) * 128], in_=qst[tg % 2][:]),
                r=[("qst", tg % 2, qc) for qc in range(10)], w=[("qT_d", tg)], chan=f"a_qst{tg % 2}")
            cnt['pmi'] = pmi
            cnt['ksi'] = ksi

        for bi in range(4):
            lnt(0, bi)
            tr(0, bi)
        for tg in range(ntg):
            gen = mm(tg)
            for gi_, _ in enumerate(gen):
                if tg + 1 < ntg and gi_ in (0, 8, 16, 24):
                    lnt(tg + 1, gi_ // 8)
                if tg + 1 < ntg and gi_ in (6, 14, 22, 30):
                    tr(tg + 1, (gi_ - 6) // 8)
        P.emit_phase()


def phase_b(P, nc, g, groups=(0, 1, 2, 3)):
    st = contextlib.ExitStack()
    with st:
        sb = lambda name, shape, dt: st.enter_context(nc.sbuf_tensor(name, shape, dt))
        ps = lambda name, shape, dt: st.enter_context(nc.psum_tensor(name, shape, dt))
        nblk_max = 16 * (max(groups) + 1)
        kTsb = sb("b_kT", [128, 4, S], BF16)
        vsb = sb("b_v", [128, NB, 512], BF16)
        sbmask = sb("b_mask", [128, 16, 512], BF16)
        ident = sb("b_ident", [128, 128], BF16)
        negU = sb("b_negU", [128, 128], BF16)
        negOnes = sb("b_negOnes", [128, 128], BF16)
        zer = sb("b_zer", [128, 64], BF16)
        qsb = [sb(f"b_q{i}", [128, 4, 512], BF16) for i in range(2)]
        e_sb = [sb(f"b_e{i}", [128, 512], F32) for i in range(4)]
        sp_sb = [sb(f"b_sp{i}", [128, 512], BF16) for i in range(4)]
        w_sb = [sb(f"b_w{i}", [128, 512], BF16) for i in range(4)]
        srun = [[sb(f"b_srun{i}_{j}", [128, 512], BF16) for j in range(2)] for i in range(4)]
        ost = [sb(f"b_ost{i}", [64, 512], BF16) for i in range(4)]
        pz = [ps(f"b_pz{i}", [128, 512], F32) for i in range(4)]
        po = [ps(f"b_po{i}", [64, 512], F32) for i in range(4)]

        P.add("sync", lambda e: e.dma_start(out=ident[:], in_=g.ident), w=["ident"], chan="b_c")
        P.add("sync", lambda e: e.dma_start(out=negU[:], in_=g.negU), w=["negU"], chan="b_c")
        P.add("sync", lambda e: e.dma_start(out=negOnes[:], in_=g.negOnes), w=["negOnes"], chan="b_c")
        P.add("sync", lambda e: e.dma_start(out=sbmask[:], in_=g.sbmask), w=["sbmask"], chan="b_c")
        P.add("gpsimd", lambda e: e.memset(zer[:], 0.0), w=["zer"])
        ntok = nblk_max * 128
        for kc in range(4):
            for hf in range(0, ntok, 2048):
                P.add("sync", lambda e, kc=kc, hf=hf: e.dma_start(
                    out=kTsb[:, kc, hf:hf + 2048], in_=g.kT_d[kc, :, hf:hf + 2048]),
                    w=[("kTsb", kc, hf // 2048)], chan="b_k")
        for cb in range(0, nblk_max, 16):
            P.add("sync", lambda e, cb=cb: e.dma_start(
                out=vsb[:, cb:cb + 16, :],
                in_=g.v_d[cb * 128:(cb + 16) * 128, 0:512].rearrange("(c p) n -> p c n", p=128)),
                w=[("vsb", cb // 16)], chan="b_v")

        for gi, gq in enumerate(groups):
            q_t = qsb[gi % 2]
            qk = ("qsb", gi % 2)
            P.add("sync", lambda e, q_t=q_t, gq=gq: e.dma_start(
                out=q_t[:], in_=g.qT_d[:, 0:4, gq * 512:(gq + 1) * 512]),
                w=[qk], chan=f"b_q{gi % 2}")
            nblk = 16 * (gq + 1)
            for hq in range(2):
                heads = [4 * hq + i for i in range(4)]
                for i in range(4):
                    for par in range(2):
                        P.add("gpsimd", lambda e, i=i, par=par: e.memset(srun[i][par][:], 0.0), w=[("srun", i, par)])
                    P.add("tensor", lambda e, i=i: e.matmul(
                        po[i][:], lhsT=zer[:], rhs=sbmask[:, 0, :], start=True, stop=True),
                        r=["zer", "sbmask"], w=[("po", i)])
                cs = list(range(nblk - 1, -1, -1))

                def prm(step):
                    c = cs[step]
                    rel_c = c - 16 * gq
                    q0 = 128 * (rel_c // 4) if rel_c >= 4 else 0
                    return c, rel_c, (rel_c >= 3), step % 2, q0

                def s1(step, i):
                    c, rel_c, need_mask, par, q0 = prm(step)
                    h = heads[i]
                    hc, half = h // 2, h % 2
                    p0 = 64 * half
                    P.add("tensor", lambda e, i=i, hc=hc, p0=p0, c=c, q_t=q_t, nm=need_mask, q0=q0: e.matmul(
                        pz[i][:, q0:512], lhsT=kTsb[p0:p0 + 64, hc, c * 128:(c + 1) * 128],
                        rhs=q_t[p0:p0 + 64, hc, q0:512], start=True, stop=(not nm)),
                        r=[("kTsb", hc, c // 16), qk], w=[("pz", i)])
                    if need_mask:
                        P.add("tensor", lambda e, i=i, rel_c=rel_c, q0=q0: e.matmul(
                            pz[i][:, q0:512], lhsT=ident[:], rhs=sbmask[:, rel_c, q0:512], start=False, stop=True),
                            r=["ident", "sbmask"], w=[("pz", i)])

                for i in range(4):
                    s1(0, i)
                for step in range(nblk):
                    c, rel_c, need_mask, par, q0 = prm(step)
                    for i, h in enumerate(heads):
                        P.add("scalar", lambda e, i=i, q0=q0: e.activation(
                            out=e_sb[i][:, q0:512], in_=pz[i][:, q0:512], func=AF.Exp),
                            r=[("pz", i)], w=[("e", i)])
                        P.add("scalar", lambda e, i=i, q0=q0: e.activation(
                            out=sp_sb[i][:, q0:512], in_=e_sb[i][:, q0:512], func=AF.Ln, bias=1.0),
                            r=[("e", i)], w=[("sp", i)])
                    for i, h in enumerate(heads):
                        last = (step == 0)
                        P.add("tensor", lambda e, i=i, last=last, q0=q0: e.matmul(
                            pz[i][:, q0:512], lhsT=negU[:], rhs=sp_sb[i][:, q0:512], start=False, stop=last,
                            skip_group_check=True),
                            r=["negU", ("sp", i)], w=[("pz", i)])
                        if step > 0:
                            P.add("tensor", lambda e, i=i, par=par, q0=q0: e.matmul(
                                pz[i][:, q0:512], lhsT=negOnes[:], rhs=srun[i][1 - par][:, q0:512], start=False,
                                stop=True, skip_group_check=True),
                                r=["negOnes", ("srun", i, 1 - par)], w=[("pz", i)])
                    for i, h in enumerate(heads):
                        if step == 0:
                            P.add("vector", lambda e, i=i, par=par, q0=q0: e.tensor_copy(
                                out=srun[i][par][:, q0:512], in_=sp_sb[i][:, q0:512]),
                                r=[("sp", i)], w=[("srun", i, par)])
                        elif c > 0:
                            P.add("vector", lambda e, i=i, par=par, q0=q0: e.tensor_tensor(
                                out=srun[i][par][:, q0:512], in0=srun[i][1 - par][:, q0:512], in1=sp_sb[i][:, q0:512],
                                op=ALU.add),
                                r=[("sp", i), ("srun", i, 1 - par)], w=[("srun", i, par)])
                    for i, h in enumerate(heads):
                        P.add("scalar", lambda e, i=i, q0=q0: e.activation(
                            out=w_sb[i][:, q0:512], in_=pz[i][:, q0:512], func=AF.Exp),
                            r=[("pz", i)], w=[("w", i)])
                    for i, h in enumerate(heads):
                        if step + 1 < nblk:
                            s1(step + 1, i)
                        P.add("tensor", lambda e, i=i, h=h, c=c, q0=q0: e.matmul(
                            po[i][:, q0:512], lhsT=vsb[:, c, h * 64:(h + 1) * 64], rhs=w_sb[i][:, q0:512],
                            start=False, stop=True, skip_group_check=True),
                            r=[("vsb", c // 16), ("w", i)], w=[("po", i)])
                for i, h in enumerate(heads):
                    P.add("vector", lambda e, i=i: e.tensor_copy(out=ost[i][:], in_=po[i][:]),
                          r=[("po", i)], w=[("ost", i)])
                    P.add("sync", lambda e, i=i, h=h, gq=gq: e.dma_start(
                        out=g.osbT_d[h, :, gq * 512:(gq + 1) * 512], in_=ost[i][:]),
                        r=[("ost", i)], w=[("osbT_d", h, gq)], chan=f"b_ost{i}")
        P.emit_phase()


DIL = ((128, 1), (512, 4), (2048, 16))
DL_NB = [w // 128 + 1 for w, _ in DIL]
DL_TOFF = [0, 4 * DL_NB[0], 4 * (DL_NB[0] + DL_NB[1])]
DL_NT = 4 * sum(DL_NB)


def phase_c(P, nc, g, slots=tuple(range(NOWN))):
    st = contextlib.ExitStack()
    with st:
        sb = lambda name, shape, dt: st.enter_context(nc.sbuf_tensor(name, shape, dt))
        ps = lambda name, shape, dt: st.enter_context(nc.psum_tensor(name, shape, dt))
        WB = 17
        kdl = [sb(f"c_k{i}", [128, 6, WB * 128], BF16) for i in range(2)]
        vdl = [sb(f"c_v{i}", [128, WB, 780], BF16) for i in range(2)]
        qdl = [sb(f"c_q{i}", [128, 6, 128], BF16) for i in range(2)]
        dlbias = sb("c_bias", [128, DL_NT, 128], BF16)
        padbias = sb("c_pad", [128, NB], F32)
        ident = sb("c_ident", [128, 128], BF16)
        pT = [sb(f"c_pT{i}", [128, 4, 128], BF16) for i in range(5)]
        rden = sb("c_rden", [128, 4], F32)
        otok = [sb(f"c_otok{i}", [128, 256], BF16) for i in range(2)]
        oT = [sb(f"c_oT{i}", [128, 2, 128], BF16) for i in range(2)]
        pzd = [ps(f"c_pz{i}", [128, 4, 128], F32) for i in range(5)]
        pd = [ps(f"c_pd{i}", [128, 4, 65], F32) for i in range(2)]
        ptr = ps("c_ptr", [128, 2, 128], BF16)

        P.add("sync", lambda e: e.dma_start(out=ident[:], in_=g.ident), w=["ident"], chan="c_c")
        P.add("sync", lambda e: e.dma_start(out=padbias[:], in_=g.padbias), w=["padbias"], chan="c_c")
        for t0 in range(0, DL_NT, 24):
            P.add("sync", lambda e, t0=t0: e.dma_start(out=dlbias[:, t0:t0 + 24, :], in_=g.dlbias[:, t0:t0 + 24, :]),
                  w=[("dlbias", t0 // 24)], chan="c_c")
        ui = 0
        for si, s_ in enumerate(slots):
            cq = 4 * s_ + 3
            c_lo = max(0, cq - 16)
            nwb = cq - c_lo + 1
            b2 = si % 2
            P.add("sync", lambda e, b2=b2, c_lo=c_lo, nwb=nwb: e.dma_start(
                out=kdl[b2][:, :, 0:nwb * 128],
                in_=g.kT_d[4:10, :, c_lo * 128:(c_lo + nwb) * 128].rearrange("c p t -> p c t")),
                w=[("kdl", b2)], chan=f"c_k{b2}")
            P.add("sync", lambda e, b2=b2, c_lo=c_lo, nwb=nwb: e.dma_start(
                out=vdl[b2][:, 0:nwb, :],
                in_=g.v_d[c_lo * 128:(c_lo + nwb) * 128, 512:VW].rearrange("(c p) n -> p c n", p=128)),
                w=[("vdl", b2)], chan=f"c_v{b2}")
            P.add("sync", lambda e, b2=b2, s_=s_: e.dma_start(
                out=qdl[b2][:], in_=g.qT_d[:, 4:10, s_ * 128:(s_ + 1) * 128]),
                w=[("qdl", b2)], chan=f"c_q{b2}")
            pdt = pd[si % 2]
            pdk = ("pd", si % 2)
            units = []
            for hg in range(4):
                uh = []
                for gi in range(3):
                    for o in range(DL_NB[gi]):
                        c = cq - o
                        if c >= 0:
                            uh.append((gi, o, c))
                for k_, (gi, o, c) in enumerate(uh):
                    units.append((hg, k_, len(uh), gi, o, c))
            NBATCH = 4
            batches = [units[i:i + NBATCH] for i in range(0, len(units), NBATCH)]

            def s1(bt, zi):
                for j, u in enumerate(bt):
                    hg, k_, n, gi, o, c = u
                    hd = 4 * gi + hg
                    chn, half = hd // 2, hd % 2
                    p0 = 64 * half
                    wb = c - c_lo
                    tix = DL_TOFF[gi] + hg * DL_NB[gi] + o
                    P.add("tensor", lambda e, zi=zi, j=j, b2=b2, chn=chn, p0=p0, wb=wb: e.matmul(
                        pzd[zi][:, j, :], lhsT=kdl[b2][p0:p0 + 64, chn, wb * 128:(wb + 1) * 128],
                        rhs=qdl[b2][p0:p0 + 64, chn, :], start=True, stop=False),
                        r=[("kdl", b2), ("qdl", b2)], w=[("pzd", zi)])
                    P.add("tensor", lambda e, zi=zi, j=j, tix=tix: e.matmul(
                        pzd[zi][:, j, :], lhsT=ident[:], rhs=dlbias[:, tix, :], start=False, stop=True),
                        r=["ident", ("dlbias", tix // 24)], w=[("pzd", zi)])
                nb_ = len(bt)
                P.add("scalar", lambda e, zi=zi, nb_=nb_: e.activation(
                    out=pT[zi][:, 0:nb_, :], in_=pzd[zi][:, 0:nb_, :], func=AF.Exp),
                    r=[("pzd", zi)], w=[("pT", zi)])

            def s2(bt, zi):
                for j, u in enumerate(bt):
                    hg, k_, n, gi, o, c = u
                    hd = 4 * gi + hg
                    wb = c - c_lo
                    P.add("tensor", lambda e, zi=zi, j=j, b2=b2, wb=wb, hd=hd, hg=hg, k_=k_, n=n, pdt=pdt: e.matmul(
                        pdt[:, hg, :], lhsT=pT[zi][:, j, :], rhs=vdl[b2][:, wb, hd * 65:(hd + 1) * 65],
                        start=(k_ == 0), stop=(k_ == n - 1)),
                        r=[("pT", zi), ("vdl", b2)], w=[pdk])

            LOOK = 4
            zis = []
            for i in range(len(batches) + LOOK):
                if i < len(batches):
                    zis.append(ui % 5)
                    ui += 1
                    s1(batches[i], zis[i])
                if i - LOOK >= 0:
                    s2(batches[i - LOOK], zis[i - LOOK])
            P.add("vector", lambda e, pdt=pdt: e.reciprocal(out=rden[:], in_=pdt[:, :, 64]),
                  r=[pdk], w=["rden"])
            ot = otok[si % 2]
            for hg in range(4):
                P.add("vector", lambda e, pdt=pdt, hg=hg, ot=ot: e.tensor_scalar(
                    out=ot[:, hg * 64:(hg + 1) * 64], in0=pdt[:, hg, 0:64], scalar1=rden[:, hg:hg + 1],
                    scalar2=None, op0=ALU.mult),
                    r=[pdk, "rden"], w=[("otok", si % 2)])
            for cc in range(2):
                P.add("tensor", lambda e, cc=cc, ot=ot: e.transpose(
                    out=ptr[:, cc, :], in_=ot[:, cc * 128:(cc + 1) * 128], identity=ident[:]),
                    r=[("otok", si % 2), "ident"], w=["ptr"])
            P.add("vector", lambda e, si=si: e.tensor_copy(out=oT[si % 2][:], in_=ptr[:]),
                  r=["ptr"], w=[("oT", si % 2)])
            P.add("gpsimd", lambda e, si=si, s_=s_: e.dma_start(
                out=g.odlT_d[:, :, s_ * 128:(s_ + 1) * 128].rearrange("c p t -> p c t"), in_=oT[si % 2][:]),
                r=[("oT", si % 2)], w=[("odlT_d", s_)], chan=f"c_oT{si % 2}")
        P.emit_phase()


def _bounds_reg(P, g):
    def fn(e):
        if not g.bcreg:
            g.bcreg.append(e.alloc_register("bc"))
        return e.reg_mov(g.bcreg[0], NEXP * CAP - 1)
    P.add("gpsimd", fn)


def phase_d(P, nc, g, qgroups=(0, 1, 2, 3), d_stop=9):
    st = contextlib.ExitStack()
    with st:
        sb = lambda name, shape, dt: st.enter_context(nc.sbuf_tensor(name, shape, dt))
        ps = lambda name, shape, dt: st.enter_context(nc.psum_tensor(name, shape, dt))
        wg = sb("d_wg", [128, 8, 2048], BF16)
        wbs = sb("d_wbs", [64, 8, 1024], BF16)
        wbd = sb("d_wbd", [128, 2, 1024], BF16)
        wo = sb("d_wo", [128, 8, 1024], BF16)
        wr = sb("d_wr", [128, 8, 256], F32)
        bgT = sb("d_bgT", [128, 16], F32)
        g1rep = sb("d_g1", [128, 1024], F32)
        b1rep = sb("d_b1", [128, 1024], F32)
        rbrep = sb("d_rb", [128, 256], F32)
        iota = sb("d_iota", [128, 256], F32)
        identb = sb("d_identb", [128, 128], BF16)
        wr_hi = sb("d_wr_hi", [128, 8, 256], BF16)
        wr_lo = sb("d_wr_lo", [128, 8, 256], BF16)
        h1lo = sb("d_h1lo", [128, 1024], BF16)
        h1Tlo = sb("d_h1Tlo", [128, 8, 128], BF16)
        lstrict = sb("d_lstrict", [128, 128], BF16)
        ones = sb("d_ones", [128, 128], BF16)
        hTo = sb("d_hTo", [128, 8, 512], BF16)
        osb = sb("d_osb", [64, 8, 512], BF16)
        odl = sb("d_odl", [128, 2, 512], BF16)
        gs = sb("d_gs", [128, 512], F32)
        gd = sb("d_gd", [128, 512], F32)
        m1 = sb("d_m1", [128, 512], F32)
        m2 = sb("d_m2", [128, 512], F32)
        mT = sb("d_mT", [128, 8, 512], BF16)
        hown = [sb(f"d_hown{i}", [128, 1024], F32) for i in range(2)]
        rr = sb("d_r", [128, 1024], F32)
        h1 = [sb(f"d_h1_{i}", [128, 1024], F32) for i in range(2)]
        h1b = [sb(f"d_h1b{i}", [128, 1024], BF16) for i in range(2)]
        h1Tb = [sb(f"d_h1Tb{i}", [128, 8, 128], BF16) for i in range(2)]
        stats = sb("d_stats", [128, 2, 6], F32)
        mv = sb("d_mv", [128, 2], F32)
        std = sb("d_std", [128, 1], F32)
        rstd = sb("d_rstd", [128, 1], F32)
        sc = [sb(f"d_sc{i}", [128, 256], F32) for i in range(2)]
        biased = [sb(f"d_biased{i}", [128, 256], F32) for i in range(2)]
        masked = sb("d_masked", [128, 256], F32)
        junk = sb("d_junk", [128, 256], F32)
        selb = sb("d_selb", [128, NOWN, 256], BF16)
        m8g = sb("d_m8g", [128, 8, 8], F32)
        gscore = sb("d_gscore", [128, 8], F32)
        gm8 = sb("d_gm8", [128, 8], F32)
        pen = sb("d_pen", [128, 8], F32)
        t8 = sb("d_t8", [128, 8], F32)
        wk = sb("d_wk", [128, 8], F32)
        rk = sb("d_rk", [128, 8], F32)
        ik = sb("d_ik", [128, 8], F32)
        wsum = sb("d_wsum", [128, 1], F32)
        sif = sb("d_sif", [128, 8], F32)
        ovf = sb("d_ovf", [128, 8], F32)
        pA = ps("d_pA", [128, 512], F32)
        pB = ps("d_pB", [128, 512], F32)
        pC = ps("d_pC", [128, 512], F32)
        pD = ps("d_pD", [128, 512], F32)
        pmix = ps("d_pmix", [128, 1024], F32)
        ptr = ps("d_ptr", [128, 8, 128], BF16)
        ptr_lo = ps("d_ptr_lo", [128, 8, 128], BF16)

        _bounds_reg(P, g)
        for dc in range(8):
            P.add("gpsimd", lambda e, dc=dc: e.dma_start(
                out=wg[:, dc, :], in_=g.w_in[dc * 128:(dc + 1) * 128, 3840:5888]), w=[("wg", dc)], chan="d_w")
        P.add("gpsimd", lambda e: e.dma_start(
            out=wbs[:], in_=g.w_br_sb.rearrange("(h p) n -> p h n", p=64)), w=["wbs"], chan="d_w")
        P.add("gpsimd", lambda e: e.dma_start(
            out=wbd[:], in_=g.w_br_dil.rearrange("(c p) n -> p c n", p=128)), w=["wbd"], chan="d_w")
        for dc in range(8):
            P.add("gpsimd", lambda e, dc=dc: e.dma_start(
                out=wo[:, dc, :], in_=g.w_out[dc * 128:(dc + 1) * 128, :]), w=[("wo", dc)], chan="d_w")
        P.add("sync", lambda e: e.dma_start(out=wr[:], in_=g.w_router.rearrange("(c p) n -> p c n", p=128)),
              w=["wr"], chan="d_wr")
        P.add("sync", lambda e: e.dma_start(out=bgT[:], in_=g.b_gateT), w=["bgT"], chan="d_c")
        P.add("sync", lambda e: e.dma_start(out=g1rep[:], in_=g.ln1_g.partition_broadcast(128)), w=["g1rep"], chan="d_c")
        P.add("sync", lambda e: e.dma_start(out=b1rep[:], in_=g.ln1_b.partition_broadcast(128)), w=["b1rep"], chan="d_c")
        P.add("sync", lambda e: e.dma_start(out=rbrep[:], in_=g.router_bias.partition_broadcast(128)), w=["rbrep"], chan="d_c")
        P.add("sync", lambda e: e.dma_start(out=iota[:], in_=g.iota256), w=["iota"], chan="d_c")
        P.add("sync", lambda e: e.dma_start(out=identb[:], in_=g.ident), w=["identb"], chan="d_c")
        P.add("gpsimd", lambda e: e.dma_start(out=wr_hi[:], in_=g.w_router.rearrange("(c p) n -> p c n", p=128)),
              w=["wr_hi"], chan="d_wrh")
        P.add("vector", lambda e: e.tensor_tensor(out=wr_lo[:], in0=wr[:], in1=wr_hi[:], op=ALU.subtract),
              r=["wr", "wr_hi"], w=["wr_lo"])
        P.add("sync", lambda e: e.dma_start(out=lstrict[:], in_=g.lstrict), w=["lstrict"], chan="d_c")
        P.add("sync", lambda e: e.dma_start(out=ones[:], in_=g.ones), w=["ones"], chan="d_c")


        pend = [None]
        allk = lambda n: [(n, k) for k in range(8)]

        def xgen(bi, blk):
            b2 = blk % 2
            P.add("sync", lambda e, blk=blk, b2=b2: e.dma_start(
                out=hown[b2][:], in_=g.h_own_d[blk * 128:(blk + 1) * 128, :]), w=[("hown", b2)], chan=f"d_hown{b2}")
            for nh in range(2):
                for oc in range(8):
                    P.add("tensor", lambda e, bi=bi, nh=nh, oc=oc: e.matmul(
                        pmix[:, nh * 512:(nh + 1) * 512], lhsT=mT[:, oc, bi * 128:(bi + 1) * 128],
                        rhs=wo[:, oc, nh * 512:(nh + 1) * 512], start=(oc == 0), stop=(oc == 7)),
                        r=[("mT", oc), ("wo", oc)], w=[("pmix", nh)])
            yield
            for nh in range(2):
                P.add("vector", lambda e, b2=b2, nh=nh: e.scalar_tensor_tensor(
                    out=rr[:, nh * 512:(nh + 1) * 512], in0=hown[b2][:, nh * 512:(nh + 1) * 512], scalar=ALPHA,
                    in1=pmix[:, nh * 512:(nh + 1) * 512], op0=ALU.mult, op1=ALU.add),
                    r=[("hown", b2), ("pmix", nh)], w=[("rr", nh)])
            for hh in range(2):
                P.add("vector", lambda e, hh=hh: e.bn_stats(out=stats[:, hh, :], in_=rr[:, hh * 512:(hh + 1) * 512]),
                      r=[("rr", hh)], w=[("dstats", hh)])
            P.add("vector", lambda e: e.bn_aggr(out=mv[:], in_=stats[:].rearrange("p a b -> p (a b)")),
                  r=[("dstats", 0), ("dstats", 1)], w=["dmv"])
            P.add("scalar", lambda e: e.activation(out=std[:], in_=mv[:, 1:2], func=AF.Sqrt, bias=LN_EPS),
                  r=["dmv"], w=["dstd"])
            yield
            P.add("vector", lambda e: e.reciprocal(out=rstd[:], in_=std[:]), r=["dstd"], w=["drstd"])
            P.add("vector", lambda e, b2=b2: e.tensor_scalar(
                out=h1[b2][:], in0=rr[:], scalar1=mv[:, 0:1], scalar2=rstd[:, 0:1], op0=ALU.subtract, op1=ALU.mult),
                r=[("rr", 0), ("rr", 1), "dmv", "drstd"], w=[("h1", b2)])
            P.add("gpsimd", lambda e, b2=b2: e.tensor_tensor(out=h1[b2][:], in0=h1[b2][:], in1=g1rep[:], op=ALU.mult),
                  r=[("h1", b2), "g1rep"], w=[("h1", b2)])
            P.add("gpsimd", lambda e, b2=b2: e.tensor_tensor(out=h1[b2][:], in0=h1[b2][:], in1=b1rep[:], op=ALU.add),
                  r=[("h1", b2), "b1rep"], w=[("h1", b2)])
            P.add("scalar", lambda e, blk=blk, b2=b2: e.dma_start(
                out=g.h1_d[blk * 128:(blk + 1) * 128, :], in_=h1[b2][:]), r=[("h1", b2)], w=[("h1_d", blk)],
                chan=f"d_h1{b2}")
            P.add("scalar", lambda e, b2=b2: e.activation(out=h1b[b2][:], in_=h1[b2][:], func=AF.Copy),
                  r=[("h1", b2)], w=[("h1b", b2)])
            yield
            P.add("vector", lambda e, b2=b2: e.tensor_tensor(out=h1lo[:], in0=h1[b2][:], in1=h1b[b2][:], op=ALU.subtract),
                  r=[("h1", b2), ("h1b", b2)], w=["h1lo"])
            for dc in range(8):
                P.add("tensor", lambda e, b2=b2, dc=dc: e.transpose(
                    out=ptr[:, dc, :], in_=h1b[b2][:, dc * 128:(dc + 1) * 128], identity=identb[:]),
                    r=[("h1b", b2), "identb"], w=["ptr"])
            for dc in range(8):
                P.add("tensor", lambda e, dc=dc: e.transpose(
                    out=ptr_lo[:, dc, :], in_=h1lo[:, dc * 128:(dc + 1) * 128], identity=identb[:]),
                    r=["h1lo", "identb"], w=["ptr_lo"])
            P.add("scalar", lambda e, b2=b2: e.activation(out=h1Tb[b2][:], in_=ptr[:], func=AF.Copy),
                  r=["ptr"], w=[("h1Tb", b2)])
            P.add("scalar", lambda e, blk=blk, b2=b2: e.dma_start(
                out=g.h1T_d[:, :, blk * 128:(blk + 1) * 128], in_=h1Tb[b2][:]), r=[("h1Tb", b2)],
                w=[("h1T_d", blk)], chan=f"d_h1T{b2}")
            yield
            P.add("vector", lambda e: e.tensor_copy(out=h1Tlo[:], in_=ptr_lo[:]), r=["ptr_lo"], w=["h1Tlo"])
            combos = [(h1Tb[b2], ("h1Tb", b2), wr_hi, "wr_hi"), (h1Tb[b2], ("h1Tb", b2), wr_lo, "wr_lo"),
                      (h1Tlo, "h1Tlo", wr_hi, "wr_hi")]
            for ci, (lt, ltk, rt, rtk) in enumerate(combos):
                for dc in range(8):
                    P.add("tensor", lambda e, dc=dc, lt=lt, rt=rt, ci=ci: e.matmul(
                        pA[:, 0:256], lhsT=lt[:, dc, :], rhs=rt[:, dc, :], start=(ci == 0 and dc == 0),
                        stop=(ci == 2 and dc == 7)), r=[ltk, rtk], w=["pA"])
            P.add("scalar", lambda e, b2=b2: e.activation(out=sc[b2][:], in_=pA[:, 0:256], func=AF.Sigmoid),
                  r=["pA"], w=[("sc", b2)])
            yield
            P.add("vector", lambda e, b2=b2: e.tensor_tensor(out=biased[b2][:], in0=sc[b2][:], in1=rbrep[:], op=ALU.add),
                  r=[("sc", b2), "rbrep"], w=[("biased", b2)])

        def ygen(blk):
            b2 = blk % 2
            bia = biased[b2]
            bk = ("biased", b2)
            sct = sc[b2]
            sck = ("sc", b2)
            for gr in range(8):
                P.add("vector", lambda e, gr=gr: e.max(out=m8g[:, gr, :], in_=bia[:, gr * 32:(gr + 1) * 32]),
                      r=[bk], w=[("m8g", gr)])
            P.add("vector", lambda e: e.tensor_tensor(out=gscore[:], in0=m8g[:, :, 0], in1=m8g[:, :, 1], op=ALU.add),
                  r=[("m8g", gr) for gr in range(8)], w=["gscore"])
            P.add("vector", lambda e: e.max(out=gm8[:], in_=gscore[:]), r=["gscore"], w=["gm8"])
            P.add("vector", lambda e: e.tensor_scalar(
                out=pen[:], in0=gscore[:], scalar1=gm8[:, 3:4], scalar2=1.0e4, op0=ALU.is_lt, op1=ALU.mult),
                r=["gscore", "gm8"], w=["pen"])
            P.add("vector", lambda e: e.tensor_tensor(
                out=masked[:].rearrange("p (a b) -> p a b", b=32), in0=bia[:].rearrange("p (a b) -> p a b", b=32),
                in1=pen[:].unsqueeze(2).to_broadcast([128, 8, 32]), op=ALU.subtract),
                r=[bk, "pen"], w=["masked"])
            P.add("vector", lambda e: e.max(out=t8[:], in_=masked[:]), r=["masked"], w=["t8"])
            P.add("vector", lambda e, blk=blk: e.tensor_scalar(
                out=selb[:, blk, :], in0=masked[:], scalar1=t8[:, 7:8], scalar2=None, op0=ALU.is_ge),
                r=["masked", "t8"], w=[("selb", blk)])
            P.add("tensor", lambda e, blk=blk: e.matmul(
                pB[:, 0:256], lhsT=lstrict[:], rhs=selb[:, blk, :], start=True, stop=(blk == 0)),
                r=["lstrict", ("selb", blk)], w=["pB"])
            for pb_ in range(blk):
                P.add("tensor", lambda e, pb_=pb_, blk=blk: e.matmul(
                    pB[:, 0:256], lhsT=ones[:], rhs=selb[:, pb_, :], start=False, stop=(pb_ == blk - 1)),
                    r=["ones", ("selb", pb_)], w=["pB"])
            yield
            for k in range(8):
                P.add("vector", lambda e, k=k: e.scalar_tensor_tensor(
                    out=junk[:], in0=masked[:], scalar=t8[:, k:k + 1], in1=sct[:], op0=ALU.is_equal, op1=ALU.mult,
                    accum_out=wk[:, k:k + 1]), r=["masked", "t8", sck], w=["junk", ("wk", k)])
                P.add("vector", lambda e, k=k: e.scalar_tensor_tensor(
                    out=junk[:], in0=masked[:], scalar=t8[:, k:k + 1], in1=iota[:], op0=ALU.is_equal,
                    op1=ALU.mult, accum_out=ik[:, k:k + 1]), r=["masked", "t8", "iota"], w=["junk", ("ik", k)])
                if k % 3 == 2:
                    yield
            yield
            for k in range(8):
                P.add("vector", lambda e, k=k: e.scalar_tensor_tensor(
                    out=junk[:], in0=masked[:], scalar=t8[:, k:k + 1], in1=pB[:, 0:256], op0=ALU.is_equal,
                    op1=ALU.mult, accum_out=rk[:, k:k + 1]), r=["masked", "t8", "pB"], w=["junk", ("rk", k)])
            P.add("vector", lambda e: e.tensor_reduce(out=wsum[:], in_=wk[:], axis=AX.X, op=ALU.add),
                  r=allk("wk"), w=["wsum"])
            P.add("vector", lambda e: e.reciprocal(out=wsum[:], in_=wsum[:]), r=["wsum"], w=["wsum"])
            P.add("vector", lambda e, blk=blk: e.tensor_scalar(
                out=g.gk[:, blk, :], in0=wk[:], scalar1=wsum[:, 0:1], scalar2=2.5, op0=ALU.mult, op1=ALU.mult),
                r=allk("wk") + ["wsum"], w=[("gk", blk)])
            P.add("vector", lambda e: e.tensor_scalar(
                out=ovf[:], in0=rk[:], scalar1=float(CAP), scalar2=1.0e6, op0=ALU.is_ge, op1=ALU.mult),
                r=allk("rk"), w=["ovf"])
            P.add("vector", lambda e: e.scalar_tensor_tensor(
                out=sif[:], in0=ik[:], scalar=float(CAP), in1=rk[:], op0=ALU.mult, op1=ALU.add),
                r=allk("ik") + allk("rk"), w=["sif"])
            P.add("vector", lambda e: e.tensor_tensor(out=sif[:], in0=sif[:], in1=ovf[:], op=ALU.add),
                  r=["sif", "ovf"], w=["sif"])
            P.add("vector", lambda e, blk=blk: e.tensor_copy(out=g.sidx[:, blk, :], in_=sif[:]),
                  r=["sif"], w=[("sidx", blk)])
            for k in range(8):
                P.add("gpsimd", lambda e, blk=blk, k=k, b2=b2: e.indirect_dma_start(
                    out=g.xs_d[:, :], out_offset=bass.IndirectOffsetOnAxis(ap=g.sidx[:, blk, k:k + 1], axis=0),
                    in_=h1b[b2][:, :], in_offset=None, bounds_check=g.bcreg[0], oob_is_err=False),
                    r=[("h1b", b2), ("sidx", blk)], w=[("xs_d", blk, k)], chan=f"d_sc{b2}")

        for gq in qgroups:
            t0 = gq * 512
            P.add("sync", lambda e, t0=t0: e.dma_start(out=hTo[:], in_=g.hT_own_d[:, :, t0:t0 + 512]),
                  w=["hTo"], chan="d_hTo")
            P.add("sync", lambda e, t0=t0: e.dma_start(
                out=osb[:], in_=g.osbT_d[:, :, t0:t0 + 512].rearrange("h p t -> p h t")), w=["osb"], chan="d_osb")
            P.add("sync", lambda e, t0=t0: e.dma_start(
                out=odl[:], in_=g.odlT_d[:, :, t0:t0 + 512].rearrange("c p t -> p c t")), w=["odl"], chan="d_odl")
            for oc in range(8):
                for dc in range(8):
                    P.add("tensor", lambda e, oc=oc, dc=dc: e.matmul(
                        pA[:], lhsT=wg[:, dc, oc * 128:(oc + 1) * 128], rhs=hTo[:, dc, :],
                        start=(dc == 0), stop=(dc == 7)), r=[("wg", dc), "hTo"], w=["pA"])
                for dc in range(8):
                    P.add("tensor", lambda e, oc=oc, dc=dc: e.matmul(
                        pB[:], lhsT=wg[:, dc, 1024 + oc * 128:1024 + (oc + 1) * 128], rhs=hTo[:, dc, :],
                        start=(dc == 0), stop=(dc == 7)), r=[("wg", dc), "hTo"], w=["pB"])
                for h in range(8):
                    P.add("tensor", lambda e, oc=oc, h=h: e.matmul(
                        pC[:], lhsT=wbs[:, h, oc * 128:(oc + 1) * 128], rhs=osb[:, h, :],
                        start=(h == 0), stop=(h == 7)), r=["wbs", "osb"], w=["pC"])
                for c2 in range(2):
                    P.add("tensor", lambda e, oc=oc, c2=c2: e.matmul(
                        pD[:], lhsT=wbd[:, c2, oc * 128:(oc + 1) * 128], rhs=odl[:, c2, :],
                        start=(c2 == 0), stop=(c2 == 1)), r=["wbd", "odl"], w=["pD"])
                P.add("scalar", lambda e, oc=oc: e.activation(
                    out=gs[:], in_=pA[:], func=AF.Sigmoid, bias=bgT[:, oc:oc + 1]), r=["pA", "bgT"], w=["gs"])
                P.add("scalar", lambda e, oc=oc: e.activation(
                    out=gd[:], in_=pB[:], func=AF.Sigmoid, bias=bgT[:, 8 + oc:9 + oc]), r=["pB", "bgT"], w=["gd"])
                P.add("vector", lambda e: e.tensor_tensor(out=m1[:], in0=gs[:], in1=pC[:], op=ALU.mult),
                      r=["gs", "pC"], w=["m1"])
                P.add("vector", lambda e: e.tensor_tensor(out=m2[:], in0=gd[:], in1=pD[:], op=ALU.mult),
                      r=["gd", "pD"], w=["m2"])
                P.add("gpsimd", lambda e, oc=oc: e.tensor_tensor(out=mT[:, oc, :], in0=m1[:], in1=m2[:], op=ALU.add),
                      r=["m1", "m2"], w=[("mT", oc)])
            for bi in range(4):
                blk = gq * 4 + bi
                xg = xgen(bi, blk)
                yg = pend[0]
                xa, ya = True, yg is not None
                while xa or ya:
                    if xa:
                        try:
                            next(xg)
                        except StopIteration:
                            xa = False
                    if ya:
                        try:
                            next(yg)
                        except StopIteration:
                            ya = False
                pend[0] = ygen(blk)
        if pend[0] is not None:
            for _ in pend[0]:
                pass
        P.emit_phase()


def _layer_norm(P, x, xk0, xk1, stats, mv, std, rstd, out, outk, grep, gk_, brep, bk_, tag):
    for hh, xk in ((0, xk0), (1, xk1)):
        P.add("vector", lambda e, hh=hh: e.bn_stats(out=stats[:, hh, :], in_=x[:, hh * 512:(hh + 1) * 512]),
              r=[xk], w=[(tag + "stats", hh)])
    P.add("vector", lambda e: e.bn_aggr(out=mv[:], in_=stats[:].rearrange("p a b -> p (a b)")),
          r=[(tag + "stats", 0), (tag + "stats", 1)], w=[tag + "mv"])
    P.add("scalar", lambda e: e.activation(out=std[:], in_=mv[:, 1:2], func=AF.Sqrt, bias=LN_EPS),
          r=[tag + "mv"], w=[tag + "std"])
    P.add("vector", lambda e: e.reciprocal(out=rstd[:], in_=std[:]), r=[tag + "std"], w=[tag + "rstd"])
    P.add("vector", lambda e: e.tensor_scalar(
        out=out[:], in0=x[:], scalar1=mv[:, 0:1], scalar2=rstd[:, 0:1], op0=ALU.subtract, op1=ALU.mult),
        r=[xk0, xk1, tag + "mv", tag + "rstd"], w=[outk])
    P.add("gpsimd", lambda e: e.tensor_tensor(out=out[:], in0=out[:], in1=grep[:], op=ALU.mult),
          r=[outk, gk_], w=[outk])
    P.add("gpsimd", lambda e: e.tensor_tensor(out=out[:], in0=out[:], in1=brep[:], op=ALU.add),
          r=[outk, bk_], w=[outk])


def _swiglu_block(P, xT, xTk, wg_t, wu_t, wd_t, wkeys, pgu, pguk, sg, sgk, hid, hidk, py, pyk):
    for j, wt in enumerate((wg_t, wu_t)):
        for hh in range(2):
            for dc in range(8):
                P.add("tensor", lambda e, j=j, wt=wt, hh=hh, dc=dc: e.matmul(
                    pgu[:, 2 * j + hh, :], lhsT=wt[:, dc, hh * 128:(hh + 1) * 128], rhs=xT[:, dc, :],
                    start=(dc == 0), stop=(dc == 7)), r=[wkeys[j], xTk], w=[pguk])
    P.add("scalar", lambda e: e.activation(out=sg[:], in_=pgu[:, 0:2, :], func=AF.Silu), r=[pguk], w=[sgk])
    P.add("vector", lambda e: e.tensor_tensor(out=hid[:], in0=sg[:], in1=pgu[:, 2:4, :], op=ALU.mult),
          r=[sgk, pguk], w=[hidk])
    for nh in range(2):
        for hh in range(2):
            P.add("tensor", lambda e, nh=nh, hh=hh: e.matmul(
                py[:, nh * 512:(nh + 1) * 512], lhsT=hid[:, hh, :], rhs=wd_t[:, hh, nh * 512:(nh + 1) * 512],
                start=(hh == 0), stop=(hh == 1)), r=[hidk, wkeys[2]], w=[pyk + (nh,)])


def phase_f(P, nc, g, experts=tuple(range(NEXP))):
    st = contextlib.ExitStack()
    with st:
        sb = lambda name, shape, dt: st.enter_context(nc.sbuf_tensor(name, shape, dt))
        ps = lambda name, shape, dt: st.enter_context(nc.psum_tensor(name, shape, dt))
        NS = 3
        ident = sb("f_ident", [128, 128], BF16)
        xs = [sb(f"f_xs{i}", [128, 2, 1024], BF16) for i in range(3)]
        wg32 = [sb(f"f_wg32_{i}", [128, 8, 256], F32) for i in range(NS)]
        wu32 = [sb(f"f_wu32_{i}", [128, 8, 256], F32) for i in range(NS)]
        wd32 = [sb(f"f_wd32_{i}", [128, 2, 1024], F32) for i in range(NS)]
        wg_ = [sb(f"f_wg{i}", [128, 8, 256], BF16) for i in range(2)]
        wu_ = [sb(f"f_wu{i}", [128, 8, 256], BF16) for i in range(2)]
        wd_ = [sb(f"f_wd{i}", [128, 2, 1024], BF16) for i in range(2)]
        xT = [sb(f"f_xT{i}", [128, 8, 128], BF16) for i in range(3)]
        sg = [sb(f"f_sg{i}", [128, 2, 128], F32) for i in range(2)]
        hid = [sb(f"f_hid{i}", [128, 2, 128], BF16) for i in range(2)]
        ys = [sb(f"f_ys{i}", [128, 2, 1024], BF16) for i in range(2)]
        ptr = [ps(f"f_ptr{i}", [128, 8, 128], BF16) for i in range(2)]
        pgu = [ps(f"f_pgu{i}", [128, 4, 128], F32) for i in range(2)]
        py = [ps(f"f_py{i}", [128, 1024], F32) for i in range(2)]
        P.add("sync", lambda e: e.dma_start(out=ident[:], in_=g.ident), w=["ident"], chan="f_c")
        ne = len(experts)

        def load(n_):
            ex = experts[n_]
            b3 = n_ % NS
            P.add("sync", lambda e, ex=ex, b3=b3: e.dma_start(
                out=wg32[b3][:], in_=g.w_gate_e[ex].rearrange("(p c) n -> p c n", p=128)), w=[("wg32", b3)],
                chan=f"f_wg{b3}")
            P.add("scalar", lambda e, ex=ex, b3=b3: e.dma_start(
                out=wu32[b3][:], in_=g.w_up_e[ex].rearrange("(p c) n -> p c n", p=128)), w=[("wu32", b3)],
                chan=f"f_wu{b3}")
            P.add("sync", lambda e, ex=ex, b3=b3: e.dma_start(
                out=wd32[b3][:], in_=g.w_down_e[ex].rearrange("(p c) n -> p c n", p=128)), w=[("wd32", b3)],
                chan=f"f_wd{b3}")

        def cast(n_):
            b3 = n_ % NS
            b2 = n_ % 2
            P.add("scalar", lambda e, b3=b3, b2=b2: e.activation(out=wg_[b2][:], in_=wg32[b3][:], func=AF.Copy),
                  r=[("wg32", b3)], w=[("wg", b2)])
            P.add("vector", lambda e, b3=b3, b2=b2: e.tensor_copy(out=wu_[b2][:], in_=wu32[b3][:]),
                  r=[("wu32", b3)], w=[("wu", b2)])
            P.add("scalar", lambda e, b3=b3, b2=b2: e.activation(out=wd_[b2][:, 0, :], in_=wd32[b3][:, 0, :], func=AF.Copy),
                  r=[("wd32", b3)], w=[("wd", b2, 0)])
            P.add("vector", lambda e, b3=b3, b2=b2: e.tensor_copy(out=wd_[b2][:, 1, :], in_=wd32[b3][:, 1, :]),
                  r=[("wd32", b3)], w=[("wd", b2, 1)])

        NH = CAP // 128
        items = [(n_, half) for n_ in range(ne) for half in range(NH)]

        def st_lx(n_):
            x3 = n_ % 3
            r0 = experts[n_] * CAP
            P.add("sync", lambda e, r0=r0, x3=x3: e.dma_start(
                out=xs[x3][:], in_=g.xs_d[r0:r0 + CAP, :].rearrange("(p h) d -> p h d", h=2)),
                w=[("xs", x3)], chan=f"f_xs{x3}")

        def st_t(i):
            n_, half = items[i]
            x3 = i % 3
            xe = n_ % 3
            b2 = i % 2
            for dc in range(8):
                P.add("tensor", lambda e, dc=dc, b2=b2, xe=xe, half=half: e.transpose(
                    out=ptr[b2][:, dc, :], in_=xs[xe][:, half, :].rearrange("t (p c) -> t c p", c=8)[:, dc, :],
                    identity=ident[:]),
                    r=[("xs", xe), "ident"], w=[("ptr", b2)])
            P.add("vector", lambda e, b2=b2, x3=x3: e.tensor_copy(out=xT[x3][:], in_=ptr[b2][:]),
                  r=[("ptr", b2)], w=[("xT", x3)])

        def st_gu(i):
            n_, half = items[i]
            x3 = i % 3
            b2 = i % 2
            wb = n_ % 2
            for j, wt in enumerate((wg_[wb], wu_[wb])):
                wk_ = ("wg", wb) if j == 0 else ("wu", wb)
                for hh in range(2):
                    for dc in range(8):
                        P.add("tensor", lambda e, j=j, wt=wt, hh=hh, dc=dc, b2=b2, x3=x3: e.matmul(
                            pgu[b2][:, 2 * j + hh, :], lhsT=wt[:, dc, :].rearrange("p (m h) -> p h m", h=2)[:, hh, :],
                            rhs=xT[x3][:, dc, :],
                            start=(dc == 0), stop=(dc == 7)), r=[wk_, ("xT", x3)], w=[("pgu", b2)])
            P.add("scalar", lambda e, b2=b2: e.activation(out=sg[b2][:], in_=pgu[b2][:, 0:2, :], func=AF.Silu),
                  r=[("pgu", b2)], w=[("sg", b2)])
            P.add("vector", lambda e, b2=b2: e.tensor_tensor(out=hid[b2][:], in0=sg[b2][:], in1=pgu[b2][:, 2:4, :],
                                                            op=ALU.mult),
                  r=[("sg", b2), ("pgu", b2)], w=[("hid", b2)])

        def st_dn(i):
            n_, half = items[i]
            b2 = i % 2
            wb = n_ % 2
            r0 = experts[n_] * CAP + half * 128
            for nh in range(2):
                for hh in range(2):
                    P.add("tensor", lambda e, nh=nh, hh=hh, b2=b2, wb=wb: e.matmul(
                        py[b2][:, nh * 512:(nh + 1) * 512], lhsT=hid[b2][:, hh, :],
                        rhs=wd_[wb][:, hh, nh * 512:(nh + 1) * 512], start=(hh == 0), stop=(hh == 1)),
                        r=[("hid", b2), ("wd", wb, hh)], w=[("py", b2, nh)])
            y2 = n_ % 2
            P.add("scalar", lambda e, b2=b2, y2=y2, half=half: e.activation(
                out=ys[y2][:, half, 0:512], in_=py[b2][:, 0:512], func=AF.Copy),
                r=[("py", b2, 0)], w=[("ys", y2, half, 0)])
            P.add("vector", lambda e, b2=b2, y2=y2, half=half: e.tensor_copy(
                out=ys[y2][:, half, 512:1024], in_=py[b2][:, 512:1024]),
                r=[("py", b2, 1)], w=[("ys", y2, half, 1)])
            if half == NH - 1:
                rbase = experts[n_] * CAP
                P.add("gpsimd", lambda e, rbase=rbase, y2=y2: e.dma_start(
                    out=g.ys_d[rbase:rbase + CAP, :].rearrange("(p h) d -> p h d", h=2), in_=ys[y2][:]),
                    r=[("ys", y2, h_, q_) for h_ in range(2) for q_ in range(2)], w=[("ys_d", rbase)],
                    chan=f"f_ys{y2}")

        load(0)
        if ne > 1:
            load(1)
        cast(0)
        ni = len(items)
        st_lx(0)
        if ne > 1:
            st_lx(1)
        for step in range(ni + 2):
            if step < ni:
                n_t, half_t = items[step]
                if half_t == 0 and n_t + 2 < ne:
                    st_lx(n_t + 2)
                st_t(step)
            i1 = step - 1
            if 0 <= i1 < ni:
                n_, half = items[i1]
                if half == 0 and n_ + 2 < ne:
                    load(n_ + 2)
                st_gu(i1)
                if half == NH - 1 and n_ + 1 < ne:
                    cast(n_ + 1)
            i2 = step - 2
            if 0 <= i2 < ni:
                st_dn(i2)
        P.emit_phase()


def phase_g(P, nc, g, blocks=tuple(range(NOWN))):
    st = contextlib.ExitStack()
    with st:
        sb = lambda name, shape, dt: st.enter_context(nc.sbuf_tensor(name, shape, dt))
        ps = lambda name, shape, dt: st.enter_context(nc.psum_tensor(name, shape, dt))
        wgs = sb("g_wgs", [128, 8, 256], BF16)
        wus = sb("g_wus", [128, 8, 256], BF16)
        wds = sb("g_wds", [128, 2, 1024], BF16)
        g2rep = sb("g_g2", [128, 1024], F32)
        b2rep = sb("g_b2", [128, 1024], F32)
        h1 = [sb(f"g_h1_{i}", [128, 1024], F32) for i in range(2)]
        h1T = [sb(f"g_h1T{i}", [128, 8, 128], BF16) for i in range(2)]
        yk = [sb(f"g_yk{i}", [128, 1024], BF16) for i in range(8)]
        acc = sb("g_acc", [128, 1024], F32)
        sg = sb("g_sg", [128, 2, 128], F32)
        hid = sb("g_hid", [128, 2, 128], BF16)
        ot = [sb(f"g_ot{i}", [128, 1024], F32) for i in range(2)]
        stats = sb("g_stats", [128, 2, 6], F32)
        mv = sb("g_mv", [128, 2], F32)
        std = sb("g_std", [128, 1], F32)
        rstd = sb("g_rstd", [128, 1], F32)
        pgu = ps("g_pgu", [128, 4, 128], F32)
        py = ps("g_py", [128, 1024], F32)
        _bounds_reg(P, g)
        P.add("gpsimd", lambda e: e.dma_start(out=wgs[:], in_=g.w_gate_s.rearrange("(c p) n -> p c n", p=128)),
              w=["wgs"], chan="g_w")
        P.add("gpsimd", lambda e: e.dma_start(out=wus[:], in_=g.w_up_s.rearrange("(c p) n -> p c n", p=128)),
              w=["wus"], chan="g_w")
        P.add("gpsimd", lambda e: e.dma_start(out=wds[:], in_=g.w_down_s.rearrange("(c p) n -> p c n", p=128)),
              w=["wds"], chan="g_w")
        P.add("sync", lambda e: e.dma_start(out=g2rep[:], in_=g.ln2_g.partition_broadcast(128)), w=["g2rep"], chan="g_c")
        P.add("sync", lambda e: e.dma_start(out=b2rep[:], in_=g.ln2_b.partition_broadcast(128)), w=["b2rep"], chan="g_c")
        for i in range(8):
            P.add("gpsimd", lambda e, i=i: e.memset(yk[i][:], 0.0), w=[("yk", i)])
        yi = 0
        for blk in blocks:
            b2 = blk % 2
            P.add("sync", lambda e, blk=blk, b2=b2: e.dma_start(out=h1[b2][:], in_=g.h1_d[blk * 128:(blk + 1) * 128, :]),
                  w=[("h1", b2)], chan=f"g_h1{b2}")
            P.add("sync", lambda e, blk=blk, b2=b2: e.dma_start(out=h1T[b2][:], in_=g.h1T_d[:, :, blk * 128:(blk + 1) * 128]),
                  w=[("h1T", b2)], chan=f"g_h1T{b2}")
            for k in range(8):
                y3 = yi % 8
                yi += 1
                P.add("gpsimd", lambda e, blk=blk, k=k, y3=y3: e.indirect_dma_start(
                    out=yk[y3][:, :], out_offset=None, in_=g.ys_d[:, :],
                    in_offset=bass.IndirectOffsetOnAxis(ap=g.sidx[:, blk, k:k + 1], axis=0),
                    bounds_check=g.bcreg[0], oob_is_err=False),
                    r=[("sidx", blk)], w=[("yk", y3)], chan=f"g_yk{y3}")
                if k == 0:
                    P.add("vector", lambda e, blk=blk, y3=y3: e.tensor_scalar(
                        out=acc[:], in0=yk[y3][:], scalar1=g.gk[:, blk, 0:1], scalar2=None, op0=ALU.mult),
                        r=[("yk", y3), ("gk", blk)], w=["acc"])
                else:
                    P.add("vector", lambda e, blk=blk, k=k, y3=y3: e.scalar_tensor_tensor(
                        out=acc[:], in0=yk[y3][:], scalar=g.gk[:, blk, k:k + 1], in1=acc[:], op0=ALU.mult, op1=ALU.add),
                        r=[("yk", y3), ("gk", blk), "acc"], w=["acc"])
            _swiglu_block(P, h1T[b2], ("h1T", b2), wgs, wus, wds, ["wgs", "wus", "wds"],
                          pgu, "pgu", sg, "sg", hid, "hid", py, ("py",))
            o_t = ot[b2]
            P.add("vector", lambda e, b2=b2: e.scalar_tensor_tensor(
                out=acc[:], in0=h1[b2][:], scalar=ALPHA, in1=acc[:], op0=ALU.mult, op1=ALU.add),
                r=[("h1", b2), "acc"], w=["acc"])
            for nh in range(2):
                P.add("vector", lambda e, nh=nh: e.tensor_tensor(
                    out=acc[:, nh * 512:(nh + 1) * 512], in0=acc[:, nh * 512:(nh + 1) * 512],
                    in1=py[:, nh * 512:(nh + 1) * 512], op=ALU.add), r=["acc", ("py", nh)], w=["acc"])
            _layer_norm(P, acc, "acc", "acc", stats, mv, std, rstd, o_t, ("ot", b2), g2rep, "g2rep", b2rep, "b2rep", "g")
            P.add("scalar", lambda e, blk=blk, o_t=o_t: e.dma_start(out=g.out[blk * 128:(blk + 1) * 128, :], in_=o_t[:]),
                  r=[("ot", b2)], w=[("out", blk)], chan=f"g_out{b2}")
        P.emit_phase()


def build_program(debug=None, ntg=16, sb_groups=(0, 1, 2, 3), dl_slots=tuple(range(NOWN)), phases="abcdfg", d_groups=(0, 1, 2, 3),
                  f_experts=tuple(range(NEXP)), g_blocks=tuple(range(NOWN)), d_stop=9):
    nc = bass.Bass("TRN2", target_bir_lowering=False)
    g = Ctx()
    g.bcreg = []

    def din(name, shape, dt=F32):
        return nc.dram_tensor(name, shape, dt, kind="ExternalInput").ap()

    dbgnames = set(debug or ())

    def dscr(name, shape, dt):
        kind = "ExternalOutput" if name in dbgnames else "Internal"
        return nc.dram_tensor(name, shape, dt, kind=kind).ap()

    g.x_ctx = din("x_ctx", [NB * 128, D])
    g.valid = din("valid", [128, NB])
    g.ident = din("ident", [128, 128], BF16)
    g.negU = din("negU", [128, 128], BF16)
    g.negOnes = din("negOnes", [128, 128], BF16)
    g.sbmask = din("sbmask", [128, 16, 512], BF16)
    g.dlbias = din("dlbias", [128, DL_NT, 128], BF16)
    g.padbias = din("padbias", [128, NB])
    g.w_br_sb = din("w_br_sb", [512, D])
    g.w_br_dil = din("w_br_dil", [256, D])
    g.w_out = din("w_out", [D, D])
    g.w_router = din("w_router", [D, NEXP])
    g.b_gateT = din("b_gateT", [128, 16])
    g.ln1_g = din("ln1_g", [D])
    g.ln1_b = din("ln1_b", [D])
    g.router_bias = din("router_bias", [NEXP])
    ne_decl = NEXP if "f" in phases else 1
    g.w_gate_e = din("w_gate_e", [ne_decl, D, 256])
    g.w_up_e = din("w_up_e", [ne_decl, D, 256])
    g.w_down_e = din("w_down_e", [ne_decl, 256, D])
    g.w_gate_s = din("w_gate_s", [D, 256])
    g.w_up_s = din("w_up_s", [D, 256])
    g.w_down_s = din("w_down_s", [256, D])
    g.ln2_g = din("ln2_g", [D])
    g.ln2_b = din("ln2_b", [D])
    g.iota256 = din("iota256", [128, 256])
    g.lstrict = din("lstrict", [128, 128], BF16)
    g.ones = din("ones", [128, 128], BF16)
    g.ln_in_g = din("ln_in_g", [D])
    g.ln_in_b = din("ln_in_b", [D])
    g.ln_in_gT = din("ln_in_gT", [128, 8])
    g.ln_in_bT = din("ln_in_bT", [128, 8])
    g.w_in = din("w_in", [D, 5888])
    g.out = nc.dram_tensor("out", [TOWN, D], F32, kind="ExternalOutput").ap()

    g.kT_d = dscr("kT_d", [10, 128, S], BF16)
    g.v_d = dscr("v_d", [S, VW], BF16)
    g.qT_d = dscr("qT_d", [128, 10, TOWN], BF16)
    g.osbT_d = dscr("osbT_d", [8, 64, TOWN], BF16)
    g.odlT_d = dscr("odlT_d", [2, 128, TOWN], BF16)
    g.h_own_d = dscr("h_own_d", [TOWN, D], F32)
    g.h1_d = dscr("h1_d", [TOWN, D], F32)
    g.h1T_d = dscr("h1T_d", [128, 8, TOWN], BF16)
    g.xs_d = dscr("xs_d", [NEXP * CAP, D], BF16)
    g.ys_d = dscr("ys_d", [NEXP * CAP, D], BF16)
    g.hT_own_d = dscr("hT_own_d", [128, 8, TOWN], BF16)

    with contextlib.ExitStack() as stack:
        P = Prog(nc, stack)
        g.gk = stack.enter_context(nc.sbuf_tensor("gk", [128, NOWN, 8], F32))
        g.sidx = stack.enter_context(nc.sbuf_tensor("sidx", [128, NOWN, 8], I32))
        if "a" in phases:
            phase_a(P, nc, g, ntg)
        if "b" in phases:
            phase_b(P, nc, g, sb_groups)
        if "c" in phases:
            phase_c(P, nc, g, dl_slots)
        if "d" in phases:
            phase_d(P, nc, g, d_groups, d_stop)
        if "f" in phases:
            phase_f(P, nc, g, f_experts)
        if "g" in phases:
            phase_g(P, nc, g, g_blocks)
        if "gk" in dbgnames:
            dgk = nc.dram_tensor("dbg_gk", [128, NOWN, 8], F32, kind="ExternalOutput").ap()
            dsi = nc.dram_tensor("dbg_sidx", [128, NOWN, 8], I32, kind="ExternalOutput").ap()
            nbk = 4 * len(d_groups)
            P.add("sync", lambda e: e.dma_start(out=dgk[:, 0:nbk, :], in_=g.gk[:, 0:nbk, :]), w=["dgk"], chan="dbg")
            P.add("sync", lambda e: e.dma_start(out=dsi[:, 0:nbk, :], in_=g.sidx[:, 0:nbk, :]), w=["dsi"], chan="dbg")
            P.emit_phase()
    return nc


def _rel_bucket_np(dist):
    dist = np.asarray(dist, np.int64)
    max_exact = 16
    d = np.maximum(dist, 1).astype(np.float32)
    large = max_exact + (np.log(d / np.float32(max_exact)) / np.float32(np.log(2048 / 16))
                         * np.float32(32 - max_exact)).astype(np.int32)
    large = np.minimum(large, 31)
    return np.where(dist < max_exact, dist, large)


def host_consts(inputs):
    bf = ml_dtypes.bfloat16
    kl = np.arange(128)[:, None]
    ql = np.arange(128)[None, :]
    ident = np.eye(128, dtype=np.float32).astype(bf)
    negU = np.where(kl >= ql, -1.0, 0.0).astype(np.float32).astype(bf)
    negOnes = np.full((128, 128), -1.0, np.float32).astype(bf)
    sbmask = np.zeros((128, 16, 512), np.float32)
    for rel_c in range(16):
        for sl in range(4):
            rel_cq = 4 * sl + 3
            if rel_c == rel_cq:
                sbmask[:, rel_c, sl * 128:(sl + 1) * 128] = np.where(kl < ql, 0.0, NEG)
            elif rel_c > rel_cq:
                sbmask[:, rel_c, sl * 128:(sl + 1) * 128] = NEG
    rel_bias = np.asarray(inputs["rel_bias"], np.float32)
    dlbias = np.zeros((128, DL_NT, 128), np.float32)
    for gi, (w, dil) in enumerate(DIL):
        for hg in range(4):
            for o in range(DL_NB[gi]):
                dist = 128 * o + ql - kl
                ok = (dist >= 0) & (dist <= w) & (dist % dil == 0)
                bk = _rel_bucket_np(np.clip(dist, 0, None))
                val = rel_bias[bk, 4 * gi + hg]
                dlbias[:, DL_TOFF[gi] + hg * DL_NB[gi] + o, :] = np.where(ok, val, NEG)
    f32 = lambda k: np.ascontiguousarray(np.asarray(inputs[k], np.float32)[0])
    return dict(ident=ident, negU=negU, negOnes=negOnes, sbmask=sbmask.astype(bf), dlbias=dlbias.astype(bf),
                w_br_sb=f32("w_br_sb"), w_br_dil=f32("w_br_dil"), w_out=f32("w_out"), w_router=f32("w_router"),
                b_gateT=np.ascontiguousarray(f32("b_gate").reshape(16, 128).T),
                ln1_g=f32("ln1_g"), ln1_b=f32("ln1_b"), router_bias=f32("router_bias"),
                w_gate_e=f32("w_gate_e"), w_up_e=f32("w_up_e"), w_down_e=f32("w_down_e"),
                w_gate_s=f32("w_gate_s"), w_up_s=f32("w_up_s"), w_down_s=f32("w_down_s"),
                ln2_g=f32("ln2_g"), ln2_b=f32("ln2_b"),
                iota256=np.tile(np.arange(256, dtype=np.float32)[None, :], (128, 1)),
                lstrict=np.where(kl < ql, 1.0, 0.0).astype(np.float32).astype(bf),
                ones=np.ones((128, 128), np.float32).astype(bf))


def host_inputs(inputs):
    x = np.asarray(inputs["x"], dtype=np.float32)
    maps = []
    consts = host_consts(inputs)
    for core in range(NCORES):
        b, j = core // 4, core % 4
        xc = np.zeros((NB, 128, D), np.float32)
        valid = np.zeros((128, NB), np.float32)
        xb = x[b].reshape(64, 128, D)
        for c in range(NB):
            gb = c + j - 3
            if gb >= 0:
                xc[c] = xb[gb]
                valid[:, c] = 1.0
        m = {
            "x_ctx": xc.reshape(NB * 128, D),
            "valid": valid,
            "padbias": np.where(valid > 0, 0.0, NEG).astype(np.float32),
            "ln_in_g": np.asarray(inputs["ln_in_g"], np.float32),
            "ln_in_b": np.asarray(inputs["ln_in_b"], np.float32),
            "ln_in_gT": np.ascontiguousarray(np.asarray(inputs["ln_in_g"], np.float32).reshape(8, 128).T),
            "ln_in_bT": np.ascontiguousarray(np.asarray(inputs["ln_in_b"], np.float32).reshape(8, 128).T),
            "w_in": np.ascontiguousarray(np.asarray(inputs["w_in"], np.float32)[0]),
        }
        m.update(consts)
        maps.append(m)
    return maps


def kernel(**inputs):
    nc = build_program()
    maps = host_inputs(inputs)
    res = run_bass_kernel_spmd(nc, maps, core_ids=list(range(NCORES)))
    out = np.zeros((2, S, D), np.float32)
    for core in range(NCORES):
        b, j = core // 4, core % 4
        o = res.results[core]["out"].reshape(NOWN, 128, D)
        ob = out[b].reshape(64, 128, D)
        for s in range(NOWN):
            ob[4 * s + j] = o[s]
    return out
```

```python
import contextlib
import numpy as np
import ml_dtypes
import concourse.bass as bass
import concourse.mybir as mybir
from concourse.bass_utils import run_bass_kernel_spmd

F32 = mybir.dt.float32
BF16 = mybir.dt.bfloat16
I32 = mybir.dt.int32
U32 = mybir.dt.uint32
AF = mybir.ActivationFunctionType
ALU = mybir.AluOpType
AX = mybir.AxisListType

NCORES = 8
D = 1024
S = 8192
NB = 64
NOWN = 16
TOWN = NOWN * 128
LN_EPS = 1e-5
ALPHA = 2.0 ** 0.25
NEG = -30000.0
NEXP = 256
CAP = 256
TOPK = 8
VW = 512 + 12 * 65


class Op:
    __slots__ = ("eng", "fn", "deps", "chan", "sig", "count", "idx", "chan_count")


class Prog:
    ENGS = ("tensor", "vector", "scalar", "gpsimd", "sync")

    def __init__(self, nc, stack):
        self.nc = nc
        self.stack = stack
        self.sems = {e: stack.enter_context(nc.semaphore("sem_" + e)) for e in self.ENGS}
        self.sig_total = {e: 0 for e in self.ENGS}
        self.chan_sem = {}
        self.chan_total = {}
        self.reset_phase()

    def reset_phase(self):
        self.ops = []
        self.last_w = {}
        self.readers = {}
        self.chan_emitted = dict(self.chan_total)

    def chan(self, name):
        if name not in self.chan_sem:
            self.chan_sem[name] = self.stack.enter_context(self.nc.semaphore("ch_" + name))
            self.chan_total[name] = 0
            self.chan_emitted[name] = 0
        return name

    def add(self, eng, fn, r=(), w=(), chan=None):
        op = Op()
        op.eng = eng
        op.fn = fn
        op.chan = chan
        op.sig = False
        op.idx = len(self.ops)
        deps = set()
        for k in r:
            lw = self.last_w.get(k)
            if lw is not None:
                deps.add(lw)
        for k in w:
            lw = self.last_w.get(k)
            if lw is not None:
                deps.add(lw)
            for rd in self.readers.get(k, ()):
                deps.add(rd)
        deps.discard(op.idx)
        op.deps = []
        for d in deps:
            dop = self.ops[d]
            if dop.chan is not None:
                op.deps.append(("chan", dop.chan, self.chan_emitted[dop.chan]))
            else:
                if dop.eng == "tensor" and eng == "tensor":
                    continue
                dop.sig = True
                op.deps.append(("eng", dop.eng, dop))
        if chan is not None:
            self.chan(chan)
            self.chan_emitted[chan] += 16
            op.chan_count = self.chan_emitted[chan]
        self.ops.append(op)
        for k in r:
            self.readers.setdefault(k, []).append(op.idx)
        for k in w:
            self.last_w[k] = op.idx
            self.readers[k] = []
        return op

    def emit_phase(self):
        nc = self.nc
        last = {}
        for op in self.ops:
            if op.chan is None:
                last[op.eng] = op
        for op in last.values():
            op.sig = True
        tot = dict(self.sig_total)
        for op in self.ops:
            if op.chan is None and op.sig:
                tot[op.eng] += 1
                op.count = tot[op.eng]
        final_eng = dict(tot)
        final_chan = dict(self.chan_emitted)
        per_eng = {e: [o for o in self.ops if o.eng == e] for e in self.ENGS}
        sems = self.sems
        chan_sem = self.chan_sem

        def run(e, eobj):
            waited = {}

            def wait(kind, name, val):
                key = (kind, name)
                if waited.get(key, -1) >= val:
                    return
                waited[key] = val
                eobj.wait_ge(sems[name] if kind == "eng" else chan_sem[name], val)

            for op in per_eng[e]:
                for kind, name, v in op.deps:
                    wait(kind, name, v.count if kind == "eng" else v)
                ins = op.fn(eobj)
                if op.chan is not None:
                    ins.then_inc(chan_sem[op.chan], 16)
                elif op.sig:
                    ins.then_inc(sems[e], 1)
            for name, v in final_chan.items():
                if v > 0:
                    wait("chan", name, v)
            for name, v in final_eng.items():
                if v > 0 and name != e:
                    wait("eng", name, v)

        with nc.Block() as block:
            @block.tensor
            def _(e):
                run("tensor", e)

            @block.vector
            def _(e):
                run("vector", e)

            @block.scalar
            def _(e):
                run("scalar", e)

            @block.gpsimd
            def _(e):
                run("gpsimd", e)

            @block.sync
            def _(e):
                run("sync", e)

        self.sig_total = final_eng
        self.chan_total = final_chan
        self.reset_phase()


class Ctx:
    pass


def phase_a(P, nc, g, ntg=16):
    st = contextlib.ExitStack()
    with st:
        sb = lambda name, shape, dt: st.enter_context(nc.sbuf_tensor(name, shape, dt))
        ps = lambda name, shape, dt: st.enter_context(nc.psum_tensor(name, shape, dt))
        winb = sb("a_winb", [128, 8, 3840], BF16)
        gT = sb("a_gT", [128, 8], F32)
        bT = sb("a_bT", [128, 8], F32)
        grep = sb("a_grep", [128, 1024], F32)
        brep = sb("a_brep", [128, 1024], F32)
        valid = sb("a_valid", [128, NB], F32)
        ident = sb("a_ident", [128, 128], BF16)
        NX = 3
        xt = [sb(f"a_x{i}", [128, 1024], F32) for i in range(NX)]
        stats = [sb(f"a_stats{i}", [128, 2, 6], F32) for i in range(2)]
        mv = [sb(f"a_mv{i}", [128, 2], F32) for i in range(2)]
        rstd = [sb(f"a_rstd{i}", [128, 1], F32) for i in range(2)]
        std = [sb(f"a_std{i}", [128, 1], F32) for i in range(2)]
        ybf = [sb(f"a_ybf{i}", [128, 1024], BF16) for i in range(2)]
        y32 = sb("a_y32", [128, 1024], F32)
        hTg = [sb(f"a_hTg{i}", [128, 8, 512], BF16) for i in range(2)]
        kst = [sb(f"a_kst{i}", [128, 512], BF16) for i in range(3)]
        vst = [sb(f"a_vst{i}", [128, VW], BF16) for i in range(2)]
        qst = [sb(f"a_qst{i}", [128, 10, 128], BF16) for i in range(2)]
        ones12 = sb("a_ones12", [128, 12, 1], F32)
        tp = [ps(f"a_tp{i}", [128, 8, 128], BF16) for i in range(2)]
        pm = [ps(f"a_pm{i}", [128, 512], F32) for i in range(6)]

        for dc in range(8):
            P.add("gpsimd", lambda e, dc=dc: e.dma_start(
                out=winb[:, dc, :], in_=g.w_in[dc * 128:(dc + 1) * 128, 0:3840]),
                w=[("winb", dc)], chan="a_w")
        P.add("sync", lambda e: e.dma_start(out=gT[:], in_=g.ln_in_gT),
              w=["gT"], chan="a_c")
        P.add("sync", lambda e: e.dma_start(out=bT[:], in_=g.ln_in_bT),
              w=["bT"], chan="a_c")
        P.add("sync", lambda e: e.dma_start(out=grep[:], in_=g.ln_in_g.partition_broadcast(128)),
              w=["grep"], chan="a_c")
        P.add("sync", lambda e: e.dma_start(out=brep[:], in_=g.ln_in_b.partition_broadcast(128)),
              w=["brep"], chan="a_c")
        P.add("sync", lambda e: e.dma_start(out=valid[:], in_=g.valid), w=["valid"], chan="a_c")
        P.add("sync", lambda e: e.dma_start(out=ident[:], in_=g.ident), w=["ident"], chan="a_c")

        P.add("vector", lambda e: e.memset(ones12[:], 1.0), w=["ones12"])
        kcols = [512 + 128 * i for i in range(4)] + [2304 + 128 * i for i in range(6)]
        qcols = [0 + 128 * i for i in range(4)] + [1536 + 128 * i for i in range(6)]
        vgroups = [(1024, 512, 0), (3072, 512, 512), (3584, 256, 1024)]
        cnt = {'pmi': 0, 'ksi': 0}

        def lnt(tg, bi):
            hT = hTg[tg % 2]
            hk = ("hTg", tg % 2)
            c = 4 * tg + bi
            xs = c % NX
            s2 = c % 2
            x_t = xt[xs]
            P.add("sync", lambda e, x_t=x_t, c=c: e.dma_start(
                out=x_t[:], in_=g.x_ctx[c * 128:(c + 1) * 128, :]),
                w=[("x", xs)], chan=f"a_x{xs}")
            for hh in range(2):
                P.add("vector", lambda e, x_t=x_t, s2=s2, hh=hh: e.bn_stats(
                    out=stats[s2][:, hh, :], in_=x_t[:, hh * 512:(hh + 1) * 512]),
                    r=[("x", xs)], w=[("stats", s2, hh)])
            P.add("vector", lambda e, s2=s2: e.bn_aggr(
                out=mv[s2][:], in_=stats[s2][:].rearrange("p a b -> p (a b)")),
                r=[("stats", s2, 0), ("stats", s2, 1)], w=[("mv", s2)])
            P.add("scalar", lambda e, s2=s2: e.activation(
                out=std[s2][:], in_=mv[s2][:, 1:2], func=AF.Sqrt, bias=LN_EPS),
                r=[("mv", s2)], w=[("std", s2)])
            P.add("vector", lambda e, s2=s2: e.reciprocal(out=rstd[s2][:], in_=std[s2][:]),
                r=[("std", s2)], w=[("rstd", s2)])
            P.add("vector", lambda e, x_t=x_t, s2=s2: e.tensor_scalar(
                out=ybf[s2][:], in0=x_t[:], scalar1=mv[s2][:, 0:1], scalar2=rstd[s2][:, 0:1],
                op0=ALU.subtract, op1=ALU.mult),
                r=[("x", xs), ("mv", s2), ("rstd", s2)], w=[("ybf", s2)])
            if bi == 3:
                so = tg
                P.add("gpsimd", lambda e, x_t=x_t, s2=s2: e.tensor_scalar(
                    out=y32[:], in0=x_t[:], scalar1=mv[s2][:, 0:1], scalar2=rstd[s2][:, 0:1],
                    op0=ALU.subtract, op1=ALU.mult),
                    r=[("x", xs), ("mv", s2), ("rstd", s2)], w=["y32"])
                P.add("gpsimd", lambda e: e.tensor_tensor(out=y32[:], in0=y32[:], in1=grep[:], op=ALU.mult),
                      r=["y32", "grep"], w=["y32"])
                P.add("gpsimd", lambda e: e.tensor_tensor(out=y32[:], in0=y32[:], in1=brep[:], op=ALU.add),
                      r=["y32", "brep"], w=["y32"])
                P.add("gpsimd", lambda e, so=so: e.dma_start(
                    out=g.h_own_d[so * 128:(so + 1) * 128, :], in_=y32[:]),
                    r=["y32"], w=[("h_own_d", so)], chan="a_y32")

        def tr(tg, bi):
            hT = hTg[tg % 2]
            hk = ("hTg", tg % 2)
            c = 4 * tg + bi
            s2 = c % 2
            tps = tp[c % 2]
            for dc in range(8):
                P.add("tensor", lambda e, tps=tps, s2=s2, dc=dc: e.transpose(
                    out=tps[:, dc, :], in_=ybf[s2][:, dc * 128:(dc + 1) * 128], identity=ident[:]),
                    r=[("ybf", s2), "ident"], w=[("tp", c % 2)])
            for dc in range(8):
                P.add("scalar", lambda e, tps=tps, dc=dc, hT=hT, bi=bi: e.activation(
                    out=hT[:, dc, bi * 128:(bi + 1) * 128], in_=tps[:, dc, :], func=AF.Identity,
                    scale=gT[:, dc:dc + 1], bias=bT[:, dc:dc + 1]),
                    r=[("tp", c % 2), "gT", "bT"], w=[hk + (bi,)])

        def mm(tg):
            hT = hTg[tg % 2]
            hk = ("hTg", tg % 2)
            pmi = cnt['pmi']
            ksi = cnt['ksi']
            hkall = [hk + (bi,) for bi in range(4)]
            P.add("gpsimd", lambda e, hT=hT, tg=tg: e.dma_start(
                out=g.hT_own_d[:, :, tg * 128:(tg + 1) * 128], in_=hT[:, :, 384:512]),
                r=[hk + (3,)], w=[("hT_own_d", tg)], chan=f"a_hT{tg % 2}")
            for kc in range(10):
                pmt = pm[pmi % 6]
                pk = ("pm", pmi % 6)
                pmi += 1
                for dc in range(8):
                    P.add("tensor", lambda e, pmt=pmt, dc=dc, kc=kc, hT=hT: e.matmul(
                        pmt[:], lhsT=winb[:, dc, kcols[kc]:kcols[kc] + 128], rhs=hT[:, dc, :],
                        start=(dc == 0), stop=(dc == 7)),
                        r=[("winb", dc)] + hkall, w=[pk])
                ks = kst[ksi % 3]
                kk = ("kst", ksi % 3)
                ksi_l = ksi % 3
                ksi += 1
                eng = "scalar" if kc % 2 == 0 else "vector"
                if eng == "scalar":
                    P.add("scalar", lambda e, ks=ks, pmt=pmt: e.activation(out=ks[:], in_=pmt[:], func=AF.Copy),
                          r=[pk], w=[kk])
                else:
                    P.add("vector", lambda e, ks=ks, pmt=pmt: e.tensor_copy(out=ks[:], in_=pmt[:]),
                          r=[pk], w=[kk])
                P.add("gpsimd", lambda e, ks=ks, kc=kc, tg=tg: e.dma_start(
                    out=g.kT_d[kc, :, tg * 512:(tg + 1) * 512], in_=ks[:]),
                    r=[kk], w=[("kT_d", kc, tg)], chan=f"a_kst{ksi_l}")
                yield
            for bi in range(4):
                c = 4 * tg + bi
                vs = vst[c % 2]
                vk = ("vst", c % 2)
                for (c0, ncol, o0) in vgroups:
                    pmt = pm[pmi % 6]
                    pk = ("pm", pmi % 6)
                    pmi += 1
                    for dc in range(8):
                        P.add("tensor", lambda e, pmt=pmt, dc=dc, c0=c0, ncol=ncol, hT=hT, bi=bi: e.matmul(
                            pmt[:, 0:ncol], lhsT=hT[:, dc, bi * 128:(bi + 1) * 128], rhs=winb[:, dc, c0:c0 + ncol],
                            start=(dc == 0), stop=(dc == 7)),
                            r=[("winb", dc), hk + (bi,)], w=[pk])
                    if o0 == 0:
                        o_ap = vs[:, 0:512]
                        i_ap = pmt[:, 0:512]
                    else:
                        h0 = (o0 - 512) // 64
                        nh = ncol // 64
                        o_ap = vs[:, 512:VW].rearrange("p (h e) -> p h e", e=65)[:, h0:h0 + nh, 0:64]
                        i_ap = pmt[:, 0:ncol].rearrange("p (h e) -> p h e", e=64)
                    P.add("vector", lambda e, o_ap=o_ap, i_ap=i_ap, c=c: e.tensor_scalar(
                        out=o_ap, in0=i_ap, scalar1=valid[:, c:c + 1], scalar2=None,
                        op0=ALU.mult),
                        r=[pk, "valid"], w=[vk + (o0,)])
                    yield
                P.add("vector", lambda e, vs=vs, c=c: e.tensor_scalar(
                    out=vs[:, 512:VW].rearrange("p (h e) -> p h e", e=65)[:, :, 64:65], in0=ones12[:],
                    scalar1=valid[:, c:c + 1], scalar2=None, op0=ALU.mult),
                    r=["ones12", "valid"], w=[vk + (1024,)])
                P.add("gpsimd", lambda e, vs=vs, c=c: e.dma_start(
                    out=g.v_d[c * 128:(c + 1) * 128, :], in_=vs[:]),
                    r=[vk + (0,), vk + (512,), vk + (1024,)], w=[("v_d", c)], chan=f"a_vst{c % 2}")
            for qc in range(10):
                pmt = pm[pmi % 6]
                pk = ("pm", pmi % 6)
                pmi += 1
                for dc in range(8):
                    P.add("tensor", lambda e, pmt=pmt, dc=dc, qc=qc, hT=hT: e.matmul(
                        pmt[:, 0:128], lhsT=winb[:, dc, qcols[qc]:qcols[qc] + 128], rhs=hT[:, dc, 384:512],
                        start=(dc == 0), stop=(dc == 7)),
                        r=[("winb", dc), hk + (3,)], w=[pk])
                P.add("scalar", lambda e, pmt=pmt, qc=qc, tg=tg: e.activation(
                    out=qst[tg % 2][:, qc, :], in_=pmt[:, 0:128], func=AF.Copy, scale=0.125),
                    r=[pk], w=[("qst", tg % 2, qc)])
                yield
            P.add("gpsimd", lambda e, tg=tg: e.dma_start(
                out=g.qT_d[:, :, tg * 128:(tg + 1) * 128], in_=qst[tg % 2][:]),
                r=[("qst", tg % 2, qc) for qc in range(10)], w=[("qT_d", tg)], chan=f"a_qst{tg % 2}")
            cnt['pmi'] = pmi
            cnt['ksi'] = ksi

        for bi in range(4):
            lnt(0, bi)
            tr(0, bi)
        for tg in range(ntg):
            gen = mm(tg)
            for gi_, _ in enumerate(gen):
                if tg + 1 < ntg and gi_ in (0, 8, 16, 24):
                    lnt(tg + 1, gi_ // 8)
                if tg + 1 < ntg and gi_ in (6, 14, 22, 30):
                    tr(tg + 1, (gi_ - 6) // 8)
        P.emit_phase()


def phase_b(P, nc, g, groups=(0, 1, 2, 3)):
    st = contextlib.ExitStack()
    with st:
        sb = lambda name, shape, dt: st.enter_context(nc.sbuf_tensor(name, shape, dt))
        ps = lambda name, shape, dt: st.enter_context(nc.psum_tensor(name, shape, dt))
        nblk_max = 16 * (max(groups) + 1)
        kTsb = sb("b_kT", [128, 4, S], BF16)
        vsb = sb("b_v", [128, NB, 512], BF16)
        sbmask = sb("b_mask", [128, 16, 512], BF16)
        ident = sb("b_ident", [128, 128], BF16)
        negU = sb("b_negU", [128, 128], BF16)
        negOnes = sb("b_negOnes", [128, 128], BF16)
        zer = sb("b_zer", [128, 64], BF16)
        qsb = [sb(f"b_q{i}", [128, 4, 512], BF16) for i in range(2)]
        e_sb = [sb(f"b_e{i}", [128, 512], F32) for i in range(4)]
        sp_sb = [sb(f"b_sp{i}", [128, 512], BF16) for i in range(4)]
        w_sb = [sb(f"b_w{i}", [128, 512], BF16) for i in range(4)]
        srun = [[sb(f"b_srun{i}_{j}", [128, 512], BF16) for j in range(2)] for i in range(4)]
        ost = [sb(f"b_ost{i}", [64, 512], BF16) for i in range(4)]
        pz = [ps(f"b_pz{i}", [128, 512], F32) for i in range(4)]
        po = [ps(f"b_po{i}", [64, 512], F32) for i in range(4)]

        P.add("sync", lambda e: e.dma_start(out=ident[:], in_=g.ident), w=["ident"], chan="b_c")
        P.add("sync", lambda e: e.dma_start(out=negU[:], in_=g.negU), w=["negU"], chan="b_c")
        P.add("sync", lambda e: e.dma_start(out=negOnes[:], in_=g.negOnes), w=["negOnes"], chan="b_c")
        P.add("sync", lambda e: e.dma_start(out=sbmask[:], in_=g.sbmask), w=["sbmask"], chan="b_c")
        P.add("gpsimd", lambda e: e.memset(zer[:], 0.0), w=["zer"])
        ntok = nblk_max * 128
        for kc in range(4):
            for hf in range(0, ntok, 2048):
                P.add("sync", lambda e, kc=kc, hf=hf: e.dma_start(
                    out=kTsb[:, kc, hf:hf + 2048], in_=g.kT_d[kc, :, hf:hf + 2048]),
                    w=[("kTsb", kc, hf // 2048)], chan="b_k")
        for cb in range(0, nblk_max, 16):
            P.add("sync", lambda e, cb=cb: e.dma_start(
                out=vsb[:, cb:cb + 16, :],
                in_=g.v_d[cb * 128:(cb + 16) * 128, 0:512].rearrange("(c p) n -> p c n", p=128)),
                w=[("vsb", cb // 16)], chan="b_v")

        for gi, gq in enumerate(groups):
            q_t = qsb[gi % 2]
            qk = ("qsb", gi % 2)
            P.add("sync", lambda e, q_t=q_t, gq=gq: e.dma_start(
                out=q_t[:], in_=g.qT_d[:, 0:4, gq * 512:(gq + 1) * 512]),
                w=[qk], chan=f"b_q{gi % 2}")
            nblk = 16 * (gq + 1)
            for hq in range(2):
                heads = [4 * hq + i for i in range(4)]
                for i in range(4):
                    for par in range(2):
                        P.add("gpsimd", lambda e, i=i, par=par: e.memset(srun[i][par][:], 0.0), w=[("srun", i, par)])
                    P.add("tensor", lambda e, i=i: e.matmul(
                        po[i][:], lhsT=zer[:], rhs=sbmask[:, 0, :], start=True, stop=True),
                        r=["zer", "sbmask"], w=[("po", i)])
                cs = list(range(nblk - 1, -1, -1))

                def prm(step):
                    c = cs[step]
                    rel_c = c - 16 * gq
                    q0 = 128 * (rel_c // 4) if rel_c >= 4 else 0
                    return c, rel_c, (rel_c >= 3), step % 2, q0

                def s1(step, i):
                    c, rel_c, need_mask, par, q0 = prm(step)
                    h = heads[i]
                    hc, half = h // 2, h % 2
                    p0 = 64 * half
                    P.add("tensor", lambda e, i=i, hc=hc, p0=p0, c=c, q_t=q_t, nm=need_mask, q0=q0: e.matmul(
                        pz[i][:, q0:512], lhsT=kTsb[p0:p0 + 64, hc, c * 128:(c + 1) * 128],
                        rhs=q_t[p0:p0 + 64, hc, q0:512], start=True, stop=(not nm)),
                        r=[("kTsb", hc, c // 16), qk], w=[("pz", i)])
                    if need_mask:
                        P.add("tensor", lambda e, i=i, rel_c=rel_c, q0=q0: e.matmul(
                            pz[i][:, q0:512], lhsT=ident[:], rhs=sbmask[:, rel_c, q0:512], start=False, stop=True),
                            r=["ident", "sbmask"], w=[("pz", i)])

                for i in range(4):
                    s1(0, i)
                for step in range(nblk):
                    c, rel_c, need_mask, par, q0 = prm(step)
                    for i, h in enumerate(heads):
                        P.add("scalar", lambda e, i=i, q0=q0: e.activation(
                            out=e_sb[i][:, q0:512], in_=pz[i][:, q0:512], func=AF.Exp),
                            r=[("pz", i)], w=[("e", i)])
                        P.add("scalar", lambda e, i=i, q0=q0: e.activation(
                            out=sp_sb[i][:, q0:512], in_=e_sb[i][:, q0:512], func=AF.Ln, bias=1.0),
                            r=[("e", i)], w=[("sp", i)])
                    for i, h in enumerate(heads):
                        last = (step == 0)
                        P.add("tensor", lambda e, i=i, last=last, q0=q0: e.matmul(
                            pz[i][:, q0:512], lhsT=negU[:], rhs=sp_sb[i][:, q0:512], start=False, stop=last,
                            skip_group_check=True),
                            r=["negU", ("sp", i)], w=[("pz", i)])
                        if step > 0:
                            P.add("tensor", lambda e, i=i, par=par, q0=q0: e.matmul(
                                pz[i][:, q0:512], lhsT=negOnes[:], rhs=srun[i][1 - par][:, q0:512], start=False,
                                stop=True, skip_group_check=True),
                                r=["negOnes", ("srun", i, 1 - par)], w=[("pz", i)])
                    for i, h in enumerate(heads):
                        if step == 0:
                            P.add("vector", lambda e, i=i, par=par, q0=q0: e.tensor_copy(
                                out=srun[i][par][:, q0:512], in_=sp_sb[i][:, q0:512]),
                                r=[("sp", i)], w=[("srun", i, par)])
                        elif c > 0:
                            P.add("vector", lambda e, i=i, par=par, q0=q0: e.tensor_tensor(
                                out=srun[i][par][:, q0:512], in0=srun[i][1 - par][:, q0:512], in1=sp_sb[i][:, q0:512],
                                op=ALU.add),
                                r=[("sp", i), ("srun", i, 1 - par)], w=[("srun", i, par)])
                    for i, h in enumerate(heads):
                        P.add("scalar", lambda e, i=i, q0=q0: e.activation(
                            out=w_sb[i][:, q0:512], in_=pz[i][:, q0:512], func=AF.Exp),
                            r=[("pz", i)], w=[("w", i)])
                    for i, h in enumerate(heads):
                        if step + 1 < nblk:
                            s1(step + 1, i)
                        P.add("tensor", lambda e, i=i, h=h, c=c, q0=q0: e.matmul(
                            po[i][:, q0:512], lhsT=vsb[:, c, h * 64:(h + 1) * 64], rhs=w_sb[i][:, q0:512],
                            start=False, stop=True, skip_group_check=True),
                            r=[("vsb", c // 16), ("w", i)], w=[("po", i)])
                for i, h in enumerate(heads):
                    P.add("vector", lambda e, i=i: e.tensor_copy(out=ost[i][:], in_=po[i][:]),
                          r=[("po", i)], w=[("ost", i)])
                    P.add("sync", lambda e, i=i, h=h, gq=gq: e.dma_start(
                        out=g.osbT_d[h, :, gq * 512:(gq + 1) * 512], in_=ost[i][:]),
                        r=[("ost", i)], w=[("osbT_d", h, gq)], chan=f"b_ost{i}")
        P.emit_phase()


DIL = ((128, 1), (512, 4), (2048, 16))
DL_NB = [w // 128 + 1 for w, _ in DIL]
DL_TOFF = [0, 4 * DL_NB[0], 4 * (DL_NB[0] + DL_NB[1])]
DL_NT = 4 * sum(DL_NB)


def phase_c(P, nc, g, slots=tuple(range(NOWN))):
    st = contextlib.ExitStack()
    with st:
        sb = lambda name, shape, dt: st.enter_context(nc.sbuf_tensor(name, shape, dt))
        ps = lambda name, shape, dt: st.enter_context(nc.psum_tensor(name, shape, dt))
        WB = 17
        kdl = [sb(f"c_k{i}", [128, 6, WB * 128], BF16) for i in range(2)]
        vdl = [sb(f"c_v{i}", [128, WB, 780], BF16) for i in range(2)]
        qdl = [sb(f"c_q{i}", [128, 6, 128], BF16) for i in range(2)]
        dlbias = sb("c_bias", [128, DL_NT, 128], BF16)
        padbias = sb("c_pad", [128, NB], F32)
        ident = sb("c_ident", [128, 128], BF16)
        pT = [sb(f"c_pT{i}", [128, 4, 128], BF16) for i in range(4)]
        rden = sb("c_rden", [128, 4], F32)
        otok = [sb(f"c_otok{i}", [128, 256], BF16) for i in range(2)]
        oT = [sb(f"c_oT{i}", [128, 2, 128], BF16) for i in range(2)]
        pzd = [ps(f"c_pz{i}", [128, 4, 128], F32) for i in range(4)]
        pd = [ps(f"c_pd{i}", [128, 4, 65], F32) for i in range(2)]
        ptr = ps("c_ptr", [128, 2, 128], BF16)

        P.add("sync", lambda e: e.dma_start(out=ident[:], in_=g.ident), w=["ident"], chan="c_c")
        P.add("sync", lambda e: e.dma_start(out=padbias[:], in_=g.padbias), w=["padbias"], chan="c_c")
        for t0 in range(0, DL_NT, 24):
            P.add("sync", lambda e, t0=t0: e.dma_start(out=dlbias[:, t0:t0 + 24, :], in_=g.dlbias[:, t0:t0 + 24, :]),
                  w=[("dlbias", t0 // 24)], chan="c_c")
        ui = 0
        for si, s_ in enumerate(slots):
            cq = 4 * s_ + 3
            c_lo = max(0, cq - 16)
            nwb = cq - c_lo + 1
            b2 = si % 2
            P.add("sync", lambda e, b2=b2, c_lo=c_lo, nwb=nwb: e.dma_start(
                out=kdl[b2][:, :, 0:nwb * 128],
                in_=g.kT_d[4:10, :, c_lo * 128:(c_lo + nwb) * 128].rearrange("c p t -> p c t")),
                w=[("kdl", b2)], chan=f"c_k{b2}")
            P.add("sync", lambda e, b2=b2, c_lo=c_lo, nwb=nwb: e.dma_start(
                out=vdl[b2][:, 0:nwb, :],
                in_=g.v_d[c_lo * 128:(c_lo + nwb) * 128, 512:VW].rearrange("(c p) n -> p c n", p=128)),
                w=[("vdl", b2)], chan=f"c_v{b2}")
            P.add("sync", lambda e, b2=b2, s_=s_: e.dma_start(
                out=qdl[b2][:], in_=g.qT_d[:, 4:10, s_ * 128:(s_ + 1) * 128]),
                w=[("qdl", b2)], chan=f"c_q{b2}")
            pdt = pd[si % 2]
            pdk = ("pd", si % 2)
            units = []
            for hg in range(4):
                uh = []
                for gi in range(3):
                    for o in range(DL_NB[gi]):
                        c = cq - o
                        if c >= 0:
                            uh.append((gi, o, c))
                for k_, (gi, o, c) in enumerate(uh):
                    units.append((hg, k_, len(uh), gi, o, c))
            NBATCH = 4
            batches = [units[i:i + NBATCH] for i in range(0, len(units), NBATCH)]

            def s1(bt, zi):
                for j, u in enumerate(bt):
                    hg, k_, n, gi, o, c = u
                    hd = 4 * gi + hg
                    chn, half = hd // 2, hd % 2
                    p0 = 64 * half
                    wb = c - c_lo
                    tix = DL_TOFF[gi] + hg * DL_NB[gi] + o
                    P.add("tensor", lambda e, zi=zi, j=j, b2=b2, chn=chn, p0=p0, wb=wb: e.matmul(
                        pzd[zi][:, j, :], lhsT=kdl[b2][p0:p0 + 64, chn, wb * 128:(wb + 1) * 128],
                        rhs=qdl[b2][p0:p0 + 64, chn, :], start=True, stop=False),
                        r=[("kdl", b2), ("qdl", b2)], w=[("pzd", zi)])
                    P.add("tensor", lambda e, zi=zi, j=j, tix=tix: e.matmul(
                        pzd[zi][:, j, :], lhsT=ident[:], rhs=dlbias[:, tix, :], start=False, stop=True),
                        r=["ident", ("dlbias", tix // 24)], w=[("pzd", zi)])
                nb_ = len(bt)
                P.add("scalar", lambda e, zi=zi, nb_=nb_: e.activation(
                    out=pT[zi][:, 0:nb_, :], in_=pzd[zi][:, 0:nb_, :], func=AF.Exp),
                    r=[("pzd", zi)], w=[("pT", zi)])

            def s2(bt, zi):
                for j, u in enumerate(bt):
                    hg, k_, n, gi, o, c = u
                    hd = 4 * gi + hg
                    wb = c - c_lo
                    P.add("tensor", lambda e, zi=zi, j=j, b2=b2, wb=wb, hd=hd, hg=hg, k_=k_, n=n, pdt=pdt: e.matmul(
                        pdt[:, hg, :], lhsT=pT[zi][:, j, :], rhs=vdl[b2][:, wb, hd * 65:(hd + 1) * 65],
                        start=(k_ == 0), stop=(k_ == n - 1)),
                        r=[("pT", zi), ("vdl", b2)], w=[pdk])

            LOOK = 3
            zis = []
            for i in range(len(batches) + LOOK):
                if i < len(batches):
                    zis.append(ui % 4)
                    ui += 1
                    s1(batches[i], zis[i])
                if i - LOOK >= 0:
                    s2(batches[i - LOOK], zis[i - LOOK])
            P.add("vector", lambda e, pdt=pdt: e.reciprocal(out=rden[:], in_=pdt[:, :, 64]),
                  r=[pdk], w=["rden"])
            ot = otok[si % 2]
            for hg in range(4):
                P.add("vector", lambda e, pdt=pdt, hg=hg, ot=ot: e.tensor_scalar(
                    out=ot[:, hg * 64:(hg + 1) * 64], in0=pdt[:, hg, 0:64], scalar1=rden[:, hg:hg + 1],
                    scalar2=None, op0=ALU.mult),
                    r=[pdk, "rden"], w=[("otok", si % 2)])
            for cc in range(2):
                P.add("tensor", lambda e, cc=cc, ot=ot: e.transpose(
                    out=ptr[:, cc, :], in_=ot[:, cc * 128:(cc + 1) * 128], identity=ident[:]),
                    r=[("otok", si % 2), "ident"], w=["ptr"])
            P.add("vector", lambda e, si=si: e.tensor_copy(out=oT[si % 2][:], in_=ptr[:]),
                  r=["ptr"], w=[("oT", si % 2)])
            P.add("gpsimd", lambda e, si=si, s_=s_: e.dma_start(
                out=g.odlT_d[:, :, s_ * 128:(s_ + 1) * 128].rearrange("c p t -> p c t"), in_=oT[si % 2][:]),
                r=[("oT", si % 2)], w=[("odlT_d", s_)], chan=f"c_oT{si % 2}")
        P.emit_phase()


def _bounds_reg(P, g):
    def fn(e):
        if not g.bcreg:
            g.bcreg.append(e.alloc_register("bc"))
        return e.reg_mov(g.bcreg[0], NEXP * CAP - 1)
    P.add("gpsimd", fn)


def phase_d(P, nc, g, qgroups=(0, 1, 2, 3), d_stop=9):
    st = contextlib.ExitStack()
    with st:
        sb = lambda name, shape, dt: st.enter_context(nc.sbuf_tensor(name, shape, dt))
        ps = lambda name, shape, dt: st.enter_context(nc.psum_tensor(name, shape, dt))
        wg = sb("d_wg", [128, 8, 2048], BF16)
        wbs = sb("d_wbs", [64, 8, 1024], BF16)
        wbd = sb("d_wbd", [128, 2, 1024], BF16)
        wo = sb("d_wo", [128, 8, 1024], BF16)
        wr = sb("d_wr", [128, 8, 256], F32)
        bgT = sb("d_bgT", [128, 16], F32)
        g1rep = sb("d_g1", [128, 1024], F32)
        b1rep = sb("d_b1", [128, 1024], F32)
        rbrep = sb("d_rb", [128, 256], F32)
        iota = sb("d_iota", [128, 256], F32)
        identb = sb("d_identb", [128, 128], BF16)
        wr_hi = sb("d_wr_hi", [128, 8, 256], BF16)
        wr_lo = sb("d_wr_lo", [128, 8, 256], BF16)
        h1lo = sb("d_h1lo", [128, 1024], BF16)
        h1Tlo = sb("d_h1Tlo", [128, 8, 128], BF16)
        lstrict = sb("d_lstrict", [128, 128], BF16)
        ones = sb("d_ones", [128, 128], BF16)
        hTo = sb("d_hTo", [128, 8, 512], BF16)
        osb = sb("d_osb", [64, 8, 512], BF16)
        odl = sb("d_odl", [128, 2, 512], BF16)
        gs = sb("d_gs", [128, 512], F32)
        gd = sb("d_gd", [128, 512], F32)
        m1 = sb("d_m1", [128, 512], F32)
        m2 = sb("d_m2", [128, 512], F32)
        mT = sb("d_mT", [128, 8, 512], BF16)
        hown = [sb(f"d_hown{i}", [128, 1024], F32) for i in range(2)]
        rr = sb("d_r", [128, 1024], F32)
        h1 = [sb(f"d_h1_{i}", [128, 1024], F32) for i in range(2)]
        h1b = [sb(f"d_h1b{i}", [128, 1024], BF16) for i in range(2)]
        h1Tb = [sb(f"d_h1Tb{i}", [128, 8, 128], BF16) for i in range(2)]
        stats = sb("d_stats", [128, 2, 6], F32)
        mv = sb("d_mv", [128, 2], F32)
        std = sb("d_std", [128, 1], F32)
        rstd = sb("d_rstd", [128, 1], F32)
        sc = [sb(f"d_sc{i}", [128, 256], F32) for i in range(2)]
        biased = [sb(f"d_biased{i}", [128, 256], F32) for i in range(2)]
        masked = sb("d_masked", [128, 256], F32)
        junk = sb("d_junk", [128, 256], F32)
        selb = sb("d_selb", [128, NOWN, 256], BF16)
        m8g = sb("d_m8g", [128, 8, 8], F32)
        gscore = sb("d_gscore", [128, 8], F32)
        gm8 = sb("d_gm8", [128, 8], F32)
        pen = sb("d_pen", [128, 8], F32)
        t8 = sb("d_t8", [128, 8], F32)
        wk = sb("d_wk", [128, 8], F32)
        rk = sb("d_rk", [128, 8], F32)
        ik = sb("d_ik", [128, 8], F32)
        wsum = sb("d_wsum", [128, 1], F32)
        sif = sb("d_sif", [128, 8], F32)
        ovf = sb("d_ovf", [128, 8], F32)
        pA = ps("d_pA", [128, 512], F32)
        pB = ps("d_pB", [128, 512], F32)
        pC = ps("d_pC", [128, 512], F32)
        pD = ps("d_pD", [128, 512], F32)
        pmix = ps("d_pmix", [128, 1024], F32)
        ptr = ps("d_ptr", [128, 8, 128], BF16)
        ptr_lo = ps("d_ptr_lo", [128, 8, 128], BF16)

        _bounds_reg(P, g)
        for dc in range(8):
            P.add("gpsimd", lambda e, dc=dc: e.dma_start(
                out=wg[:, dc, :], in_=g.w_in[dc * 128:(dc + 1) * 128, 3840:5888]), w=[("wg", dc)], chan="d_w")
        P.add("gpsimd", lambda e: e.dma_start(
            out=wbs[:], in_=g.w_br_sb.rearrange("(h p) n -> p h n", p=64)), w=["wbs"], chan="d_w")
        P.add("gpsimd", lambda e: e.dma_start(
            out=wbd[:], in_=g.w_br_dil.rearrange("(c p) n -> p c n", p=128)), w=["wbd"], chan="d_w")
        for dc in range(8):
            P.add("gpsimd", lambda e, dc=dc: e.dma_start(
                out=wo[:, dc, :], in_=g.w_out[dc * 128:(dc + 1) * 128, :]), w=[("wo", dc)], chan="d_w")
        P.add("sync", lambda e: e.dma_start(out=wr[:], in_=g.w_router.rearrange("(c p) n -> p c n", p=128)),
              w=["wr"], chan="d_wr")
        P.add("sync", lambda e: e.dma_start(out=bgT[:], in_=g.b_gateT), w=["bgT"], chan="d_c")
        P.add("sync", lambda e: e.dma_start(out=g1rep[:], in_=g.ln1_g.partition_broadcast(128)), w=["g1rep"], chan="d_c")
        P.add("sync", lambda e: e.dma_start(out=b1rep[:], in_=g.ln1_b.partition_broadcast(128)), w=["b1rep"], chan="d_c")
        P.add("sync", lambda e: e.dma_start(out=rbrep[:], in_=g.router_bias.partition_broadcast(128)), w=["rbrep"], chan="d_c")
        P.add("sync", lambda e: e.dma_start(out=iota[:], in_=g.iota256), w=["iota"], chan="d_c")
        P.add("sync", lambda e: e.dma_start(out=identb[:], in_=g.ident), w=["identb"], chan="d_c")
        P.add("gpsimd", lambda e: e.dma_start(out=wr_hi[:], in_=g.w_router.rearrange("(c p) n -> p c n", p=128)),
              w=["wr_hi"], chan="d_wrh")
        P.add("vector", lambda e: e.tensor_tensor(out=wr_lo[:], in0=wr[:], in1=wr_hi[:], op=ALU.subtract),
              r=["wr", "wr_hi"], w=["wr_lo"])
        P.add("sync", lambda e: e.dma_start(out=lstrict[:], in_=g.lstrict), w=["lstrict"], chan="d_c")
        P.add("sync", lambda e: e.dma_start(out=ones[:], in_=g.ones), w=["ones"], chan="d_c")


        pend = [None]
        allk = lambda n: [(n, k) for k in range(8)]

        def xgen(bi, blk):
            b2 = blk % 2
            P.add("sync", lambda e, blk=blk, b2=b2: e.dma_start(
                out=hown[b2][:], in_=g.h_own_d[blk * 128:(blk + 1) * 128, :]), w=[("hown", b2)], chan=f"d_hown{b2}")
            for nh in range(2):
                for oc in range(8):
                    P.add("tensor", lambda e, bi=bi, nh=nh, oc=oc: e.matmul(
                        pmix[:, nh * 512:(nh + 1) * 512], lhsT=mT[:, oc, bi * 128:(bi + 1) * 128],
                        rhs=wo[:, oc, nh * 512:(nh + 1) * 512], start=(oc == 0), stop=(oc == 7)),
                        r=[("mT", oc), ("wo", oc)], w=[("pmix", nh)])
            yield
            for nh in range(2):
                P.add("vector", lambda e, b2=b2, nh=nh: e.scalar_tensor_tensor(
                    out=rr[:, nh * 512:(nh + 1) * 512], in0=hown[b2][:, nh * 512:(nh + 1) * 512], scalar=ALPHA,
                    in1=pmix[:, nh * 512:(nh + 1) * 512], op0=ALU.mult, op1=ALU.add),
                    r=[("hown", b2), ("pmix", nh)], w=[("rr", nh)])
            for hh in range(2):
                P.add("vector", lambda e, hh=hh: e.bn_stats(out=stats[:, hh, :], in_=rr[:, hh * 512:(hh + 1) * 512]),
                      r=[("rr", hh)], w=[("dstats", hh)])
            P.add("vector", lambda e: e.bn_aggr(out=mv[:], in_=stats[:].rearrange("p a b -> p (a b)")),
                  r=[("dstats", 0), ("dstats", 1)], w=["dmv"])
            P.add("scalar", lambda e: e.activation(out=std[:], in_=mv[:, 1:2], func=AF.Sqrt, bias=LN_EPS),
                  r=["dmv"], w=["dstd"])
            yield
            P.add("vector", lambda e: e.reciprocal(out=rstd[:], in_=std[:]), r=["dstd"], w=["drstd"])
            P.add("vector", lambda e, b2=b2: e.tensor_scalar(
                out=h1[b2][:], in0=rr[:], scalar1=mv[:, 0:1], scalar2=rstd[:, 0:1], op0=ALU.subtract, op1=ALU.mult),
                r=[("rr", 0), ("rr", 1), "dmv", "drstd"], w=[("h1", b2)])
            P.add("gpsimd", lambda e, b2=b2: e.tensor_tensor(out=h1[b2][:], in0=h1[b2][:], in1=g1rep[:], op=ALU.mult),
                  r=[("h1", b2), "g1rep"], w=[("h1", b2)])
            P.add("gpsimd", lambda e, b2=b2: e.tensor_tensor(out=h1[b2][:], in0=h1[b2][:], in1=b1rep[:], op=ALU.add),
                  r=[("h1", b2), "b1rep"], w=[("h1", b2)])
            P.add("scalar", lambda e, blk=blk, b2=b2: e.dma_start(
                out=g.h1_d[blk * 128:(blk + 1) * 128, :], in_=h1[b2][:]), r=[("h1", b2)], w=[("h1_d", blk)],
                chan=f"d_h1{b2}")
            P.add("scalar", lambda e, b2=b2: e.activation(out=h1b[b2][:], in_=h1[b2][:], func=AF.Copy),
                  r=[("h1", b2)], w=[("h1b", b2)])
            yield
            P.add("vector", lambda e, b2=b2: e.tensor_tensor(out=h1lo[:], in0=h1[b2][:], in1=h1b[b2][:], op=ALU.subtract),
                  r=[("h1", b2), ("h1b", b2)], w=["h1lo"])
            for dc in range(8):
                P.add("tensor", lambda e, b2=b2, dc=dc: e.transpose(
                    out=ptr[:, dc, :], in_=h1b[b2][:, dc * 128:(dc + 1) * 128], identity=identb[:]),
                    r=[("h1b", b2), "identb"], w=["ptr"])
            for dc in range(8):
                P.add("tensor", lambda e, dc=dc: e.transpose(
                    out=ptr_lo[:, dc, :], in_=h1lo[:, dc * 128:(dc + 1) * 128], identity=identb[:]),
                    r=["h1lo", "identb"], w=["ptr_lo"])
            P.add("scalar", lambda e, b2=b2: e.activation(out=h1Tb[b2][:], in_=ptr[:], func=AF.Copy),
                  r=["ptr"], w=[("h1Tb", b2)])
            P.add("scalar", lambda e, blk=blk, b2=b2: e.dma_start(
                out=g.h1T_d[:, :, blk * 128:(blk + 1) * 128], in_=h1Tb[b2][:]), r=[("h1Tb", b2)],
                w=[("h1T_d", blk)], chan=f"d_h1T{b2}")
            yield
            P.add("vector", lambda e: e.tensor_copy(out=h1Tlo[:], in_=ptr_lo[:]), r=["ptr_lo"], w=["h1Tlo"])
            combos = [(h1Tb[b2], ("h1Tb", b2), wr_hi, "wr_hi"), (h1Tb[b2], ("h1Tb", b2), wr_lo, "wr_lo"),
                      (h1Tlo, "h1Tlo", wr_hi, "wr_hi")]
            for ci, (lt, ltk, rt, rtk) in enumerate(combos):
                for dc in range(8):
                    P.add("tensor", lambda e, dc=dc, lt=lt, rt=rt, ci=ci: e.matmul(
                        pA[:, 0:256], lhsT=lt[:, dc, :], rhs=rt[:, dc, :], start=(ci == 0 and dc == 0),
                        stop=(ci == 2 and dc == 7)), r=[ltk, rtk], w=["pA"])
            P.add("scalar", lambda e, b2=b2: e.activation(out=sc[b2][:], in_=pA[:, 0:256], func=AF.Sigmoid),
                  r=["pA"], w=[("sc", b2)])
            yield
            P.add("vector", lambda e, b2=b2: e.tensor_tensor(out=biased[b2][:], in0=sc[b2][:], in1=rbrep[:], op=ALU.add),
                  r=[("sc", b2), "rbrep"], w=[("biased", b2)])

        def ygen(blk):
            b2 = blk % 2
            bia = biased[b2]
            bk = ("biased", b2)
            sct = sc[b2]
            sck = ("sc", b2)
            for gr in range(8):
                P.add("vector", lambda e, gr=gr: e.max(out=m8g[:, gr, :], in_=bia[:, gr * 32:(gr + 1) * 32]),
                      r=[bk], w=[("m8g", gr)])
            P.add("vector", lambda e: e.tensor_tensor(out=gscore[:], in0=m8g[:, :, 0], in1=m8g[:, :, 1], op=ALU.add),
                  r=[("m8g", gr) for gr in range(8)], w=["gscore"])
            P.add("vector", lambda e: e.max(out=gm8[:], in_=gscore[:]), r=["gscore"], w=["gm8"])
            P.add("vector", lambda e: e.tensor_scalar(
                out=pen[:], in0=gscore[:], scalar1=gm8[:, 3:4], scalar2=1.0e4, op0=ALU.is_lt, op1=ALU.mult),
                r=["gscore", "gm8"], w=["pen"])
            P.add("vector", lambda e: e.tensor_tensor(
                out=masked[:].rearrange("p (a b) -> p a b", b=32), in0=bia[:].rearrange("p (a b) -> p a b", b=32),
                in1=pen[:].unsqueeze(2).to_broadcast([128, 8, 32]), op=ALU.subtract),
                r=[bk, "pen"], w=["masked"])
            P.add("vector", lambda e: e.max(out=t8[:], in_=masked[:]), r=["masked"], w=["t8"])
            P.add("vector", lambda e, blk=blk: e.tensor_scalar(
                out=selb[:, blk, :], in0=masked[:], scalar1=t8[:, 7:8], scalar2=None, op0=ALU.is_ge),
                r=["masked", "t8"], w=[("selb", blk)])
            P.add("tensor", lambda e, blk=blk: e.matmul(
                pB[:, 0:256], lhsT=lstrict[:], rhs=selb[:, blk, :], start=True, stop=(blk == 0)),
                r=["lstrict", ("selb", blk)], w=["pB"])
            for pb_ in range(blk):
                P.add("tensor", lambda e, pb_=pb_, blk=blk: e.matmul(
                    pB[:, 0:256], lhsT=ones[:], rhs=selb[:, pb_, :], start=False, stop=(pb_ == blk - 1)),
                    r=["ones", ("selb", pb_)], w=["pB"])
            yield
            for k in range(8):
                P.add("vector", lambda e, k=k: e.scalar_tensor_tensor(
                    out=junk[:], in0=masked[:], scalar=t8[:, k:k + 1], in1=sct[:], op0=ALU.is_equal, op1=ALU.mult,
                    accum_out=wk[:, k:k + 1]), r=["masked", "t8", sck], w=["junk", ("wk", k)])
                P.add("vector", lambda e, k=k: e.scalar_tensor_tensor(
                    out=junk[:], in0=masked[:], scalar=t8[:, k:k + 1], in1=iota[:], op0=ALU.is_equal,
                    op1=ALU.mult, accum_out=ik[:, k:k + 1]), r=["masked", "t8", "iota"], w=["junk", ("ik", k)])
                if k % 3 == 2:
                    yield
            yield
            for k in range(8):
                P.add("vector", lambda e, k=k: e.scalar_tensor_tensor(
                    out=junk[:], in0=masked[:], scalar=t8[:, k:k + 1], in1=pB[:, 0:256], op0=ALU.is_equal,
                    op1=ALU.mult, accum_out=rk[:, k:k + 1]), r=["masked", "t8", "pB"], w=["junk", ("rk", k)])
            P.add("vector", lambda e: e.tensor_reduce(out=wsum[:], in_=wk[:], axis=AX.X, op=ALU.add),
                  r=allk("wk"), w=["wsum"])
            P.add("vector", lambda e: e.reciprocal(out=wsum[:], in_=wsum[:]), r=["wsum"], w=["wsum"])
            P.add("vector", lambda e, blk=blk: e.tensor_scalar(
                out=g.gk[:, blk, :], in0=wk[:], scalar1=wsum[:, 0:1], scalar2=2.5, op0=ALU.mult, op1=ALU.mult),
                r=allk("wk") + ["wsum"], w=[("gk", blk)])
            P.add("vector", lambda e: e.tensor_scalar(
                out=ovf[:], in0=rk[:], scalar1=float(CAP), scalar2=1.0e6, op0=ALU.is_ge, op1=ALU.mult),
                r=allk("rk"), w=["ovf"])
            P.add("vector", lambda e: e.scalar_tensor_tensor(
                out=sif[:], in0=ik[:], scalar=float(CAP), in1=rk[:], op0=ALU.mult, op1=ALU.add),
                r=allk("ik") + allk("rk"), w=["sif"])
            P.add("vector", lambda e: e.tensor_tensor(out=sif[:], in0=sif[:], in1=ovf[:], op=ALU.add),
                  r=["sif", "ovf"], w=["sif"])
            P.add("vector", lambda e, blk=blk: e.tensor_copy(out=g.sidx[:, blk, :], in_=sif[:]),
                  r=["sif"], w=[("sidx", blk)])
            for k in range(8):
                P.add("gpsimd", lambda e, blk=blk, k=k, b2=b2: e.indirect_dma_start(
                    out=g.xs_d[:, :], out_offset=bass.IndirectOffsetOnAxis(ap=g.sidx[:, blk, k:k + 1], axis=0),
                    in_=h1b[b2][:, :], in_offset=None, bounds_check=g.bcreg[0], oob_is_err=False),
                    r=[("h1b", b2), ("sidx", blk)], w=[("xs_d", blk, k)], chan=f"d_sc{b2}")

        for gq in qgroups:
            t0 = gq * 512
            P.add("sync", lambda e, t0=t0: e.dma_start(out=hTo[:], in_=g.hT_own_d[:, :, t0:t0 + 512]),
                  w=["hTo"], chan="d_hTo")
            P.add("sync", lambda e, t0=t0: e.dma_start(
                out=osb[:], in_=g.osbT_d[:, :, t0:t0 + 512].rearrange("h p t -> p h t")), w=["osb"], chan="d_osb")
            P.add("sync", lambda e, t0=t0: e.dma_start(
                out=odl[:], in_=g.odlT_d[:, :, t0:t0 + 512].rearrange("c p t -> p c t")), w=["odl"], chan="d_odl")
            for oc in range(8):
                for dc in range(8):
                    P.add("tensor", lambda e, oc=oc, dc=dc: e.matmul(
                        pA[:], lhsT=wg[:, dc, oc * 128:(oc + 1) * 128], rhs=hTo[:, dc, :],
                        start=(dc == 0), stop=(dc == 7)), r=[("wg", dc), "hTo"], w=["pA"])
                for dc in range(8):
                    P.add("tensor", lambda e, oc=oc, dc=dc: e.matmul(
                        pB[:], lhsT=wg[:, dc, 1024 + oc * 128:1024 + (oc + 1) * 128], rhs=hTo[:, dc, :],
                        start=(dc == 0), stop=(dc == 7)), r=[("wg", dc), "hTo"], w=["pB"])
                for h in range(8):
                    P.add("tensor", lambda e, oc=oc, h=h: e.matmul(
                        pC[:], lhsT=wbs[:, h, oc * 128:(oc + 1) * 128], rhs=osb[:, h, :],
                        start=(h == 0), stop=(h == 7)), r=["wbs", "osb"], w=["pC"])
                for c2 in range(2):
                    P.add("tensor", lambda e, oc=oc, c2=c2: e.matmul(
                        pD[:], lhsT=wbd[:, c2, oc * 128:(oc + 1) * 128], rhs=odl[:, c2, :],
                        start=(c2 == 0), stop=(c2 == 1)), r=["wbd", "odl"], w=["pD"])
                P.add("scalar", lambda e, oc=oc: e.activation(
                    out=gs[:], in_=pA[:], func=AF.Sigmoid, bias=bgT[:, oc:oc + 1]), r=["pA", "bgT"], w=["gs"])
                P.add("scalar", lambda e, oc=oc: e.activation(
                    out=gd[:], in_=pB[:], func=AF.Sigmoid, bias=bgT[:, 8 + oc:9 + oc]), r=["pB", "bgT"], w=["gd"])
                P.add("vector", lambda e: e.tensor_tensor(out=m1[:], in0=gs[:], in1=pC[:], op=ALU.mult),
                      r=["gs", "pC"], w=["m1"])
                P.add("vector", lambda e: e.tensor_tensor(out=m2[:], in0=gd[:], in1=pD[:], op=ALU.mult),
                      r=["gd", "pD"], w=["m2"])
                P.add("gpsimd", lambda e, oc=oc: e.tensor_tensor(out=mT[:, oc, :], in0=m1[:], in1=m2[:], op=ALU.add),
                      r=["m1", "m2"], w=[("mT", oc)])
            for bi in range(4):
                blk = gq * 4 + bi
                xg = xgen(bi, blk)
                yg = pend[0]
                xa, ya = True, yg is not None
                while xa or ya:
                    if xa:
                        try:
                            next(xg)
                        except StopIteration:
                            xa = False
                    if ya:
                        try:
                            next(yg)
                        except StopIteration:
                            ya = False
                pend[0] = ygen(blk)
        if pend[0] is not None:
            for _ in pend[0]:
                pass
        P.emit_phase()


def _layer_norm(P, x, xk0, xk1, stats, mv, std, rstd, out, outk, grep, gk_, brep, bk_, tag):
    for hh, xk in ((0, xk0), (1, xk1)):
        P.add("vector", lambda e, hh=hh: e.bn_stats(out=stats[:, hh, :], in_=x[:, hh * 512:(hh + 1) * 512]),
              r=[xk], w=[(tag + "stats", hh)])
    P.add("vector", lambda e: e.bn_aggr(out=mv[:], in_=stats[:].rearrange("p a b -> p (a b)")),
          r=[(tag + "stats", 0), (tag + "stats", 1)], w=[tag + "mv"])
    P.add("scalar", lambda e: e.activation(out=std[:], in_=mv[:, 1:2], func=AF.Sqrt, bias=LN_EPS),
          r=[tag + "mv"], w=[tag + "std"])
    P.add("vector", lambda e: e.reciprocal(out=rstd[:], in_=std[:]), r=[tag + "std"], w=[tag + "rstd"])
    P.add("vector", lambda e: e.tensor_scalar(
        out=out[:], in0=x[:], scalar1=mv[:, 0:1], scalar2=rstd[:, 0:1], op0=ALU.subtract, op1=ALU.mult),
        r=[xk0, xk1, tag + "mv", tag + "rstd"], w=[outk])
    P.add("gpsimd", lambda e: e.tensor_tensor(out=out[:], in0=out[:], in1=grep[:], op=ALU.mult),
          r=[outk, gk_], w=[outk])
    P.add("gpsimd", lambda e: e.tensor_tensor(out=out[:], in0=out[:], in1=brep[:], op=ALU.add),
          r=[outk, bk_], w=[outk])


def _swiglu_block(P, xT, xTk, wg_t, wu_t, wd_t, wkeys, pgu, pguk, sg, sgk, hid, hidk, py, pyk):
    for j, wt in enumerate((wg_t, wu_t)):
        for hh in range(2):
            for dc in range(8):
                P.add("tensor", lambda e, j=j, wt=wt, hh=hh, dc=dc: e.matmul(
                    pgu[:, 2 * j + hh, :], lhsT=wt[:, dc, hh * 128:(hh + 1) * 128], rhs=xT[:, dc, :],
                    start=(dc == 0), stop=(dc == 7)), r=[wkeys[j], xTk], w=[pguk])
    P.add("scalar", lambda e: e.activation(out=sg[:], in_=pgu[:, 0:2, :], func=AF.Silu), r=[pguk], w=[sgk])
    P.add("vector", lambda e: e.tensor_tensor(out=hid[:], in0=sg[:], in1=pgu[:, 2:4, :], op=ALU.mult),
          r=[sgk, pguk], w=[hidk])
    for nh in range(2):
        for hh in range(2):
            P.add("tensor", lambda e, nh=nh, hh=hh: e.matmul(
                py[:, nh * 512:(nh + 1) * 512], lhsT=hid[:, hh, :], rhs=wd_t[:, hh, nh * 512:(nh + 1) * 512],
                start=(hh == 0), stop=(hh == 1)), r=[hidk, wkeys[2]], w=[pyk + (nh,)])


def phase_f(P, nc, g, experts=tuple(range(NEXP))):
    st = contextlib.ExitStack()
    with st:
        sb = lambda name, shape, dt: st.enter_context(nc.sbuf_tensor(name, shape, dt))
        ps = lambda name, shape, dt: st.enter_context(nc.psum_tensor(name, shape, dt))
        NS = 3
        ident = sb("f_ident", [128, 128], BF16)
        xs = [sb(f"f_xs{i}", [128, 2, 1024], BF16) for i in range(3)]
        wg32 = [sb(f"f_wg32_{i}", [128, 8, 256], F32) for i in range(NS)]
        wu32 = [sb(f"f_wu32_{i}", [128, 8, 256], F32) for i in range(NS)]
        wd32 = [sb(f"f_wd32_{i}", [128, 2, 1024], F32) for i in range(NS)]
        wg_ = [sb(f"f_wg{i}", [128, 8, 256], BF16) for i in range(2)]
        wu_ = [sb(f"f_wu{i}", [128, 8, 256], BF16) for i in range(2)]
        wd_ = [sb(f"f_wd{i}", [128, 2, 1024], BF16) for i in range(2)]
        xT = [sb(f"f_xT{i}", [128, 8, 128], BF16) for i in range(3)]
        sg = [sb(f"f_sg{i}", [128, 2, 128], F32) for i in range(2)]
        hid = [sb(f"f_hid{i}", [128, 2, 128], BF16) for i in range(2)]
        ys = [sb(f"f_ys{i}", [128, 2, 1024], BF16) for i in range(2)]
        ptr = [ps(f"f_ptr{i}", [128, 8, 128], BF16) for i in range(2)]
        pgu = [ps(f"f_pgu{i}", [128, 4, 128], F32) for i in range(2)]
        py = [ps(f"f_py{i}", [128, 1024], F32) for i in range(2)]
        P.add("sync", lambda e: e.dma_start(out=ident[:], in_=g.ident), w=["ident"], chan="f_c")
        ne = len(experts)

        def load(n_):
            ex = experts[n_]
            b3 = n_ % NS
            P.add("sync", lambda e, ex=ex, b3=b3: e.dma_start(
                out=wg32[b3][:], in_=g.w_gate_e[ex].rearrange("(p c) n -> p c n", p=128)), w=[("wg32", b3)],
                chan=f"f_wg{b3}")
            P.add("scalar", lambda e, ex=ex, b3=b3: e.dma_start(
                out=wu32[b3][:], in_=g.w_up_e[ex].rearrange("(p c) n -> p c n", p=128)), w=[("wu32", b3)],
                chan=f"f_wu{b3}")
            P.add("sync", lambda e, ex=ex, b3=b3: e.dma_start(
                out=wd32[b3][:], in_=g.w_down_e[ex].rearrange("(p c) n -> p c n", p=128)), w=[("wd32", b3)],
                chan=f"f_wd{b3}")

        def cast(n_):
            b3 = n_ % NS
            b2 = n_ % 2
            P.add("scalar", lambda e, b3=b3, b2=b2: e.activation(out=wg_[b2][:], in_=wg32[b3][:], func=AF.Copy),
                  r=[("wg32", b3)], w=[("wg", b2)])
            P.add("vector", lambda e, b3=b3, b2=b2: e.tensor_copy(out=wu_[b2][:], in_=wu32[b3][:]),
                  r=[("wu32", b3)], w=[("wu", b2)])
            P.add("scalar", lambda e, b3=b3, b2=b2: e.activation(out=wd_[b2][:, 0, :], in_=wd32[b3][:, 0, :], func=AF.Copy),
                  r=[("wd32", b3)], w=[("wd", b2, 0)])
            P.add("vector", lambda e, b3=b3, b2=b2: e.tensor_copy(out=wd_[b2][:, 1, :], in_=wd32[b3][:, 1, :]),
                  r=[("wd32", b3)], w=[("wd", b2, 1)])

        NH = CAP // 128
        items = [(n_, half) for n_ in range(ne) for half in range(NH)]

        def st_lx(n_):
            x3 = n_ % 3
            r0 = experts[n_] * CAP
            P.add("sync", lambda e, r0=r0, x3=x3: e.dma_start(
                out=xs[x3][:], in_=g.xs_d[r0:r0 + CAP, :].rearrange("(p h) d -> p h d", h=2)),
                w=[("xs", x3)], chan=f"f_xs{x3}")

        def st_t(i):
            n_, half = items[i]
            x3 = i % 3
            xe = n_ % 3
            b2 = i % 2
            for dc in range(8):
                P.add("tensor", lambda e, dc=dc, b2=b2, xe=xe, half=half: e.transpose(
                    out=ptr[b2][:, dc, :], in_=xs[xe][:, half, :].rearrange("t (p c) -> t c p", c=8)[:, dc, :],
                    identity=ident[:]),
                    r=[("xs", xe), "ident"], w=[("ptr", b2)])
            P.add("vector", lambda e, b2=b2, x3=x3: e.tensor_copy(out=xT[x3][:], in_=ptr[b2][:]),
                  r=[("ptr", b2)], w=[("xT", x3)])

        def st_gu(i):
            n_, half = items[i]
            x3 = i % 3
            b2 = i % 2
            wb = n_ % 2
            for j, wt in enumerate((wg_[wb], wu_[wb])):
                wk_ = ("wg", wb) if j == 0 else ("wu", wb)
                for hh in range(2):
                    for dc in range(8):
                        P.add("tensor", lambda e, j=j, wt=wt, hh=hh, dc=dc, b2=b2, x3=x3: e.matmul(
                            pgu[b2][:, 2 * j + hh, :], lhsT=wt[:, dc, :].rearrange("p (m h) -> p h m", h=2)[:, hh, :],
                            rhs=xT[x3][:, dc, :],
                            start=(dc == 0), stop=(dc == 7)), r=[wk_, ("xT", x3)], w=[("pgu", b2)])
            P.add("scalar", lambda e, b2=b2: e.activation(out=sg[b2][:], in_=pgu[b2][:, 0:2, :], func=AF.Silu),
                  r=[("pgu", b2)], w=[("sg", b2)])
            P.add("vector", lambda e, b2=b2: e.tensor_tensor(out=hid[b2][:], in0=sg[b2][:], in1=pgu[b2][:, 2:4, :],
                                                            op=ALU.mult),
                  r=[("sg", b2), ("pgu", b2)], w=[("hid", b2)])

        def st_dn(i):
            n_, half = items[i]
            b2 = i % 2
            wb = n_ % 2
            r0 = experts[n_] * CAP + half * 128
            for nh in range(2):
                for hh in range(2):
                    P.add("tensor", lambda e, nh=nh, hh=hh, b2=b2, wb=wb: e.matmul(
                        py[b2][:, nh * 512:(nh + 1) * 512], lhsT=hid[b2][:, hh, :],
                        rhs=wd_[wb][:, hh, nh * 512:(nh + 1) * 512], start=(hh == 0), stop=(hh == 1)),
                        r=[("hid", b2), ("wd", wb, hh)], w=[("py", b2, nh)])
            y2 = n_ % 2
            P.add("scalar", lambda e, b2=b2, y2=y2, half=half: e.activation(
                out=ys[y2][:, half, 0:512], in_=py[b2][:, 0:512], func=AF.Copy),
                r=[("py", b2, 0)], w=[("ys", y2, half, 0)])
            P.add("vector", lambda e, b2=b2, y2=y2, half=half: e.tensor_copy(
                out=ys[y2][:, half, 512:1024], in_=py[b2][:, 512:1024]),
                r=[("py", b2, 1)], w=[("ys", y2, half, 1)])
            if half == NH - 1:
                rbase = experts[n_] * CAP
                P.add("gpsimd", lambda e, rbase=rbase, y2=y2: e.dma_start(
                    out=g.ys_d[rbase:rbase + CAP, :].rearrange("(p h) d -> p h d", h=2), in_=ys[y2][:]),
                    r=[("ys", y2, h_, q_) for h_ in range(2) for q_ in range(2)], w=[("ys_d", rbase)],
                    chan=f"f_ys{y2}")

        load(0)
        if ne > 1:
            load(1)
        cast(0)
        ni = len(items)
        st_lx(0)
        if ne > 1:
            st_lx(1)
        for step in range(ni + 2):
            if step < ni:
                n_t, half_t = items[step]
                if half_t == 0 and n_t + 2 < ne:
                    st_lx(n_t + 2)
                st_t(step)
            i1 = step - 1
            if 0 <= i1 < ni:
                n_, half = items[i1]
                if half == 0 and n_ + 2 < ne:
                    load(n_ + 2)
                st_gu(i1)
                if half == NH - 1 and n_ + 1 < ne:
                    cast(n_ + 1)
            i2 = step - 2
            if 0 <= i2 < ni:
                st_dn(i2)
        P.emit_phase()


def phase_g(P, nc, g, blocks=tuple(range(NOWN))):
    st = contextlib.ExitStack()
    with st:
        sb = lambda name, shape, dt: st.enter_context(nc.sbuf_tensor(name, shape, dt))
        ps = lambda name, shape, dt: st.enter_context(nc.psum_tensor(name, shape, dt))
        wgs = sb("g_wgs", [128, 8, 256], BF16)
        wus = sb("g_wus", [128, 8, 256], BF16)
        wds = sb("g_wds", [128, 2, 1024], BF16)
        g2rep = sb("g_g2", [128, 1024], F32)
        b2rep = sb("g_b2", [128, 1024], F32)
        h1 = [sb(f"g_h1_{i}", [128, 1024], F32) for i in range(2)]
        h1T = [sb(f"g_h1T{i}", [128, 8, 128], BF16) for i in range(2)]
        yk = [sb(f"g_yk{i}", [128, 1024], BF16) for i in range(6)]
        acc = sb("g_acc", [128, 1024], F32)
        sg = sb("g_sg", [128, 2, 128], F32)
        hid = sb("g_hid", [128, 2, 128], BF16)
        ot = [sb(f"g_ot{i}", [128, 1024], F32) for i in range(2)]
        stats = sb("g_stats", [128, 2, 6], F32)
        mv = sb("g_mv", [128, 2], F32)
        std = sb("g_std", [128, 1], F32)
        rstd = sb("g_rstd", [128, 1], F32)
        pgu = ps("g_pgu", [128, 4, 128], F32)
        py = ps("g_py", [128, 1024], F32)
        _bounds_reg(P, g)
        P.add("gpsimd", lambda e: e.dma_start(out=wgs[:], in_=g.w_gate_s.rearrange("(c p) n -> p c n", p=128)),
              w=["wgs"], chan="g_w")
        P.add("gpsimd", lambda e: e.dma_start(out=wus[:], in_=g.w_up_s.rearrange("(c p) n -> p c n", p=128)),
              w=["wus"], chan="g_w")
        P.add("gpsimd", lambda e: e.dma_start(out=wds[:], in_=g.w_down_s.rearrange("(c p) n -> p c n", p=128)),
              w=["wds"], chan="g_w")
        P.add("sync", lambda e: e.dma_start(out=g2rep[:], in_=g.ln2_g.partition_broadcast(128)), w=["g2rep"], chan="g_c")
        P.add("sync", lambda e: e.dma_start(out=b2rep[:], in_=g.ln2_b.partition_broadcast(128)), w=["b2rep"], chan="g_c")
        for i in range(6):
            P.add("gpsimd", lambda e, i=i: e.memset(yk[i][:], 0.0), w=[("yk", i)])
        yi = 0
        for blk in blocks:
            b2 = blk % 2
            P.add("sync", lambda e, blk=blk, b2=b2: e.dma_start(out=h1[b2][:], in_=g.h1_d[blk * 128:(blk + 1) * 128, :]),
                  w=[("h1", b2)], chan=f"g_h1{b2}")
            P.add("sync", lambda e, blk=blk, b2=b2: e.dma_start(out=h1T[b2][:], in_=g.h1T_d[:, :, blk * 128:(blk + 1) * 128]),
                  w=[("h1T", b2)], chan=f"g_h1T{b2}")
            for k in range(8):
                y3 = yi % 6
                yi += 1
                P.add("gpsimd", lambda e, blk=blk, k=k, y3=y3: e.indirect_dma_start(
                    out=yk[y3][:, :], out_offset=None, in_=g.ys_d[:, :],
                    in_offset=bass.IndirectOffsetOnAxis(ap=g.sidx[:, blk, k:k + 1], axis=0),
                    bounds_check=g.bcreg[0], oob_is_err=False),
                    r=[("sidx", blk)], w=[("yk", y3)], chan=f"g_yk{y3}")
                if k == 0:
                    P.add("vector", lambda e, blk=blk, y3=y3: e.tensor_scalar(
                        out=acc[:], in0=yk[y3][:], scalar1=g.gk[:, blk, 0:1], scalar2=None, op0=ALU.mult),
                        r=[("yk", y3), ("gk", blk)], w=["acc"])
                else:
                    P.add("vector", lambda e, blk=blk, k=k, y3=y3: e.scalar_tensor_tensor(
                        out=acc[:], in0=yk[y3][:], scalar=g.gk[:, blk, k:k + 1], in1=acc[:], op0=ALU.mult, op1=ALU.add),
                        r=[("yk", y3), ("gk", blk), "acc"], w=["acc"])
            _swiglu_block(P, h1T[b2], ("h1T", b2), wgs, wus, wds, ["wgs", "wus", "wds"],
                          pgu, "pgu", sg, "sg", hid, "hid", py, ("py",))
            o_t = ot[b2]
            P.add("vector", lambda e, b2=b2: e.scalar_tensor_tensor(
                out=acc[:], in0=h1[b2][:], scalar=ALPHA, in1=acc[:], op0=ALU.mult, op1=ALU.add),
                r=[("h1", b2), "acc"], w=["acc"])
            for nh in range(2):
                P.add("vector", lambda e, nh=nh: e.tensor_tensor(
                    out=acc[:, nh * 512:(nh + 1) * 512], in0=acc[:, nh * 512:(nh + 1) * 512],
                    in1=py[:, nh * 512:(nh + 1) * 512], op=ALU.add), r=["acc", ("py", nh)], w=["acc"])
            _layer_norm(P, acc, "acc", "acc", stats, mv, std, rstd, o_t, ("ot", b2), g2rep, "g2rep", b2rep, "b2rep", "g")
            P.add("scalar", lambda e, blk=blk, o_t=o_t: e.dma_start(out=g.out[blk * 128:(blk + 1) * 128, :], in_=o_t[:]),
                  r=[("ot", b2)], w=[("out", blk)], chan=f"g_out{b2}")
        P.emit_phase()


def build_program(debug=None, ntg=16, sb_groups=(0, 1, 2, 3), dl_slots=tuple(range(NOWN)), phases="abcdfg", d_groups=(0, 1, 2, 3),
                  f_experts=tuple(range(NEXP)), g_blocks=tuple(range(NOWN)), d_stop=9):
    nc = bass.Bass("TRN2", target_bir_lowering=False)
    g = Ctx()
    g.bcreg = []

    def din(name, shape, dt=F32):
        return nc.dram_tensor(name, shape, dt, kind="ExternalInput").ap()

    dbgnames = set(debug or ())

    def dscr(name, shape, dt):
        kind = "ExternalOutput" if name in dbgnames else "Internal"
        return nc.dram_tensor(name, shape, dt, kind=kind).ap()

    g.x_ctx = din("x_ctx", [NB * 128, D])
    g.valid = din("valid", [128, NB])
    g.ident = din("ident", [128, 128], BF16)
    g.negU = din("negU", [128, 128], BF16)
    g.negOnes = din("negOnes", [128, 128], BF16)
    g.sbmask = din("sbmask", [128, 16, 512], BF16)
    g.dlbias = din("dlbias", [128, DL_NT, 128], BF16)
    g.padbias = din("padbias", [128, NB])
    g.w_br_sb = din("w_br_sb", [512, D])
    g.w_br_dil = din("w_br_dil", [256, D])
    g.w_out = din("w_out", [D, D])
    g.w_router = din("w_router", [D, NEXP])
    g.b_gateT = din("b_gateT", [128, 16])
    g.ln1_g = din("ln1_g", [D])
    g.ln1_b = din("ln1_b", [D])
    g.router_bias = din("router_bias", [NEXP])
    ne_decl = NEXP if "f" in phases else 1
    g.w_gate_e = din("w_gate_e", [ne_decl, D, 256])
    g.w_up_e = din("w_up_e", [ne_decl, D, 256])
    g.w_down_e = din("w_down_e", [ne_decl, 256, D])
    g.w_gate_s = din("w_gate_s", [D, 256])
    g.w_up_s = din("w_up_s", [D, 256])
    g.w_down_s = din("w_down_s", [256, D])
    g.ln2_g = din("ln2_g", [D])
    g.ln2_b = din("ln2_b", [D])
    g.iota256 = din("iota256", [128, 256])
    g.lstrict = din("lstrict", [128, 128], BF16)
    g.ones = din("ones", [128, 128], BF16)
    g.ln_in_g = din("ln_in_g", [D])
    g.ln_in_b = din("ln_in_b", [D])
    g.ln_in_gT = din("ln_in_gT", [128, 8])
    g.ln_in_bT = din("ln_in_bT", [128, 8])
    g.w_in = din("w_in", [D, 5888])
    g.out = nc.dram_tensor("out", [TOWN, D], F32, kind="ExternalOutput").ap()

    g.kT_d = dscr("kT_d", [10, 128, S], BF16)
    g.v_d = dscr("v_d", [S, VW], BF16)
    g.qT_d = dscr("qT_d", [128, 10, TOWN], BF16)
    g.osbT_d = dscr("osbT_d", [8, 64, TOWN], BF16)
    g.odlT_d = dscr("odlT_d", [2, 128, TOWN], BF16)
    g.h_own_d = dscr("h_own_d", [TOWN, D], F32)
    g.h1_d = dscr("h1_d", [TOWN, D], F32)
    g.h1T_d = dscr("h1T_d", [128, 8, TOWN], BF16)
    g.xs_d = dscr("xs_d", [NEXP * CAP, D], BF16)
    g.ys_d = dscr("ys_d", [NEXP * CAP, D], BF16)
    g.hT_own_d = dscr("hT_own_d", [128, 8, TOWN], BF16)

    with contextlib.ExitStack() as stack:
        P = Prog(nc, stack)
        g.gk = stack.enter_context(nc.sbuf_tensor("gk", [128, NOWN, 8], F32))
        g.sidx = stack.enter_context(nc.sbuf_tensor("sidx", [128, NOWN, 8], I32))
        if "a" in phases:
            phase_a(P, nc, g, ntg)
        if "b" in phases:
            phase_b(P, nc, g, sb_groups)
        if "c" in phases:
            phase_c(P, nc, g, dl_slots)
        if "d" in phases:
            phase_d(P, nc, g, d_groups, d_stop)
        if "f" in phases:
            phase_f(P, nc, g, f_experts)
        if "g" in phases:
            phase_g(P, nc, g, g_blocks)
        if "gk" in dbgnames:
            dgk = nc.dram_tensor("dbg_gk", [128, NOWN, 8], F32, kind="ExternalOutput").ap()
            dsi = nc.dram_tensor("dbg_sidx", [128, NOWN, 8], I32, kind="ExternalOutput").ap()
            nbk = 4 * len(d_groups)
            P.add("sync", lambda e: e.dma_start(out=dgk[:, 0:nbk, :], in_=g.gk[:, 0:nbk, :]), w=["dgk"], chan="dbg")
            P.add("sync", lambda e: e.dma_start(out=dsi[:, 0:nbk, :], in_=g.sidx[:, 0:nbk, :]), w=["dsi"], chan="dbg")
            P.emit_phase()
    return nc


def _rel_bucket_np(dist):
    dist = np.asarray(dist, np.int64)
    max_exact = 16
    d = np.maximum(dist, 1).astype(np.float32)
    large = max_exact + (np.log(d / np.float32(max_exact)) / np.float32(np.log(2048 / 16))
                         * np.float32(32 - max_exact)).astype(np.int32)
    large = np.minimum(large, 31)
    return np.where(dist < max_exact, dist, large)


def host_consts(inputs):
    bf = ml_dtypes.bfloat16
    kl = np.arange(128)[:, None]
    ql = np.arange(128)[None, :]
    ident = np.eye(128, dtype=np.float32).astype(bf)
    negU = np.where(kl >= ql, -1.0, 0.0).astype(np.float32).astype(bf)
    negOnes = np.full((128, 128), -1.0, np.float32).astype(bf)
    sbmask = np.zeros((128, 16, 512), np.float32)
    for rel_c in range(16):
        for sl in range(4):
            rel_cq = 4 * sl + 3
            if rel_c == rel_cq:
                sbmask[:, rel_c, sl * 128:(sl + 1) * 128] = np.where(kl < ql, 0.0, NEG)
            elif rel_c > rel_cq:
                sbmask[:, rel_c, sl * 128:(sl + 1) * 128] = NEG
    rel_bias = np.asarray(inputs["rel_bias"], np.float32)
    dlbias = np.zeros((128, DL_NT, 128), np.float32)
    for gi, (w, dil) in enumerate(DIL):
        for hg in range(4):
            for o in range(DL_NB[gi]):
                dist = 128 * o + ql - kl
                ok = (dist >= 0) & (dist <= w) & (dist % dil == 0)
                bk = _rel_bucket_np(np.clip(dist, 0, None))
                val = rel_bias[bk, 4 * gi + hg]
                dlbias[:, DL_TOFF[gi] + hg * DL_NB[gi] + o, :] = np.where(ok, val, NEG)
    f32 = lambda k: np.ascontiguousarray(np.asarray(inputs[k], np.float32)[0])
    return dict(ident=ident, negU=negU, negOnes=negOnes, sbmask=sbmask.astype(bf), dlbias=dlbias.astype(bf),
                w_br_sb=f32("w_br_sb"), w_br_dil=f32("w_br_dil"), w_out=f32("w_out"), w_router=f32("w_router"),
                b_gateT=np.ascontiguousarray(f32("b_gate").reshape(16, 128).T),
                ln1_g=f32("ln1_g"), ln1_b=f32("ln1_b"), router_bias=f32("router_bias"),
                w_gate_e=f32("w_gate_e"), w_up_e=f32("w_up_e"), w_down_e=f32("w_down_e"),
                w_gate_s=f32("w_gate_s"), w_up_s=f32("w_up_s"), w_down_s=f32("w_down_s"),
                ln2_g=f32("ln2_g"), ln2_b=f32("ln2_b"),
                iota256=np.tile(np.arange(256, dtype=np.float32)[None, :], (128, 1)),
                lstrict=np.where(kl < ql, 1.0, 0.0).astype(np.float32).astype(bf),
                ones=np.ones((128, 128), np.float32).astype(bf))


def host_inputs(inputs):
    x = np.asarray(inputs["x"], dtype=np.float32)
    maps = []
    consts = host_consts(inputs)
    for core in range(NCORES):
        b, j = core // 4, core % 4
        xc = np.zeros((NB, 128, D), np.float32)
        valid = np.zeros((128, NB), np.float32)
        xb = x[b].reshape(64, 128, D)
        for c in range(NB):
            gb = c + j - 3
            if gb >= 0:
                xc[c] = xb[gb]
                valid[:, c] = 1.0
        m = {
            "x_ctx": xc.reshape(NB * 128, D),
            "valid": valid,
            "padbias": np.where(valid > 0, 0.0, NEG).astype(np.float32),
            "ln_in_g": np.asarray(inputs["ln_in_g"], np.float32),
            "ln_in_b": np.asarray(inputs["ln_in_b"], np.float32),
            "ln_in_gT": np.ascontiguousarray(np.asarray(inputs["ln_in_g"], np.float32).reshape(8, 128).T),
            "ln_in_bT": np.ascontiguousarray(np.asarray(inputs["ln_in_b"], np.float32).reshape(8, 128).T),
            "w_in": np.ascontiguousarray(np.asarray(inputs["w_in"], np.float32)[0]),
        }
        m.update(consts)
        maps.append(m)
    return maps


def kernel(**inputs):
    nc = build_program()
    maps = host_inputs(inputs)
    res = run_bass_kernel_spmd(nc, maps, core_ids=list(range(NCORES)))
    out = np.zeros((2, S, D), np.float32)
    for core in range(NCORES):
        b, j = core // 4, core % 4
        o = res.results[core]["out"].reshape(NOWN, 128, D)
        ob = out[b].reshape(64, 128, D)
        for s in range(NOWN):
            ob[4 * s + j] = o[s]
    return out
```

```python
import contextlib
import numpy as np
import ml_dtypes
import concourse.bass as bass
import concourse.mybir as mybir
from concourse.bass_utils import run_bass_kernel_spmd

F32 = mybir.dt.float32
BF16 = mybir.dt.bfloat16
I32 = mybir.dt.int32
U32 = mybir.dt.uint32
AF = mybir.ActivationFunctionType
ALU = mybir.AluOpType
AX = mybir.AxisListType

NCORES = 8
D = 1024
S = 8192
NB = 64
NOWN = 16
TOWN = NOWN * 128
LN_EPS = 1e-5
ALPHA = 2.0 ** 0.25
NEG = -30000.0
NEXP = 256
CAP = 256
TOPK = 8
VW = 512 + 12 * 65


class Op:
    __slots__ = ("eng", "fn", "deps", "chan", "sig", "count", "idx", "chan_count")


class Prog:
    ENGS = ("tensor", "vector", "scalar", "gpsimd", "sync")

    def __init__(self, nc, stack):
        self.nc = nc
        self.stack = stack
        self.sems = {e: stack.enter_context(nc.semaphore("sem_" + e)) for e in self.ENGS}
        self.sig_total = {e: 0 for e in self.ENGS}
        self.chan_sem = {}
        self.chan_total = {}
        self.reset_phase()

    def reset_phase(self):
        self.ops = []
        self.last_w = {}
        self.readers = {}
        self.chan_emitted = dict(self.chan_total)

    def chan(self, name):
        if name not in self.chan_sem:
            self.chan_sem[name] = self.stack.enter_context(self.nc.semaphore("ch_" + name))
            self.chan_total[name] = 0
            self.chan_emitted[name] = 0
        return name

    def add(self, eng, fn, r=(), w=(), chan=None):
        op = Op()
        op.eng = eng
        op.fn = fn
        op.chan = chan
        op.sig = False
        op.idx = len(self.ops)
        deps = set()
        for k in r:
            lw = self.last_w.get(k)
            if lw is not None:
                deps.add(lw)
        for k in w:
            lw = self.last_w.get(k)
            if lw is not None:
                deps.add(lw)
            for rd in self.readers.get(k, ()):
                deps.add(rd)
        deps.discard(op.idx)
        op.deps = []
        for d in deps:
            dop = self.ops[d]
            if dop.chan is not None:
                op.deps.append(("chan", dop.chan, self.chan_emitted[dop.chan]))
            else:
                if dop.eng == "tensor" and eng == "tensor":
                    continue
                dop.sig = True
                op.deps.append(("eng", dop.eng, dop))
        if chan is not None:
            self.chan(chan)
            self.chan_emitted[chan] += 16
            op.chan_count = self.chan_emitted[chan]
        self.ops.append(op)
        for k in r:
            self.readers.setdefault(k, []).append(op.idx)
        for k in w:
            self.last_w[k] = op.idx
            self.readers[k] = []
        return op

    def emit_phase(self):
        nc = self.nc
        last = {}
        for op in self.ops:
            if op.chan is None:
                last[op.eng] = op
        for op in last.values():
            op.sig = True
        tot = dict(self.sig_total)
        for op in self.ops:
            if op.chan is None and op.sig:
                tot[op.eng] += 1
                op.count = tot[op.eng]
        final_eng = dict(tot)
        final_chan = dict(self.chan_emitted)
        per_eng = {e: [o for o in self.ops if o.eng == e] for e in self.ENGS}
        sems = self.sems
        chan_sem = self.chan_sem

        def run(e, eobj):
            waited = {}

            def wait(kind, name, val):
                key = (kind, name)
                if waited.get(key, -1) >= val:
                    return
                waited[key] = val
                eobj.wait_ge(sems[name] if kind == "eng" else chan_sem[name], val)

            for op in per_eng[e]:
                for kind, name, v in op.deps:
                    wait(kind, name, v.count if kind == "eng" else v)
                ins = op.fn(eobj)
                if op.chan is not None:
                    ins.then_inc(chan_sem[op.chan], 16)
                elif op.sig:
                    ins.then_inc(sems[e], 1)
            for name, v in final_chan.items():
                if v > 0:
                    wait("chan", name, v)
            for name, v in final_eng.items():
                if v > 0 and name != e:
                    wait("eng", name, v)

        with nc.Block() as block:
            @block.tensor
            def _(e):
                run("tensor", e)

            @block.vector
            def _(e):
                run("vector", e)

            @block.scalar
            def _(e):
                run("scalar", e)

            @block.gpsimd
            def _(e):
                run("gpsimd", e)

            @block.sync
            def _(e):
                run("sync", e)

        self.sig_total = final_eng
        self.chan_total = final_chan
        self.reset_phase()


class Ctx:
    pass


def phase_a(P, nc, g, ntg=16):
    st = contextlib.ExitStack()
    with st:
        sb = lambda name, shape, dt: st.enter_context(nc.sbuf_tensor(name, shape, dt))
        ps = lambda name, shape, dt: st.enter_context(nc.psum_tensor(name, shape, dt))
        winb = sb("a_winb", [128, 8, 3840], BF16)
        gT = sb("a_gT", [128, 8], F32)
        bT = sb("a_bT", [128, 8], F32)
        grep = sb("a_grep", [128, 1024], F32)
        brep = sb("a_brep", [128, 1024], F32)
        valid = sb("a_valid", [128, NB], F32)
        ident = sb("a_ident", [128, 128], BF16)
        NX = 3
        xt = [sb(f"a_x{i}", [128, 1024], F32) for i in range(NX)]
        stats = [sb(f"a_stats{i}", [128, 2, 6], F32) for i in range(2)]
        mv = [sb(f"a_mv{i}", [128, 2], F32) for i in range(2)]
        rstd = [sb(f"a_rstd{i}", [128, 1], F32) for i in range(2)]
        std = [sb(f"a_std{i}", [128, 1], F32) for i in range(2)]
        ybf = [sb(f"a_ybf{i}", [128, 1024], BF16) for i in range(2)]
        y32 = sb("a_y32", [128, 1024], F32)
        hTg = [sb(f"a_hTg{i}", [128, 8, 512], BF16) for i in range(2)]
        kst = [sb(f"a_kst{i}", [128, 512], BF16) for i in range(3)]
        vst = [sb(f"a_vst{i}", [128, VW], BF16) for i in range(2)]
        qst = [sb(f"a_qst{i}", [128, 10, 128], BF16) for i in range(2)]
        ones12 = sb("a_ones12", [128, 12, 1], F32)
        tp = [ps(f"a_tp{i}", [128, 8, 128], BF16) for i in range(2)]
        pm = [ps(f"a_pm{i}", [128, 512], F32) for i in range(4)]

        for dc in range(8):
            P.add("gpsimd", lambda e, dc=dc: e.dma_start(
                out=winb[:, dc, :], in_=g.w_in[dc * 128:(dc + 1) * 128, 0:3840]),
                w=[("winb", dc)], chan="a_w")
        P.add("sync", lambda e: e.dma_start(out=gT[:], in_=g.ln_in_gT),
              w=["gT"], chan="a_c")
        P.add("sync", lambda e: e.dma_start(out=bT[:], in_=g.ln_in_bT),
              w=["bT"], chan="a_c")
        P.add("sync", lambda e: e.dma_start(out=grep[:], in_=g.ln_in_g.partition_broadcast(128)),
              w=["grep"], chan="a_c")
        P.add("sync", lambda e: e.dma_start(out=brep[:], in_=g.ln_in_b.partition_broadcast(128)),
              w=["brep"], chan="a_c")
        P.add("sync", lambda e: e.dma_start(out=valid[:], in_=g.valid), w=["valid"], chan="a_c")
        P.add("sync", lambda e: e.dma_start(out=ident[:], in_=g.ident), w=["ident"], chan="a_c")

        P.add("vector", lambda e: e.memset(ones12[:], 1.0), w=["ones12"])
        kcols = [512 + 128 * i for i in range(4)] + [2304 + 128 * i for i in range(6)]
        qcols = [0 + 128 * i for i in range(4)] + [1536 + 128 * i for i in range(6)]
        vgroups = [(1024, 512, 0), (3072, 512, 512), (3584, 256, 1024)]
        cnt = {'pmi': 0, 'ksi': 0}

        def lnt(tg, bi):
            hT = hTg[tg % 2]
            hk = ("hTg", tg % 2)
            c = 4 * tg + bi
            xs = c % NX
            s2 = c % 2
            x_t = xt[xs]
            P.add("sync", lambda e, x_t=x_t, c=c: e.dma_start(
                out=x_t[:], in_=g.x_ctx[c * 128:(c + 1) * 128, :]),
                w=[("x", xs)], chan=f"a_x{xs}")
            for hh in range(2):
                P.add("vector", lambda e, x_t=x_t, s2=s2, hh=hh: e.bn_stats(
                    out=stats[s2][:, hh, :], in_=x_t[:, hh * 512:(hh + 1) * 512]),
                    r=[("x", xs)], w=[("stats", s2, hh)])
            P.add("vector", lambda e, s2=s2: e.bn_aggr(
                out=mv[s2][:], in_=stats[s2][:].rearrange("p a b -> p (a b)")),
                r=[("stats", s2, 0), ("stats", s2, 1)], w=[("mv", s2)])
            P.add("scalar", lambda e, s2=s2: e.activation(
                out=std[s2][:], in_=mv[s2][:, 1:2], func=AF.Sqrt, bias=LN_EPS),
                r=[("mv", s2)], w=[("std", s2)])
            P.add("vector", lambda e, s2=s2: e.reciprocal(out=rstd[s2][:], in_=std[s2][:]),
                r=[("std", s2)], w=[("rstd", s2)])
            P.add("vector", lambda e, x_t=x_t, s2=s2: e.tensor_scalar(
                out=ybf[s2][:], in0=x_t[:], scalar1=mv[s2][:, 0:1], scalar2=rstd[s2][:, 0:1],
                op0=ALU.subtract, op1=ALU.mult),
                r=[("x", xs), ("mv", s2), ("rstd", s2)], w=[("ybf", s2)])
            if bi == 3:
                so = tg
                P.add("gpsimd", lambda e, x_t=x_t, s2=s2: e.tensor_scalar(
                    out=y32[:], in0=x_t[:], scalar1=mv[s2][:, 0:1], scalar2=rstd[s2][:, 0:1],
                    op0=ALU.subtract, op1=ALU.mult),
                    r=[("x", xs), ("mv", s2), ("rstd", s2)], w=["y32"])
                P.add("gpsimd", lambda e: e.tensor_tensor(out=y32[:], in0=y32[:], in1=grep[:], op=ALU.mult),
                      r=["y32", "grep"], w=["y32"])
                P.add("gpsimd", lambda e: e.tensor_tensor(out=y32[:], in0=y32[:], in1=brep[:], op=ALU.add),
                      r=["y32", "brep"], w=["y32"])
                P.add("gpsimd", lambda e, so=so: e.dma_start(
                    out=g.h_own_d[so * 128:(so + 1) * 128, :], in_=y32[:]),
                    r=["y32"], w=[("h_own_d", so)], chan="a_y32")

        def tr(tg, bi):
            hT = hTg[tg % 2]
            hk = ("hTg", tg % 2)
            c = 4 * tg + bi
            s2 = c % 2
            tps = tp[c % 2]
            for dc in range(8):
                P.add("tensor", lambda e, tps=tps, s2=s2, dc=dc: e.transpose(
                    out=tps[:, dc, :], in_=ybf[s2][:, dc * 128:(dc + 1) * 128], identity=ident[:]),
                    r=[("ybf", s2), "ident"], w=[("tp", c % 2)])
            for dc in range(8):
                P.add("scalar", lambda e, tps=tps, dc=dc, hT=hT, bi=bi: e.activation(
                    out=hT[:, dc, bi * 128:(bi + 1) * 128], in_=tps[:, dc, :], func=AF.Identity,
                    scale=gT[:, dc:dc + 1], bias=bT[:, dc:dc + 1]),
                    r=[("tp", c % 2), "gT", "bT"], w=[hk + (bi,)])

        def mm(tg):
            hT = hTg[tg % 2]
            hk = ("hTg", tg % 2)
            pmi = cnt['pmi']
            ksi = cnt['ksi']
            hkall = [hk + (bi,) for bi in range(4)]
            P.add("gpsimd", lambda e, hT=hT, tg=tg: e.dma_start(
                out=g.hT_own_d[:, :, tg * 128:(tg + 1) * 128], in_=hT[:, :, 384:512]),
                r=[hk + (3,)], w=[("hT_own_d", tg)], chan=f"a_hT{tg % 2}")
            for kc in range(10):
                pmt = pm[pmi % 4]
                pk = ("pm", pmi % 4)
                pmi += 1
                for dc in range(8):
                    P.add("tensor", lambda e, pmt=pmt, dc=dc, kc=kc, hT=hT: e.matmul(
                        pmt[:], lhsT=winb[:, dc, kcols[kc]:kcols[kc] + 128], rhs=hT[:, dc, :],
                        start=(dc == 0), stop=(dc == 7)),
                        r=[("winb", dc)] + hkall, w=[pk])
                ks = kst[ksi % 3]
                kk = ("kst", ksi % 3)
                ksi_l = ksi % 3
                ksi += 1
                eng = "scalar" if kc % 2 == 0 else "vector"
                if eng == "scalar":
                    P.add("scalar", lambda e, ks=ks, pmt=pmt: e.activation(out=ks[:], in_=pmt[:], func=AF.Copy),
                          r=[pk], w=[kk])
                else:
                    P.add("vector", lambda e, ks=ks, pmt=pmt: e.tensor_copy(out=ks[:], in_=pmt[:]),
                          r=[pk], w=[kk])
                P.add("gpsimd", lambda e, ks=ks, kc=kc, tg=tg: e.dma_start(
                    out=g.kT_d[kc, :, tg * 512:(tg + 1) * 512], in_=ks[:]),
                    r=[kk], w=[("kT_d", kc, tg)], chan=f"a_kst{ksi_l}")
                yield
            for bi in range(4):
                c = 4 * tg + bi
                vs = vst[c % 2]
                vk = ("vst", c % 2)
                for (c0, ncol, o0) in vgroups:
                    pmt = pm[pmi % 4]
                    pk = ("pm", pmi % 4)
                    pmi += 1
                    for dc in range(8):
                        P.add("tensor", lambda e, pmt=pmt, dc=dc, c0=c0, ncol=ncol, hT=hT, bi=bi: e.matmul(
                            pmt[:, 0:ncol], lhsT=hT[:, dc, bi * 128:(bi + 1) * 128], rhs=winb[:, dc, c0:c0 + ncol],
                            start=(dc == 0), stop=(dc == 7)),
                            r=[("winb", dc), hk + (bi,)], w=[pk])
                    if o0 == 0:
                        o_ap = vs[:, 0:512]
                        i_ap = pmt[:, 0:512]
                    else:
                        h0 = (o0 - 512) // 64
                        nh = ncol // 64
                        o_ap = vs[:, 512:VW].rearrange("p (h e) -> p h e", e=65)[:, h0:h0 + nh, 0:64]
                        i_ap = pmt[:, 0:ncol].rearrange("p (h e) -> p h e", e=64)
                    P.add("vector", lambda e, o_ap=o_ap, i_ap=i_ap, c=c: e.tensor_scalar(
                        out=o_ap, in0=i_ap, scalar1=valid[:, c:c + 1], scalar2=None,
                        op0=ALU.mult),
                        r=[pk, "valid"], w=[vk + (o0,)])
                    yield
                P.add("vector", lambda e, vs=vs, c=c: e.tensor_scalar(
                    out=vs[:, 512:VW].rearrange("p (h e) -> p h e", e=65)[:, :, 64:65], in0=ones12[:],
                    scalar1=valid[:, c:c + 1], scalar2=None, op0=ALU.mult),
                    r=["ones12", "valid"], w=[vk + (1024,)])
                P.add("gpsimd", lambda e, vs=vs, c=c: e.dma_start(
                    out=g.v_d[c * 128:(c + 1) * 128, :], in_=vs[:]),
                    r=[vk + (0,), vk + (512,), vk + (1024,)], w=[("v_d", c)], chan=f"a_vst{c % 2}")
            for qc in range(10):
                pmt = pm[pmi % 4]
                pk = ("pm", pmi % 4)
                pmi += 1
                for dc in range(8):
                    P.add("tensor", lambda e, pmt=pmt, dc=dc, qc=qc, hT=hT: e.matmul(
                        pmt[:, 0:128], lhsT=winb[:, dc, qcols[qc]:qcols[qc] + 128], rhs=hT[:, dc, 384:512],
                        start=(dc == 0), stop=(dc == 7)),
                        r=[("winb", dc), hk + (3,)], w=[pk])
                P.add("scalar", lambda e, pmt=pmt, qc=qc, tg=tg: e.activation(
                    out=qst[tg % 2][:, qc, :], in_=pmt[:, 0:128], func=AF.Copy, scale=0.125),
                    r=[pk], w=[("qst", tg % 2, qc)])
                yield
            P.add("gpsimd", lambda e, tg=tg: e.dma_start(
                out=g.qT_d[:, :, tg * 128:(tg + 1) * 128], in_=qst[tg % 2][:]),
                r=[("qst", tg % 2, qc) for qc in range(10)], w=[("qT_d", tg)], chan=f"a_qst{tg % 2}")
            cnt['pmi'] = pmi
            cnt['ksi'] = ksi

        for bi in range(4):
            lnt(0, bi)
            tr(0, bi)
        for tg in range(ntg):
            gen = mm(tg)
            for gi_, _ in enumerate(gen):
                if tg + 1 < ntg and gi_ in (0, 8, 16, 24):
                    lnt(tg + 1, gi_ // 8)
                if tg + 1 < ntg and gi_ in (6, 14, 22, 30):
                    tr(tg + 1, (gi_ - 6) // 8)
        P.emit_phase()


def phase_b(P, nc, g, groups=(0, 1, 2, 3)):
    st = contextlib.ExitStack()
    with st:
        sb = lambda name, shape, dt: st.enter_context(nc.sbuf_tensor(name, shape, dt))
        ps = lambda name, shape, dt: st.enter_context(nc.psum_tensor(name, shape, dt))
        nblk_max = 16 * (max(groups) + 1)
        kTsb = sb("b_kT", [128, 4, S], BF16)
        vsb = sb("b_v", [128, NB, 512], BF16)
        sbmask = sb("b_mask", [128, 16, 512], BF16)
        ident = sb("b_ident", [128, 128], BF16)
        negU = sb("b_negU", [128, 128], BF16)
        negOnes = sb("b_negOnes", [128, 128], BF16)
        zer = sb("b_zer", [128, 64], BF16)
        qsb = [sb(f"b_q{i}", [128, 4, 512], BF16) for i in range(2)]
        e_sb = [sb(f"b_e{i}", [128, 512], F32) for i in range(4)]
        sp_sb = [sb(f"b_sp{i}", [128, 512], BF16) for i in range(4)]
        w_sb = [sb(f"b_w{i}", [128, 512], BF16) for i in range(4)]
        srun = [[sb(f"b_srun{i}_{j}", [128, 512], BF16) for j in range(2)] for i in range(4)]
        ost = [sb(f"b_ost{i}", [64, 512], BF16) for i in range(4)]
        pz = [ps(f"b_pz{i}", [128, 512], F32) for i in range(4)]
        po = [ps(f"b_po{i}", [64, 512], F32) for i in range(4)]

        P.add("sync", lambda e: e.dma_start(out=ident[:], in_=g.ident), w=["ident"], chan="b_c")
        P.add("sync", lambda e: e.dma_start(out=negU[:], in_=g.negU), w=["negU"], chan="b_c")
        P.add("sync", lambda e: e.dma_start(out=negOnes[:], in_=g.negOnes), w=["negOnes"], chan="b_c")
        P.add("sync", lambda e: e.dma_start(out=sbmask[:], in_=g.sbmask), w=["sbmask"], chan="b_c")
        P.add("gpsimd", lambda e: e.memset(zer[:], 0.0), w=["zer"])
        ntok = nblk_max * 128
        for kc in range(4):
            for hf in range(0, ntok, 2048):
                P.add("sync", lambda e, kc=kc, hf=hf: e.dma_start(
                    out=kTsb[:, kc, hf:hf + 2048], in_=g.kT_d[kc, :, hf:hf + 2048]),
                    w=[("kTsb", kc, hf // 2048)], chan="b_k")
        for cb in range(0, nblk_max, 16):
            P.add("sync", lambda e, cb=cb: e.dma_start(
                out=vsb[:, cb:cb + 16, :],
                in_=g.v_d[cb * 128:(cb + 16) * 128, 0:512].rearrange("(c p) n -> p c n", p=128)),
                w=[("vsb", cb // 16)], chan="b_v")

        for gi, gq in enumerate(groups):
            q_t = qsb[gi % 2]
            qk = ("qsb", gi % 2)
            P.add("sync", lambda e, q_t=q_t, gq=gq: e.dma_start(
                out=q_t[:], in_=g.qT_d[:, 0:4, gq * 512:(gq + 1) * 512]),
                w=[qk], chan=f"b_q{gi % 2}")
            nblk = 16 * (gq + 1)
            for hq in range(2):
                heads = [4 * hq + i for i in range(4)]
                for i in range(4):
                    for par in range(2):
                        P.add("gpsimd", lambda e, i=i, par=par: e.memset(srun[i][par][:], 0.0), w=[("srun", i, par)])
                    P.add("tensor", lambda e, i=i: e.matmul(
                        po[i][:], lhsT=zer[:], rhs=sbmask[:, 0, :], start=True, stop=True),
                        r=["zer", "sbmask"], w=[("po", i)])
                cs = list(range(nblk - 1, -1, -1))

                def prm(step):
                    c = cs[step]
                    rel_c = c - 16 * gq
                    q0 = 128 * (rel_c // 4) if rel_c >= 4 else 0
                    return c, rel_c, (rel_c >= 3), step % 2, q0

                def s1(step, i):
                    c, rel_c, need_mask, par, q0 = prm(step)
                    h = heads[i]
                    hc, half = h // 2, h % 2
                    p0 = 64 * half
                    P.add("tensor", lambda e, i=i, hc=hc, p0=p0, c=c, q_t=q_t, nm=need_mask, q0=q0: e.matmul(
                        pz[i][:, q0:512], lhsT=kTsb[p0:p0 + 64, hc, c * 128:(c + 1) * 128],
                        rhs=q_t[p0:p0 + 64, hc, q0:512], start=True, stop=(not nm)),
                        r=[("kTsb", hc, c // 16), qk], w=[("pz", i)])
                    if need_mask:
                        P.add("tensor", lambda e, i=i, rel_c=rel_c, q0=q0: e.matmul(
                            pz[i][:, q0:512], lhsT=ident[:], rhs=sbmask[:, rel_c, q0:512], start=False, stop=True),
                            r=["ident", "sbmask"], w=[("pz", i)])

                for i in range(4):
                    s1(0, i)
                for step in range(nblk):
                    c, rel_c, need_mask, par, q0 = prm(step)
                    for i, h in enumerate(heads):
                        P.add("scalar", lambda e, i=i, q0=q0: e.activation(
                            out=e_sb[i][:, q0:512], in_=pz[i][:, q0:512], func=AF.Exp),
                            r=[("pz", i)], w=[("e", i)])
                        P.add("scalar", lambda e, i=i, q0=q0: e.activation(
                            out=sp_sb[i][:, q0:512], in_=e_sb[i][:, q0:512], func=AF.Ln, bias=1.0),
                            r=[("e", i)], w=[("sp", i)])
                    for i, h in enumerate(heads):
                        last = (step == 0)
                        P.add("tensor", lambda e, i=i, last=last, q0=q0: e.matmul(
                            pz[i][:, q0:512], lhsT=negU[:], rhs=sp_sb[i][:, q0:512], start=False, stop=last,
                            skip_group_check=True),
                            r=["negU", ("sp", i)], w=[("pz", i)])
                        if step > 0:
                            P.add("tensor", lambda e, i=i, par=par, q0=q0: e.matmul(
                                pz[i][:, q0:512], lhsT=negOnes[:], rhs=srun[i][1 - par][:, q0:512], start=False,
                                stop=True, skip_group_check=True),
                                r=["negOnes", ("srun", i, 1 - par)], w=[("pz", i)])
                    for i, h in enumerate(heads):
                        if step == 0:
                            P.add("vector", lambda e, i=i, par=par, q0=q0: e.tensor_copy(
                                out=srun[i][par][:, q0:512], in_=sp_sb[i][:, q0:512]),
                                r=[("sp", i)], w=[("srun", i, par)])
                        elif c > 0:
                            P.add("vector", lambda e, i=i, par=par, q0=q0: e.tensor_tensor(
                                out=srun[i][par][:, q0:512], in0=srun[i][1 - par][:, q0:512], in1=sp_sb[i][:, q0:512],
                                op=ALU.add),
                                r=[("sp", i), ("srun", i, 1 - par)], w=[("srun", i, par)])
                    for i, h in enumerate(heads):
                        P.add("scalar", lambda e, i=i, q0=q0: e.activation(
                            out=w_sb[i][:, q0:512], in_=pz[i][:, q0:512], func=AF.Exp),
                            r=[("pz", i)], w=[("w", i)])
                    for i, h in enumerate(heads):
                        if step + 1 < nblk:
                            s1(step + 1, i)
                        P.add("tensor", lambda e, i=i, h=h, c=c, q0=q0: e.matmul(
                            po[i][:, q0:512], lhsT=vsb[:, c, h * 64:(h + 1) * 64], rhs=w_sb[i][:, q0:512],
                            start=False, stop=True, skip_group_check=True),
                            r=[("vsb", c // 16), ("w", i)], w=[("po", i)])
                for i, h in enumerate(heads):
                    P.add("vector", lambda e, i=i: e.tensor_copy(out=ost[i][:], in_=po[i][:]),
                          r=[("po", i)], w=[("ost", i)])
                    P.add("sync", lambda e, i=i, h=h, gq=gq: e.dma_start(
                        out=g.osbT_d[h, :, gq * 512:(gq + 1) * 512], in_=ost[i][:]),
                        r=[("ost", i)], w=[("osbT_d", h, gq)], chan=f"b_ost{i}")
        P.emit_phase()


DIL = ((128, 1), (512, 4), (2048, 16))
DL_NB = [w // 128 + 1 for w, _ in DIL]
DL_TOFF = [0, 4 * DL_NB[0], 4 * (DL_NB[0] + DL_NB[1])]
DL_NT = 4 * sum(DL_NB)


def phase_c(P, nc, g, slots=tuple(range(NOWN))):
    st = contextlib.ExitStack()
    with st:
        sb = lambda name, shape, dt: st.enter_context(nc.sbuf_tensor(name, shape, dt))
        ps = lambda name, shape, dt: st.enter_context(nc.psum_tensor(name, shape, dt))
        WB = 17
        kdl = [sb(f"c_k{i}", [128, 6, WB * 128], BF16) for i in range(2)]
        vdl = [sb(f"c_v{i}", [128, WB, 780], BF16) for i in range(2)]
        qdl = [sb(f"c_q{i}", [128, 6, 128], BF16) for i in range(2)]
        dlbias = sb("c_bias", [128, DL_NT, 128], BF16)
        padbias = sb("c_pad", [128, NB], F32)
        ident = sb("c_ident", [128, 128], BF16)
        pT = [sb(f"c_pT{i}", [128, 4, 128], BF16) for i in range(4)]
        rden = sb("c_rden", [128, 4], F32)
        otok = [sb(f"c_otok{i}", [128, 256], BF16) for i in range(2)]
        oT = [sb(f"c_oT{i}", [128, 2, 128], BF16) for i in range(2)]
        pzd = [ps(f"c_pz{i}", [128, 4, 128], F32) for i in range(4)]
        pd = [ps(f"c_pd{i}", [128, 4, 65], F32) for i in range(2)]
        ptr = ps("c_ptr", [128, 2, 128], BF16)

        P.add("sync", lambda e: e.dma_start(out=ident[:], in_=g.ident), w=["ident"], chan="c_c")
        P.add("sync", lambda e: e.dma_start(out=padbias[:], in_=g.padbias), w=["padbias"], chan="c_c")
        for t0 in range(0, DL_NT, 24):
            P.add("sync", lambda e, t0=t0: e.dma_start(out=dlbias[:, t0:t0 + 24, :], in_=g.dlbias[:, t0:t0 + 24, :]),
                  w=[("dlbias", t0 // 24)], chan="c_c")
        ui = 0
        for si, s_ in enumerate(slots):
            cq = 4 * s_ + 3
            c_lo = max(0, cq - 16)
            nwb = cq - c_lo + 1
            b2 = si % 2
            P.add("sync", lambda e, b2=b2, c_lo=c_lo, nwb=nwb: e.dma_start(
                out=kdl[b2][:, :, 0:nwb * 128],
                in_=g.kT_d[4:10, :, c_lo * 128:(c_lo + nwb) * 128].rearrange("c p t -> p c t")),
                w=[("kdl", b2)], chan=f"c_k{b2}")
            P.add("sync", lambda e, b2=b2, c_lo=c_lo, nwb=nwb: e.dma_start(
                out=vdl[b2][:, 0:nwb, :],
                in_=g.v_d[c_lo * 128:(c_lo + nwb) * 128, 512:VW].rearrange("(c p) n -> p c n", p=128)),
                w=[("vdl", b2)], chan=f"c_v{b2}")
            P.add("sync", lambda e, b2=b2, s_=s_: e.dma_start(
                out=qdl[b2][:], in_=g.qT_d[:, 4:10, s_ * 128:(s_ + 1) * 128]),
                w=[("qdl", b2)], chan=f"c_q{b2}")
            pdt = pd[si % 2]
            pdk = ("pd", si % 2)
            units = []
            for hg in range(4):
                uh = []
                for gi in range(3):
                    for o in range(DL_NB[gi]):
                        c = cq - o
                        if c >= 0:
                            uh.append((gi, o, c))
                for k_, (gi, o, c) in enumerate(uh):
                    units.append((hg, k_, len(uh), gi, o, c))
            NBATCH = 4
            batches = [units[i:i + NBATCH] for i in range(0, len(units), NBATCH)]

            def s1(bt, zi):
                for j, u in enumerate(bt):
                    hg, k_, n, gi, o, c = u
                    hd = 4 * gi + hg
                    chn, half = hd // 2, hd % 2
                    p0 = 64 * half
                    wb = c - c_lo
                    tix = DL_TOFF[gi] + hg * DL_NB[gi] + o
                    P.add("tensor", lambda e, zi=zi, j=j, b2=b2, chn=chn, p0=p0, wb=wb: e.matmul(
                        pzd[zi][:, j, :], lhsT=kdl[b2][p0:p0 + 64, chn, wb * 128:(wb + 1) * 128],
                        rhs=qdl[b2][p0:p0 + 64, chn, :], start=True, stop=False),
                        r=[("kdl", b2), ("qdl", b2)], w=[("pzd", zi)])
                    P.add("tensor", lambda e, zi=zi, j=j, tix=tix: e.matmul(
                        pzd[zi][:, j, :], lhsT=ident[:], rhs=dlbias[:, tix, :], start=False, stop=True),
                        r=["ident", ("dlbias", tix // 24)], w=[("pzd", zi)])
                nb_ = len(bt)
                P.add("scalar", lambda e, zi=zi, nb_=nb_: e.activation(
                    out=pT[zi][:, 0:nb_, :], in_=pzd[zi][:, 0:nb_, :], func=AF.Exp),
                    r=[("pzd", zi)], w=[("pT", zi)])

            def s2(bt, zi):
                for j, u in enumerate(bt):
                    hg, k_, n, gi, o, c = u
                    hd = 4 * gi + hg
                    wb = c - c_lo
                    P.add("tensor", lambda e, zi=zi, j=j, b2=b2, wb=wb, hd=hd, hg=hg, k_=k_, n=n, pdt=pdt: e.matmul(
                        pdt[:, hg, :], lhsT=pT[zi][:, j, :], rhs=vdl[b2][:, wb, hd * 65:(hd + 1) * 65],
                        start=(k_ == 0), stop=(k_ == n - 1)),
                        r=[("pT", zi), ("vdl", b2)], w=[pdk])

            LOOK = 3
            zis = []
            for i in range(len(batches) + LOOK):
                if i < len(batches):
                    zis.append(ui % 4)
                    ui += 1
                    s1(batches[i], zis[i])
                if i - LOOK >= 0:
                    s2(batches[i - LOOK], zis[i - LOOK])
            P.add("vector", lambda e, pdt=pdt: e.reciprocal(out=rden[:], in_=pdt[:, :, 64]),
                  r=[pdk], w=["rden"])
            ot = otok[si % 2]
            for hg in range(4):
                P.add("vector", lambda e, pdt=pdt, hg=hg, ot=ot: e.tensor_scalar(
                    out=ot[:, hg * 64:(hg + 1) * 64], in0=pdt[:, hg, 0:64], scalar1=rden[:, hg:hg + 1],
                    scalar2=None, op0=ALU.mult),
                    r=[pdk, "rden"], w=[("otok", si % 2)])
            for cc in range(2):
                P.add("tensor", lambda e, cc=cc, ot=ot: e.transpose(
                    out=ptr[:, cc, :], in_=ot[:, cc * 128:(cc + 1) * 128], identity=ident[:]),
                    r=[("otok", si % 2), "ident"], w=["ptr"])
            P.add("vector", lambda e, si=si: e.tensor_copy(out=oT[si % 2][:], in_=ptr[:]),
                  r=["ptr"], w=[("oT", si % 2)])
            P.add("gpsimd", lambda e, si=si, s_=s_: e.dma_start(
                out=g.odlT_d[:, :, s_ * 128:(s_ + 1) * 128].rearrange("c p t -> p c t"), in_=oT[si % 2][:]),
                r=[("oT", si % 2)], w=[("odlT_d", s_)], chan=f"c_oT{si % 2}")
        P.emit_phase()


def _bounds_reg(P, g):
    def fn(e):
        if not g.bcreg:
            g.bcreg.append(e.alloc_register("bc"))
        return e.reg_mov(g.bcreg[0], NEXP * CAP - 1)
    P.add("gpsimd", fn)


def phase_d(P, nc, g, qgroups=(0, 1, 2, 3), d_stop=9):
    st = contextlib.ExitStack()
    with st:
        sb = lambda name, shape, dt: st.enter_context(nc.sbuf_tensor(name, shape, dt))
        ps = lambda name, shape, dt: st.enter_context(nc.psum_tensor(name, shape, dt))
        wg = sb("d_wg", [128, 8, 2048], BF16)
        wbs = sb("d_wbs", [64, 8, 1024], BF16)
        wbd = sb("d_wbd", [128, 2, 1024], BF16)
        wo = sb("d_wo", [128, 8, 1024], BF16)
        wr = sb("d_wr", [128, 8, 256], F32)
        bgT = sb("d_bgT", [128, 16], F32)
        g1rep = sb("d_g1", [128, 1024], F32)
        b1rep = sb("d_b1", [128, 1024], F32)
        rbrep = sb("d_rb", [128, 256], F32)
        iota = sb("d_iota", [128, 256], F32)
        identb = sb("d_identb", [128, 128], BF16)
        wr_hi = sb("d_wr_hi", [128, 8, 256], BF16)
        wr_lo = sb("d_wr_lo", [128, 8, 256], BF16)
        h1lo = sb("d_h1lo", [128, 1024], BF16)
        h1Tlo = sb("d_h1Tlo", [128, 8, 128], BF16)
        lstrict = sb("d_lstrict", [128, 128], BF16)
        ones = sb("d_ones", [128, 128], BF16)
        hTo = sb("d_hTo", [128, 8, 512], BF16)
        osb = sb("d_osb", [64, 8, 512], BF16)
        odl = sb("d_odl", [128, 2, 512], BF16)
        gs = sb("d_gs", [128, 512], F32)
        gd = sb("d_gd", [128, 512], F32)
        m1 = sb("d_m1", [128, 512], F32)
        m2 = sb("d_m2", [128, 512], F32)
        mT = sb("d_mT", [128, 8, 512], BF16)
        hown = [sb(f"d_hown{i}", [128, 1024], F32) for i in range(2)]
        rr = sb("d_r", [128, 1024], F32)
        h1 = [sb(f"d_h1_{i}", [128, 1024], F32) for i in range(2)]
        h1b = [sb(f"d_h1b{i}", [128, 1024], BF16) for i in range(2)]
        h1Tb = [sb(f"d_h1Tb{i}", [128, 8, 128], BF16) for i in range(2)]
        stats = sb("d_stats", [128, 2, 6], F32)
        mv = sb("d_mv", [128, 2], F32)
        std = sb("d_std", [128, 1], F32)
        rstd = sb("d_rstd", [128, 1], F32)
        sc = [sb(f"d_sc{i}", [128, 256], F32) for i in range(2)]
        biased = [sb(f"d_biased{i}", [128, 256], F32) for i in range(2)]
        masked = sb("d_masked", [128, 256], F32)
        junk = sb("d_junk", [128, 256], F32)
        selb = sb("d_selb", [128, NOWN, 256], BF16)
        m8g = sb("d_m8g", [128, 8, 8], F32)
        gscore = sb("d_gscore", [128, 8], F32)
        gm8 = sb("d_gm8", [128, 8], F32)
        pen = sb("d_pen", [128, 8], F32)
        t8 = sb("d_t8", [128, 8], F32)
        wk = sb("d_wk", [128, 8], F32)
        rk = sb("d_rk", [128, 8], F32)
        ik = sb("d_ik", [128, 8], F32)
        wsum = sb("d_wsum", [128, 1], F32)
        sif = sb("d_sif", [128, 8], F32)
        ovf = sb("d_ovf", [128, 8], F32)
        pA = ps("d_pA", [128, 512], F32)
        pB = ps("d_pB", [128, 512], F32)
        pC = ps("d_pC", [128, 512], F32)
        pD = ps("d_pD", [128, 512], F32)
        pmix = ps("d_pmix", [128, 1024], F32)
        ptr = ps("d_ptr", [128, 8, 128], BF16)
        ptr_lo = ps("d_ptr_lo", [128, 8, 128], BF16)

        _bounds_reg(P, g)
        for dc in range(8):
            P.add("gpsimd", lambda e, dc=dc: e.dma_start(
                out=wg[:, dc, :], in_=g.w_in[dc * 128:(dc + 1) * 128, 3840:5888]), w=[("wg", dc)], chan="d_w")
        P.add("gpsimd", lambda e: e.dma_start(
            out=wbs[:], in_=g.w_br_sb.rearrange("(h p) n -> p h n", p=64)), w=["wbs"], chan="d_w")
        P.add("gpsimd", lambda e: e.dma_start(
            out=wbd[:], in_=g.w_br_dil.rearrange("(c p) n -> p c n", p=128)), w=["wbd"], chan="d_w")
        for dc in range(8):
            P.add("gpsimd", lambda e, dc=dc: e.dma_start(
                out=wo[:, dc, :], in_=g.w_out[dc * 128:(dc + 1) * 128, :]), w=[("wo", dc)], chan="d_w")
        P.add("sync", lambda e: e.dma_start(out=wr[:], in_=g.w_router.rearrange("(c p) n -> p c n", p=128)),
              w=["wr"], chan="d_wr")
        P.add("sync", lambda e: e.dma_start(out=bgT[:], in_=g.b_gateT), w=["bgT"], chan="d_c")
        P.add("sync", lambda e: e.dma_start(out=g1rep[:], in_=g.ln1_g.partition_broadcast(128)), w=["g1rep"], chan="d_c")
        P.add("sync", lambda e: e.dma_start(out=b1rep[:], in_=g.ln1_b.partition_broadcast(128)), w=["b1rep"], chan="d_c")
        P.add("sync", lambda e: e.dma_start(out=rbrep[:], in_=g.router_bias.partition_broadcast(128)), w=["rbrep"], chan="d_c")
        P.add("sync", lambda e: e.dma_start(out=iota[:], in_=g.iota256), w=["iota"], chan="d_c")
        P.add("sync", lambda e: e.dma_start(out=identb[:], in_=g.ident), w=["identb"], chan="d_c")
        P.add("gpsimd", lambda e: e.dma_start(out=wr_hi[:], in_=g.w_router.rearrange("(c p) n -> p c n", p=128)),
              w=["wr_hi"], chan="d_wrh")
        P.add("vector", lambda e: e.tensor_tensor(out=wr_lo[:], in0=wr[:], in1=wr_hi[:], op=ALU.subtract),
              r=["wr", "wr_hi"], w=["wr_lo"])
        P.add("sync", lambda e: e.dma_start(out=lstrict[:], in_=g.lstrict), w=["lstrict"], chan="d_c")
        P.add("sync", lambda e: e.dma_start(out=ones[:], in_=g.ones), w=["ones"], chan="d_c")


        pend = [None]
        allk = lambda n: [(n, k) for k in range(8)]

        def xgen(bi, blk):
            b2 = blk % 2
            P.add("sync", lambda e, blk=blk, b2=b2: e.dma_start(
                out=hown[b2][:], in_=g.h_own_d[blk * 128:(blk + 1) * 128, :]), w=[("hown", b2)], chan=f"d_hown{b2}")
            for nh in range(2):
                for oc in range(8):
                    P.add("tensor", lambda e, bi=bi, nh=nh, oc=oc: e.matmul(
                        pmix[:, nh * 512:(nh + 1) * 512], lhsT=mT[:, oc, bi * 128:(bi + 1) * 128],
                        rhs=wo[:, oc, nh * 512:(nh + 1) * 512], start=(oc == 0), stop=(oc == 7)),
                        r=[("mT", oc), ("wo", oc)], w=[("pmix", nh)])
            yield
            for nh in range(2):
                P.add("vector", lambda e, b2=b2, nh=nh: e.scalar_tensor_tensor(
                    out=rr[:, nh * 512:(nh + 1) * 512], in0=hown[b2][:, nh * 512:(nh + 1) * 512], scalar=ALPHA,
                    in1=pmix[:, nh * 512:(nh + 1) * 512], op0=ALU.mult, op1=ALU.add),
                    r=[("hown", b2), ("pmix", nh)], w=[("rr", nh)])
            for hh in range(2):
                P.add("vector", lambda e, hh=hh: e.bn_stats(out=stats[:, hh, :], in_=rr[:, hh * 512:(hh + 1) * 512]),
                      r=[("rr", hh)], w=[("dstats", hh)])
            P.add("vector", lambda e: e.bn_aggr(out=mv[:], in_=stats[:].rearrange("p a b -> p (a b)")),
                  r=[("dstats", 0), ("dstats", 1)], w=["dmv"])
            P.add("scalar", lambda e: e.activation(out=std[:], in_=mv[:, 1:2], func=AF.Sqrt, bias=LN_EPS),
                  r=["dmv"], w=["dstd"])
            yield
            P.add("vector", lambda e: e.reciprocal(out=rstd[:], in_=std[:]), r=["dstd"], w=["drstd"])
            P.add("vector", lambda e, b2=b2: e.tensor_scalar(
                out=h1[b2][:], in0=rr[:], scalar1=mv[:, 0:1], scalar2=rstd[:, 0:1], op0=ALU.subtract, op1=ALU.mult),
                r=[("rr", 0), ("rr", 1), "dmv", "drstd"], w=[("h1", b2)])
            P.add("gpsimd", lambda e, b2=b2: e.tensor_tensor(out=h1[b2][:], in0=h1[b2][:], in1=g1rep[:], op=ALU.mult),
                  r=[("h1", b2), "g1rep"], w=[("h1", b2)])
            P.add("gpsimd", lambda e, b2=b2: e.tensor_tensor(out=h1[b2][:], in0=h1[b2][:], in1=b1rep[:], op=ALU.add),
                  r=[("h1", b2), "b1rep"], w=[("h1", b2)])
            P.add("scalar", lambda e, blk=blk, b2=b2: e.dma_start(
                out=g.h1_d[blk * 128:(blk + 1) * 128, :], in_=h1[b2][:]), r=[("h1", b2)], w=[("h1_d", blk)],
                chan=f"d_h1{b2}")
            P.add("scalar", lambda e, b2=b2: e.activation(out=h1b[b2][:], in_=h1[b2][:], func=AF.Copy),
                  r=[("h1", b2)], w=[("h1b", b2)])
            yield
            P.add("vector", lambda e, b2=b2: e.tensor_tensor(out=h1lo[:], in0=h1[b2][:], in1=h1b[b2][:], op=ALU.subtract),
                  r=[("h1", b2), ("h1b", b2)], w=["h1lo"])
            for dc in range(8):
                P.add("tensor", lambda e, b2=b2, dc=dc: e.transpose(
                    out=ptr[:, dc, :], in_=h1b[b2][:, dc * 128:(dc + 1) * 128], identity=identb[:]),
                    r=[("h1b", b2), "identb"], w=["ptr"])
            for dc in range(8):
                P.add("tensor", lambda e, dc=dc: e.transpose(
                    out=ptr_lo[:, dc, :], in_=h1lo[:, dc * 128:(dc + 1) * 128], identity=identb[:]),
                    r=["h1lo", "identb"], w=["ptr_lo"])
            P.add("scalar", lambda e, b2=b2: e.activation(out=h1Tb[b2][:], in_=ptr[:], func=AF.Copy),
                  r=["ptr"], w=[("h1Tb", b2)])
            P.add("scalar", lambda e, blk=blk, b2=b2: e.dma_start(
                out=g.h1T_d[:, :, blk * 128:(blk + 1) * 128], in_=h1Tb[b2][:]), r=[("h1Tb", b2)],
                w=[("h1T_d", blk)], chan=f"d_h1T{b2}")
            yield
            P.add("vector", lambda e: e.tensor_copy(out=h1Tlo[:], in_=ptr_lo[:]), r=["ptr_lo"], w=["h1Tlo"])
            combos = [(h1Tb[b2], ("h1Tb", b2), wr_hi, "wr_hi"), (h1Tb[b2], ("h1Tb", b2), wr_lo, "wr_lo"),
                      (h1Tlo, "h1Tlo", wr_hi, "wr_hi")]
            for ci, (lt, ltk, rt, rtk) in enumerate(combos):
                for dc in range(8):
                    P.add("tensor", lambda e, dc=dc, lt=lt, rt=rt, ci=ci: e.matmul(
                        pA[:, 0:256], lhsT=lt[:, dc, :], rhs=rt[:, dc, :], start=(ci == 0 and dc == 0),
                        stop=(ci == 2 and dc == 7)), r=[ltk, rtk], w=["pA"])
            P.add("scalar", lambda e, b2=b2: e.activation(out=sc[b2][:], in_=pA[:, 0:256], func=AF.Sigmoid),
                  r=["pA"], w=[("sc", b2)])
            yield
            P.add("vector", lambda e, b2=b2: e.tensor_tensor(out=biased[b2][:], in0=sc[b2][:], in1=rbrep[:], op=ALU.add),
                  r=[("sc", b2), "rbrep"], w=[("biased", b2)])

        def ygen(blk):
            b2 = blk % 2
            bia = biased[b2]
            bk = ("biased", b2)
            sct = sc[b2]
            sck = ("sc", b2)
            for gr in range(8):
                P.add("vector", lambda e, gr=gr: e.max(out=m8g[:, gr, :], in_=bia[:, gr * 32:(gr + 1) * 32]),
                      r=[bk], w=[("m8g", gr)])
            P.add("vector", lambda e: e.tensor_tensor(out=gscore[:], in0=m8g[:, :, 0], in1=m8g[:, :, 1], op=ALU.add),
                  r=[("m8g", gr) for gr in range(8)], w=["gscore"])
            P.add("vector", lambda e: e.max(out=gm8[:], in_=gscore[:]), r=["gscore"], w=["gm8"])
            P.add("vector", lambda e: e.tensor_scalar(
                out=pen[:], in0=gscore[:], scalar1=gm8[:, 3:4], scalar2=1.0e4, op0=ALU.is_lt, op1=ALU.mult),
                r=["gscore", "gm8"], w=["pen"])
            P.add("vector", lambda e: e.tensor_tensor(
                out=masked[:].rearrange("p (a b) -> p a b", b=32), in0=bia[:].rearrange("p (a b) -> p a b", b=32),
                in1=pen[:].unsqueeze(2).to_broadcast([128, 8, 32]), op=ALU.subtract),
                r=[bk, "pen"], w=["masked"])
            P.add("vector", lambda e: e.max(out=t8[:], in_=masked[:]), r=["masked"], w=["t8"])
            P.add("vector", lambda e, blk=blk: e.tensor_scalar(
                out=selb[:, blk, :], in0=masked[:], scalar1=t8[:, 7:8], scalar2=None, op0=ALU.is_ge),
                r=["masked", "t8"], w=[("selb", blk)])
            P.add("tensor", lambda e, blk=blk: e.matmul(
                pB[:, 0:256], lhsT=lstrict[:], rhs=selb[:, blk, :], start=True, stop=(blk == 0)),
                r=["lstrict", ("selb", blk)], w=["pB"])
            for pb_ in range(blk):
                P.add("tensor", lambda e, pb_=pb_, blk=blk: e.matmul(
                    pB[:, 0:256], lhsT=ones[:], rhs=selb[:, pb_, :], start=False, stop=(pb_ == blk - 1)),
                    r=["ones", ("selb", pb_)], w=["pB"])
            yield
            for k in range(8):
                P.add("vector", lambda e, k=k: e.scalar_tensor_tensor(
                    out=junk[:], in0=masked[:], scalar=t8[:, k:k + 1], in1=sct[:], op0=ALU.is_equal, op1=ALU.mult,
                    accum_out=wk[:, k:k + 1]), r=["masked", "t8", sck], w=["junk", ("wk", k)])
                P.add("vector", lambda e, k=k: e.scalar_tensor_tensor(
                    out=junk[:], in0=masked[:], scalar=t8[:, k:k + 1], in1=iota[:], op0=ALU.is_equal,
                    op1=ALU.mult, accum_out=ik[:, k:k + 1]), r=["masked", "t8", "iota"], w=["junk", ("ik", k)])
                if k % 3 == 2:
                    yield
            yield
            for k in range(8):
                P.add("vector", lambda e, k=k: e.scalar_tensor_tensor(
                    out=junk[:], in0=masked[:], scalar=t8[:, k:k + 1], in1=pB[:, 0:256], op0=ALU.is_equal,
                    op1=ALU.mult, accum_out=rk[:, k:k + 1]), r=["masked", "t8", "pB"], w=["junk", ("rk", k)])
            P.add("vector", lambda e: e.tensor_reduce(out=wsum[:], in_=wk[:], axis=AX.X, op=ALU.add),
                  r=allk("wk"), w=["wsum"])
            P.add("vector", lambda e: e.reciprocal(out=wsum[:], in_=wsum[:]), r=["wsum"], w=["wsum"])
            P.add("vector", lambda e, blk=blk: e.tensor_scalar(
                out=g.gk[:, blk, :], in0=wk[:], scalar1=wsum[:, 0:1], scalar2=2.5, op0=ALU.mult, op1=ALU.mult),
                r=allk("wk") + ["wsum"], w=[("gk", blk)])
            P.add("vector", lambda e: e.tensor_scalar(
                out=ovf[:], in0=rk[:], scalar1=float(CAP), scalar2=1.0e6, op0=ALU.is_ge, op1=ALU.mult),
                r=allk("rk"), w=["ovf"])
            P.add("vector", lambda e: e.scalar_tensor_tensor(
                out=sif[:], in0=ik[:], scalar=float(CAP), in1=rk[:], op0=ALU.mult, op1=ALU.add),
                r=allk("ik") + allk("rk"), w=["sif"])
            P.add("vector", lambda e: e.tensor_tensor(out=sif[:], in0=sif[:], in1=ovf[:], op=ALU.add),
                  r=["sif", "ovf"], w=["sif"])
            P.add("vector", lambda e, blk=blk: e.tensor_copy(out=g.sidx[:, blk, :], in_=sif[:]),
                  r=["sif"], w=[("sidx", blk)])
            for k in range(8):
                P.add("gpsimd", lambda e, blk=blk, k=k, b2=b2: e.indirect_dma_start(
                    out=g.xs_d[:, :], out_offset=bass.IndirectOffsetOnAxis(ap=g.sidx[:, blk, k:k + 1], axis=0),
                    in_=h1b[b2][:, :], in_offset=None, bounds_check=g.bcreg[0], oob_is_err=False),
                    r=[("h1b", b2), ("sidx", blk)], w=[("xs_d", blk, k)], chan=f"d_sc{b2}")

        for gq in qgroups:
            t0 = gq * 512
            P.add("sync", lambda e, t0=t0: e.dma_start(out=hTo[:], in_=g.hT_own_d[:, :, t0:t0 + 512]),
                  w=["hTo"], chan="d_hTo")
            P.add("sync", lambda e, t0=t0: e.dma_start(
                out=osb[:], in_=g.osbT_d[:, :, t0:t0 + 512].rearrange("h p t -> p h t")), w=["osb"], chan="d_osb")
            P.add("sync", lambda e, t0=t0: e.dma_start(
                out=odl[:], in_=g.odlT_d[:, :, t0:t0 + 512].rearrange("c p t -> p c t")), w=["odl"], chan="d_odl")
            for oc in range(8):
                for dc in range(8):
                    P.add("tensor", lambda e, oc=oc, dc=dc: e.matmul(
                        pA[:], lhsT=wg[:, dc, oc * 128:(oc + 1) * 128], rhs=hTo[:, dc, :],
                        start=(dc == 0), stop=(dc == 7)), r=[("wg", dc), "hTo"], w=["pA"])
                for dc in range(8):
                    P.add("tensor", lambda e, oc=oc, dc=dc: e.matmul(
                        pB[:], lhsT=wg[:, dc, 1024 + oc * 128:1024 + (oc + 1) * 128], rhs=hTo[:, dc, :],
                        start=(dc == 0), stop=(dc == 7)), r=[("wg", dc), "hTo"], w=["pB"])
                for h in range(8):
                    P.add("tensor", lambda e, oc=oc, h=h: e.matmul(
                        pC[:], lhsT=wbs[:, h, oc * 128:(oc + 1) * 128], rhs=osb[:, h, :],
                        start=(h == 0), stop=(h == 7)), r=["wbs", "osb"], w=["pC"])
                for c2 in range(2):
                    P.add("tensor", lambda e, oc=oc, c2=c2: e.matmul(
                        pD[:], lhsT=wbd[:, c2, oc * 128:(oc + 1) * 128], rhs=odl[:, c2, :],
                        start=(c2 == 0), stop=(c2 == 1)), r=["wbd", "odl"], w=["pD"])
                P.add("scalar", lambda e, oc=oc: e.activation(
                    out=gs[:], in_=pA[:], func=AF.Sigmoid, bias=bgT[:, oc:oc + 1]), r=["pA", "bgT"], w=["gs"])
                P.add("scalar", lambda e, oc=oc: e.activation(
                    out=gd[:], in_=pB[:], func=AF.Sigmoid, bias=bgT[:, 8 + oc:9 + oc]), r=["pB", "bgT"], w=["gd"])
                P.add("vector", lambda e: e.tensor_tensor(out=m1[:], in0=gs[:], in1=pC[:], op=ALU.mult),
                      r=["gs", "pC"], w=["m1"])
                P.add("vector", lambda e: e.tensor_tensor(out=m2[:], in0=gd[:], in1=pD[:], op=ALU.mult),
                      r=["gd", "pD"], w=["m2"])
                P.add("gpsimd", lambda e, oc=oc: e.tensor_tensor(out=mT[:, oc, :], in0=m1[:], in1=m2[:], op=ALU.add),
                      r=["m1", "m2"], w=[("mT", oc)])
            for bi in range(4):
                blk = gq * 4 + bi
                xg = xgen(bi, blk)
                yg = pend[0]
                xa, ya = True, yg is not None
                while xa or ya:
                    if xa:
                        try:
                            next(xg)
                        except StopIteration:
                            xa = False
                    if ya:
                        try:
                            next(yg)
                        except StopIteration:
                            ya = False
                pend[0] = ygen(blk)
        if pend[0] is not None:
            for _ in pend[0]:
                pass
        P.emit_phase()


def _layer_norm(P, x, xk0, xk1, stats, mv, std, rstd, out, outk, grep, gk_, brep, bk_, tag):
    for hh, xk in ((0, xk0), (1, xk1)):
        P.add("vector", lambda e, hh=hh: e.bn_stats(out=stats[:, hh, :], in_=x[:, hh * 512:(hh + 1) * 512]),
              r=[xk], w=[(tag + "stats", hh)])
    P.add("vector", lambda e: e.bn_aggr(out=mv[:], in_=stats[:].rearrange("p a b -> p (a b)")),
          r=[(tag + "stats", 0), (tag + "stats", 1)], w=[tag + "mv"])
    P.add("scalar", lambda e: e.activation(out=std[:], in_=mv[:, 1:2], func=AF.Sqrt, bias=LN_EPS),
          r=[tag + "mv"], w=[tag + "std"])
    P.add("vector", lambda e: e.reciprocal(out=rstd[:], in_=std[:]), r=[tag + "std"], w=[tag + "rstd"])
    P.add("vector", lambda e: e.tensor_scalar(
        out=out[:], in0=x[:], scalar1=mv[:, 0:1], scalar2=rstd[:, 0:1], op0=ALU.subtract, op1=ALU.mult),
        r=[xk0, xk1, tag + "mv", tag + "rstd"], w=[outk])
    P.add("gpsimd", lambda e: e.tensor_tensor(out=out[:], in0=out[:], in1=grep[:], op=ALU.mult),
          r=[outk, gk_], w=[outk])
    P.add("gpsimd", lambda e: e.tensor_tensor(out=out[:], in0=out[:], in1=brep[:], op=ALU.add),
          r=[outk, bk_], w=[outk])


def _swiglu_block(P, xT, xTk, wg_t, wu_t, wd_t, wkeys, pgu, pguk, sg, sgk, hid, hidk, py, pyk):
    for j, wt in enumerate((wg_t, wu_t)):
        for hh in range(2):
            for dc in range(8):
                P.add("tensor", lambda e, j=j, wt=wt, hh=hh, dc=dc: e.matmul(
                    pgu[:, 2 * j + hh, :], lhsT=wt[:, dc, hh * 128:(hh + 1) * 128], rhs=xT[:, dc, :],
                    start=(dc == 0), stop=(dc == 7)), r=[wkeys[j], xTk], w=[pguk])
    P.add("scalar", lambda e: e.activation(out=sg[:], in_=pgu[:, 0:2, :], func=AF.Silu), r=[pguk], w=[sgk])
    P.add("vector", lambda e: e.tensor_tensor(out=hid[:], in0=sg[:], in1=pgu[:, 2:4, :], op=ALU.mult),
          r=[sgk, pguk], w=[hidk])
    for nh in range(2):
        for hh in range(2):
            P.add("tensor", lambda e, nh=nh, hh=hh: e.matmul(
                py[:, nh * 512:(nh + 1) * 512], lhsT=hid[:, hh, :], rhs=wd_t[:, hh, nh * 512:(nh + 1) * 512],
                start=(hh == 0), stop=(hh == 1)), r=[hidk, wkeys[2]], w=[pyk + (nh,)])


def phase_f(P, nc, g, experts=tuple(range(NEXP))):
    st = contextlib.ExitStack()
    with st:
        sb = lambda name, shape, dt: st.enter_context(nc.sbuf_tensor(name, shape, dt))
        ps = lambda name, shape, dt: st.enter_context(nc.psum_tensor(name, shape, dt))
        NS = 4
        ident = sb("f_ident", [128, 128], BF16)
        xs = [sb(f"f_xs{i}", [128, 2, 1024], BF16) for i in range(3)]
        wg32 = [sb(f"f_wg32_{i}", [128, 8, 256], F32) for i in range(NS)]
        wu32 = [sb(f"f_wu32_{i}", [128, 8, 256], F32) for i in range(NS)]
        wd32 = [sb(f"f_wd32_{i}", [128, 2, 1024], F32) for i in range(NS)]
        wg_ = [sb(f"f_wg{i}", [128, 8, 256], BF16) for i in range(2)]
        wu_ = [sb(f"f_wu{i}", [128, 8, 256], BF16) for i in range(2)]
        wd_ = [sb(f"f_wd{i}", [128, 2, 1024], BF16) for i in range(2)]
        xT = [sb(f"f_xT{i}", [128, 8, 128], BF16) for i in range(3)]
        sg = [sb(f"f_sg{i}", [128, 2, 128], F32) for i in range(2)]
        hid = [sb(f"f_hid{i}", [128, 2, 128], BF16) for i in range(2)]
        ys = [sb(f"f_ys{i}", [128, 2, 1024], BF16) for i in range(2)]
        ptr = [ps(f"f_ptr{i}", [128, 8, 128], BF16) for i in range(2)]
        pgu = [ps(f"f_pgu{i}", [128, 4, 128], F32) for i in range(2)]
        py = [ps(f"f_py{i}", [128, 1024], F32) for i in range(2)]
        P.add("sync", lambda e: e.dma_start(out=ident[:], in_=g.ident), w=["ident"], chan="f_c")
        ne = len(experts)

        def load(n_):
            ex = experts[n_]
            b3 = n_ % NS
            P.add("sync", lambda e, ex=ex, b3=b3: e.dma_start(
                out=wg32[b3][:], in_=g.w_gate_e[ex].rearrange("(p c) n -> p c n", p=128)), w=[("wg32", b3)],
                chan=f"f_wg{b3}")
            P.add("scalar", lambda e, ex=ex, b3=b3: e.dma_start(
                out=wu32[b3][:], in_=g.w_up_e[ex].rearrange("(p c) n -> p c n", p=128)), w=[("wu32", b3)],
                chan=f"f_wu{b3}")
            P.add("sync", lambda e, ex=ex, b3=b3: e.dma_start(
                out=wd32[b3][:], in_=g.w_down_e[ex].rearrange("(p c) n -> p c n", p=128)), w=[("wd32", b3)],
                chan=f"f_wd{b3}")

        def cast(n_):
            b3 = n_ % NS
            b2 = n_ % 2
            P.add("scalar", lambda e, b3=b3, b2=b2: e.activation(out=wg_[b2][:], in_=wg32[b3][:], func=AF.Copy),
                  r=[("wg32", b3)], w=[("wg", b2)])
            P.add("vector", lambda e, b3=b3, b2=b2: e.tensor_copy(out=wu_[b2][:], in_=wu32[b3][:]),
                  r=[("wu32", b3)], w=[("wu", b2)])
            P.add("scalar", lambda e, b3=b3, b2=b2: e.activation(out=wd_[b2][:, 0, :], in_=wd32[b3][:, 0, :], func=AF.Copy),
                  r=[("wd32", b3)], w=[("wd", b2, 0)])
            P.add("vector", lambda e, b3=b3, b2=b2: e.tensor_copy(out=wd_[b2][:, 1, :], in_=wd32[b3][:, 1, :]),
                  r=[("wd32", b3)], w=[("wd", b2, 1)])

        NH = CAP // 128
        items = [(n_, half) for n_ in range(ne) for half in range(NH)]

        def st_lx(n_):
            x3 = n_ % 3
            r0 = experts[n_] * CAP
            P.add("sync", lambda e, r0=r0, x3=x3: e.dma_start(
                out=xs[x3][:], in_=g.xs_d[r0:r0 + CAP, :].rearrange("(p h) d -> p h d", h=2)),
                w=[("xs", x3)], chan=f"f_xs{x3}")

        def st_t(i):
            n_, half = items[i]
            x3 = i % 3
            xe = n_ % 3
            b2 = i % 2
            for dc in range(8):
                P.add("tensor", lambda e, dc=dc, b2=b2, xe=xe, half=half: e.transpose(
                    out=ptr[b2][:, dc, :], in_=xs[xe][:, half, :].rearrange("t (p c) -> t c p", c=8)[:, dc, :],
                    identity=ident[:]),
                    r=[("xs", xe), "ident"], w=[("ptr", b2)])
            P.add("vector", lambda e, b2=b2, x3=x3: e.tensor_copy(out=xT[x3][:], in_=ptr[b2][:]),
                  r=[("ptr", b2)], w=[("xT", x3)])

        def st_gu(i):
            n_, half = items[i]
            x3 = i % 3
            b2 = i % 2
            wb = n_ % 2
            for j, wt in enumerate((wg_[wb], wu_[wb])):
                wk_ = ("wg", wb) if j == 0 else ("wu", wb)
                for hh in range(2):
                    for dc in range(8):
                        P.add("tensor", lambda e, j=j, wt=wt, hh=hh, dc=dc, b2=b2, x3=x3: e.matmul(
                            pgu[b2][:, 2 * j + hh, :], lhsT=wt[:, dc, :].rearrange("p (m h) -> p h m", h=2)[:, hh, :],
                            rhs=xT[x3][:, dc, :],
                            start=(dc == 0), stop=(dc == 7)), r=[wk_, ("xT", x3)], w=[("pgu", b2)])
            P.add("scalar", lambda e, b2=b2: e.activation(out=sg[b2][:], in_=pgu[b2][:, 0:2, :], func=AF.Silu),
                  r=[("pgu", b2)], w=[("sg", b2)])
            P.add("vector", lambda e, b2=b2: e.tensor_tensor(out=hid[b2][:], in0=sg[b2][:], in1=pgu[b2][:, 2:4, :],
                                                            op=ALU.mult),
                  r=[("sg", b2), ("pgu", b2)], w=[("hid", b2)])

        def st_dn(i):
            n_, half = items[i]
            b2 = i % 2
            wb = n_ % 2
            r0 = experts[n_] * CAP + half * 128
            for nh in range(2):
                for hh in range(2):
                    P.add("tensor", lambda e, nh=nh, hh=hh, b2=b2, wb=wb: e.matmul(
                        py[b2][:, nh * 512:(nh + 1) * 512], lhsT=hid[b2][:, hh, :],
                        rhs=wd_[wb][:, hh, nh * 512:(nh + 1) * 512], start=(hh == 0), stop=(hh == 1)),
                        r=[("hid", b2), ("wd", wb, hh)], w=[("py", b2, nh)])
            y2 = n_ % 2
            P.add("scalar", lambda e, b2=b2, y2=y2, half=half: e.activation(
                out=ys[y2][:, half, 0:512], in_=py[b2][:, 0:512], func=AF.Copy),
                r=[("py", b2, 0)], w=[("ys", y2, half, 0)])
            P.add("vector", lambda e, b2=b2, y2=y2, half=half: e.tensor_copy(
                out=ys[y2][:, half, 512:1024], in_=py[b2][:, 512:1024]),
                r=[("py", b2, 1)], w=[("ys", y2, half, 1)])
            if half == NH - 1:
                rbase = experts[n_] * CAP
                P.add("gpsimd", lambda e, rbase=rbase, y2=y2: e.dma_start(
                    out=g.ys_d[rbase:rbase + CAP, :].rearrange("(p h) d -> p h d", h=2), in_=ys[y2][:]),
                    r=[("ys", y2, h_, q_) for h_ in range(2) for q_ in range(2)], w=[("ys_d", rbase)],
                    chan=f"f_ys{y2}")

        load(0)
        if ne > 1:
            load(1)
        if ne > 2:
            load(2)
        cast(0)
        ni = len(items)
        st_lx(0)
        if ne > 1:
            st_lx(1)
        for step in range(ni + 2):
            if step < ni:
                n_t, half_t = items[step]
                if half_t == 0 and n_t + 2 < ne:
                    st_lx(n_t + 2)
                st_t(step)
            i1 = step - 1
            if 0 <= i1 < ni:
                n_, half = items[i1]
                if half == 0 and n_ + 3 < ne:
                    load(n_ + 3)
                st_gu(i1)
                if half == NH - 1 and n_ + 1 < ne:
                    cast(n_ + 1)
            i2 = step - 2
            if 0 <= i2 < ni:
                st_dn(i2)
        P.emit_phase()


def phase_g(P, nc, g, blocks=tuple(range(NOWN))):
    st = contextlib.ExitStack()
    with st:
        sb = lambda name, shape, dt: st.enter_context(nc.sbuf_tensor(name, shape, dt))
        ps = lambda name, shape, dt: st.enter_context(nc.psum_tensor(name, shape, dt))
        wgs = sb("g_wgs", [128, 8, 256], BF16)
        wus = sb("g_wus", [128, 8, 256], BF16)
        wds = sb("g_wds", [128, 2, 1024], BF16)
        g2rep = sb("g_g2", [128, 1024], F32)
        b2rep = sb("g_b2", [128, 1024], F32)
        h1 = [sb(f"g_h1_{i}", [128, 1024], F32) for i in range(2)]
        h1T = [sb(f"g_h1T{i}", [128, 8, 128], BF16) for i in range(2)]
        yk = [sb(f"g_yk{i}", [128, 1024], BF16) for i in range(6)]
        acc = sb("g_acc", [128, 1024], F32)
        sg = sb("g_sg", [128, 2, 128], F32)
        hid = sb("g_hid", [128, 2, 128], BF16)
        ot = [sb(f"g_ot{i}", [128, 1024], F32) for i in range(2)]
        stats = sb("g_stats", [128, 2, 6], F32)
        mv = sb("g_mv", [128, 2], F32)
        std = sb("g_std", [128, 1], F32)
        rstd = sb("g_rstd", [128, 1], F32)
        pgu = ps("g_pgu", [128, 4, 128], F32)
        py = ps("g_py", [128, 1024], F32)
        _bounds_reg(P, g)
        P.add("gpsimd", lambda e: e.dma_start(out=wgs[:], in_=g.w_gate_s.rearrange("(c p) n -> p c n", p=128)),
              w=["wgs"], chan="g_w")
        P.add("gpsimd", lambda e: e.dma_start(out=wus[:], in_=g.w_up_s.rearrange("(c p) n -> p c n", p=128)),
              w=["wus"], chan="g_w")
        P.add("gpsimd", lambda e: e.dma_start(out=wds[:], in_=g.w_down_s.rearrange("(c p) n -> p c n", p=128)),
              w=["wds"], chan="g_w")
        P.add("sync", lambda e: e.dma_start(out=g2rep[:], in_=g.ln2_g.partition_broadcast(128)), w=["g2rep"], chan="g_c")
        P.add("sync", lambda e: e.dma_start(out=b2rep[:], in_=g.ln2_b.partition_broadcast(128)), w=["b2rep"], chan="g_c")
        for i in range(6):
            P.add("gpsimd", lambda e, i=i: e.memset(yk[i][:], 0.0), w=[("yk", i)])
        yi = 0
        for blk in blocks:
            b2 = blk % 2
            P.add("sync", lambda e, blk=blk, b2=b2: e.dma_start(out=h1[b2][:], in_=g.h1_d[blk * 128:(blk + 1) * 128, :]),
                  w=[("h1", b2)], chan=f"g_h1{b2}")
            P.add("sync", lambda e, blk=blk, b2=b2: e.dma_start(out=h1T[b2][:], in_=g.h1T_d[:, :, blk * 128:(blk + 1) * 128]),
                  w=[("h1T", b2)], chan=f"g_h1T{b2}")
            for k in range(8):
                y3 = yi % 6
                yi += 1
                P.add("gpsimd", lambda e, blk=blk, k=k, y3=y3: e.indirect_dma_start(
                    out=yk[y3][:, :], out_offset=None, in_=g.ys_d[:, :],
                    in_offset=bass.IndirectOffsetOnAxis(ap=g.sidx[:, blk, k:k + 1], axis=0),
                    bounds_check=g.bcreg[0], oob_is_err=False),
                    r=[("sidx", blk)], w=[("yk", y3)], chan=f"g_yk{y3}")
                if k == 0:
                    P.add("vector", lambda e, blk=blk, y3=y3: e.tensor_scalar(
                        out=acc[:], in0=yk[y3][:], scalar1=g.gk[:, blk, 0:1], scalar2=None, op0=ALU.mult),
                        r=[("yk", y3), ("gk", blk)], w=["acc"])
                else:
                    P.add("vector", lambda e, blk=blk, k=k, y3=y3: e.scalar_tensor_tensor(
                        out=acc[:], in0=yk[y3][:], scalar=g.gk[:, blk, k:k + 1], in1=acc[:], op0=ALU.mult, op1=ALU.add),
                        r=[("yk", y3), ("gk", blk), "acc"], w=["acc"])
            _swiglu_block(P, h1T[b2], ("h1T", b2), wgs, wus, wds, ["wgs", "wus", "wds"],
                          pgu, "pgu", sg, "sg", hid, "hid", py, ("py",))
            o_t = ot[b2]
            P.add("vector", lambda e, b2=b2: e.scalar_tensor_tensor(
                out=acc[:], in0=h1[b2][:], scalar=ALPHA, in1=acc[:], op0=ALU.mult, op1=ALU.add),
                r=[("h1", b2), "acc"], w=["acc"])
            for nh in range(2):
                P.add("vector", lambda e, nh=nh: e.tensor_tensor(
                    out=acc[:, nh * 512:(nh + 1) * 512], in0=acc[:, nh * 512:(nh + 1) * 512],
                    in1=py[:, nh * 512:(nh + 1) * 512], op=ALU.add), r=["acc", ("py", nh)], w=["acc"])
            _layer_norm(P, acc, "acc", "acc", stats, mv, std, rstd, o_t, ("ot", b2), g2rep, "g2rep", b2rep, "b2rep", "g")
            P.add("scalar", lambda e, blk=blk, o_t=o_t: e.dma_start(out=g.out[blk * 128:(blk + 1) * 128, :], in_=o_t[:]),
                  r=[("ot", b2)], w=[("out", blk)], chan=f"g_out{b2}")
        P.emit_phase()


def build_program(debug=None, ntg=16, sb_groups=(0, 1, 2, 3), dl_slots=tuple(range(NOWN)), phases="abcdfg", d_groups=(0, 1, 2, 3),
                  f_experts=tuple(range(NEXP)), g_blocks=tuple(range(NOWN)), d_stop=9):
    nc = bass.Bass("TRN2", target_bir_lowering=False)
    g = Ctx()
    g.bcreg = []

    def din(name, shape, dt=F32):
        return nc.dram_tensor(name, shape, dt, kind="ExternalInput").ap()

    dbgnames = set(debug or ())

    def dscr(name, shape, dt):
        kind = "ExternalOutput" if name in dbgnames else "Internal"
        return nc.dram_tensor(name, shape, dt, kind=kind).ap()

    g.x_ctx = din("x_ctx", [NB * 128, D])
    g.valid = din("valid", [128, NB])
    g.ident = din("ident", [128, 128], BF16)
    g.negU = din("negU", [128, 128], BF16)
    g.negOnes = din("negOnes", [128, 128], BF16)
    g.sbmask = din("sbmask", [128, 16, 512], BF16)
    g.dlbias = din("dlbias", [128, DL_NT, 128], BF16)
    g.padbias = din("padbias", [128, NB])
    g.w_br_sb = din("w_br_sb", [512, D])
    g.w_br_dil = din("w_br_dil", [256, D])
    g.w_out = din("w_out", [D, D])
    g.w_router = din("w_router", [D, NEXP])
    g.b_gateT = din("b_gateT", [128, 16])
    g.ln1_g = din("ln1_g", [D])
    g.ln1_b = din("ln1_b", [D])
    g.router_bias = din("router_bias", [NEXP])
    ne_decl = NEXP if "f" in phases else 1
    g.w_gate_e = din("w_gate_e", [ne_decl, D, 256])
    g.w_up_e = din("w_up_e", [ne_decl, D, 256])
    g.w_down_e = din("w_down_e", [ne_decl, 256, D])
    g.w_gate_s = din("w_gate_s", [D, 256])
    g.w_up_s = din("w_up_s", [D, 256])
    g.w_down_s = din("w_down_s", [256, D])
    g.ln2_g = din("ln2_g", [D])
    g.ln2_b = din("ln2_b", [D])
    g.iota256 = din("iota256", [128, 256])
    g.lstrict = din("lstrict", [128, 128], BF16)
    g.ones = din("ones", [128, 128], BF16)
    g.ln_in_g = din("ln_in_g", [D])
    g.ln_in_b = din("ln_in_b", [D])
    g.ln_in_gT = din("ln_in_gT", [128, 8])
    g.ln_in_bT = din("ln_in_bT", [128, 8])
    g.w_in = din("w_in", [D, 5888])
    g.out = nc.dram_tensor("out", [TOWN, D], F32, kind="ExternalOutput").ap()

    g.kT_d = dscr("kT_d", [10, 128, S], BF16)
    g.v_d = dscr("v_d", [S, VW], BF16)
    g.qT_d = dscr("qT_d", [128, 10, TOWN], BF16)
    g.osbT_d = dscr("osbT_d", [8, 64, TOWN], BF16)
    g.odlT_d = dscr("odlT_d", [2, 128, TOWN], BF16)
    g.h_own_d = dscr("h_own_d", [TOWN, D], F32)
    g.h1_d = dscr("h1_d", [TOWN, D], F32)
    g.h1T_d = dscr("h1T_d", [128, 8, TOWN], BF16)
    g.xs_d = dscr("xs_d", [NEXP * CAP, D], BF16)
    g.ys_d = dscr("ys_d", [NEXP * CAP, D], BF16)
    g.hT_own_d = dscr("hT_own_d", [128, 8, TOWN], BF16)

    with contextlib.ExitStack() as stack:
        P = Prog(nc, stack)
        g.gk = stack.enter_context(nc.sbuf_tensor("gk", [128, NOWN, 8], F32))
        g.sidx = stack.enter_context(nc.sbuf_tensor("sidx", [128, NOWN, 8], I32))
        if "a" in phases:
            phase_a(P, nc, g, ntg)
        if "b" in phases:
            phase_b(P, nc, g, sb_groups)
        if "c" in phases:
            phase_c(P, nc, g, dl_slots)
        if "d" in phases:
            phase_d(P, nc, g, d_groups, d_stop)
        if "f" in phases:
            phase_f(P, nc, g, f_experts)
        if "g" in phases:
            phase_g(P, nc, g, g_blocks)
        if "gk" in dbgnames:
            dgk = nc.dram_tensor("dbg_gk", [128, NOWN, 8], F32, kind="ExternalOutput").ap()
            dsi = nc.dram_tensor("dbg_sidx", [128, NOWN, 8], I32, kind="ExternalOutput").ap()
            nbk = 4 * len(d_groups)
            P.add("sync", lambda e: e.dma_start(out=dgk[:, 0:nbk, :], in_=g.gk[:, 0:nbk, :]), w=["dgk"], chan="dbg")
            P.add("sync", lambda e: e.dma_start(out=dsi[:, 0:nbk, :], in_=g.sidx[:, 0:nbk, :]), w=["dsi"], chan="dbg")
            P.emit_phase()
    return nc


def _rel_bucket_np(dist):
    dist = np.asarray(dist, np.int64)
    max_exact = 16
    d = np.maximum(dist, 1).astype(np.float32)
    large = max_exact + (np.log(d / np.float32(max_exact)) / np.float32(np.log(2048 / 16))
                         * np.float32(32 - max_exact)).astype(np.int32)
    large = np.minimum(large, 31)
    return np.where(dist < max_exact, dist, large)


def host_consts(inputs):
    bf = ml_dtypes.bfloat16
    kl = np.arange(128)[:, None]
    ql = np.arange(128)[None, :]
    ident = np.eye(128, dtype=np.float32).astype(bf)
    negU = np.where(kl >= ql, -1.0, 0.0).astype(np.float32).astype(bf)
    negOnes = np.full((128, 128), -1.0, np.float32).astype(bf)
    sbmask = np.zeros((128, 16, 512), np.float32)
    for rel_c in range(16):
        for sl in range(4):
            rel_cq = 4 * sl + 3
            if rel_c == rel_cq:
                sbmask[:, rel_c, sl * 128:(sl + 1) * 128] = np.where(kl < ql, 0.0, NEG)
            elif rel_c > rel_cq:
                sbmask[:, rel_c, sl * 128:(sl + 1) * 128] = NEG
    rel_bias = np.asarray(inputs["rel_bias"], np.float32)
    dlbias = np.zeros((128, DL_NT, 128), np.float32)
    for gi, (w, dil) in enumerate(DIL):
        for hg in range(4):
            for o in range(DL_NB[gi]):
                dist = 128 * o + ql - kl
                ok = (dist >= 0) & (dist <= w) & (dist % dil == 0)
                bk = _rel_bucket_np(np.clip(dist, 0, None))
                val = rel_bias[bk, 4 * gi + hg]
                dlbias[:, DL_TOFF[gi] + hg * DL_NB[gi] + o, :] = np.where(ok, val, NEG)
    f32 = lambda k: np.ascontiguousarray(np.asarray(inputs[k], np.float32)[0])
    return dict(ident=ident, negU=negU, negOnes=negOnes, sbmask=sbmask.astype(bf), dlbias=dlbias.astype(bf),
                w_br_sb=f32("w_br_sb"), w_br_dil=f32("w_br_dil"), w_out=f32("w_out"), w_router=f32("w_router"),
                b_gateT=np.ascontiguousarray(f32("b_gate").reshape(16, 128).T),
                ln1_g=f32("ln1_g"), ln1_b=f32("ln1_b"), router_bias=f32("router_bias"),
                w_gate_e=f32("w_gate_e"), w_up_e=f32("w_up_e"), w_down_e=f32("w_down_e"),
                w_gate_s=f32("w_gate_s"), w_up_s=f32("w_up_s"), w_down_s=f32("w_down_s"),
                ln2_g=f32("ln2_g"), ln2_b=f32("ln2_b"),
                iota256=np.tile(np.arange(256, dtype=np.float32)[None, :], (128, 1)),
                lstrict=np.where(kl < ql, 1.0, 0.0).astype(np.float32).astype(bf),
                ones=np.ones((128, 128), np.float32).astype(bf))


def host_inputs(inputs):
    x = np.asarray(inputs["x"], dtype=np.float32)
    maps = []
    consts = host_consts(inputs)
    for core in range(NCORES):
        b, j = core // 4, core % 4
        xc = np.zeros((NB, 128, D), np.float32)
        valid = np.zeros((128, NB), np.float32)
        xb = x[b].reshape(64, 128, D)
        for c in range(NB):
            gb = c + j - 3
            if gb >= 0:
                xc[c] = xb[gb]
                valid[:, c] = 1.0
        m = {
            "x_ctx": xc.reshape(NB * 128, D),
            "valid": valid,
            "padbias": np.where(valid > 0, 0.0, NEG).astype(np.float32),
            "ln_in_g": np.asarray(inputs["ln_in_g"], np.float32),
            "ln_in_b": np.asarray(inputs["ln_in_b"], np.float32),
            "ln_in_gT": np.ascontiguousarray(np.asarray(inputs["ln_in_g"], np.float32).reshape(8, 128).T),
            "ln_in_bT": np.ascontiguousarray(np.asarray(inputs["ln_in_b"], np.float32).reshape(8, 128).T),
            "w_in": np.ascontiguousarray(np.asarray(inputs["w_in"], np.float32)[0]),
        }
        m.update(consts)
        maps.append(m)
    return maps


def kernel(**inputs):
    nc = build_program()
    maps = host_inputs(inputs)
    res = run_bass_kernel_spmd(nc, maps, core_ids=list(range(NCORES)))
    out = np.zeros((2, S, D), np.float32)
    for core in range(NCORES):
        b, j = core // 4, core % 4
        o = res.results[core]["out"].reshape(NOWN, 128, D)
        ob = out[b].reshape(64, 128, D)
        for s in range(NOWN):
            ob[4 * s + j] = o[s]
    return out
```
